# Optimizing a Trainium2 kernel written in Bass

```python
import jax
import jax.numpy as jnp
from jax import lax
import numpy as np

D_MODEL = 1024
BATCH = 8
SEQ = 4096
DEPTH = 1

GRID_W = 64
CTX_LEN = 256
MIX_WIDTH = D_MODEL
HG_WIDTH = MIX_WIDTH // 2
HG_HEADS = 4
HG_KDIM = 128
HG_KEYS = HG_HEADS * HG_KDIM
HG_VDIM = HG_WIDTH // HG_HEADS
SC_WIDTH = MIX_WIDTH - HG_WIDTH
CONV_W = 3
CHUNK = 32
N_EXPERTS = 16
EC_CAPACITY = 2
EXPERT_DFF = 2 * D_MODEL
N_MOD = 6
EPS = 1e-6
IN_COLS = 3 * HG_KEYS + 2 * HG_WIDTH + 3 * SC_WIDTH

kernel_name = 'hybrid_hgrn2_shortconv_ec_moe_dit'


def rms_norm(x, gain):
    xf = x.astype(jnp.float32)
    y = xf * lax.rsqrt(jnp.mean(xf * xf, axis=-1, keepdims=True) + EPS)
    return (y * gain.astype(jnp.float32)).astype(x.dtype)


def modulate(h, shift, scale):
    return h * (1 + scale) + shift


def adaln(cond, w, b):
    return jnp.split(jax.nn.silu(cond) @ w + b, N_MOD, axis=-1)


def dwconv3(u, w):
    pad = [(0, 0)] * (u.ndim - 2) + [(1, 1), (0, 0)]
    up = jnp.pad(u, pad)
    return w[0] * up[..., :-2, :] + w[1] * up[..., 1:-1, :] + w[2] * up[..., 2:, :]


def split_proj(z):
    cuts = [HG_KEYS, 2 * HG_KEYS, 3 * HG_KEYS, 3 * HG_KEYS + HG_WIDTH, 3 * HG_KEYS + 2 * HG_WIDTH,
            3 * HG_KEYS + 2 * HG_WIDTH + SC_WIDTH, 3 * HG_KEYS + 2 * HG_WIDTH + 2 * SC_WIDTH]
    return jnp.split(z, cuts, axis=-1)


def hgrn2_gates(zf, lb):
    b, t, _ = zf.shape
    f = lb + (1.0 - lb) * jax.nn.sigmoid(zf.astype(jnp.float32))
    return jnp.log(f).reshape(b, t, HG_HEADS, HG_KDIM), (1.0 - f).reshape(b, t, HG_HEADS, HG_KDIM)


def gla_chunked(q, k, v, log_f, s0):
    b, t, h, _ = q.shape
    dv = v.shape[-1]
    n = t // CHUNK
    q, k, v, log_f = [a.astype(jnp.float32).reshape(b, n, CHUNK, h, a.shape[-1]) for a in (q, k, v, log_f)]
    cum = jnp.cumsum(log_f, axis=2)
    last = cum[:, :, -1:]
    q_dec = q * jnp.exp(cum)
    k_inv = k * jnp.exp(-cum)
    k_end = k * jnp.exp(last - cum)
    lower = jnp.tril(jnp.ones((CHUNK, CHUNK), jnp.bool_))
    scores = jnp.where(lower, jnp.einsum('bnihd,bnjhd->bnhij', q_dec, k_inv), 0.0)
    o_intra = jnp.einsum('bnhij,bnjhv->bnihv', scores, v)
    kv = jnp.einsum('bnjhd,bnjhv->bnhdv', k_end, v)
    decay = jnp.exp(last[:, :, 0])

    def step(s, inp):
        dec, kv_n = inp
        return dec[..., None] * s + kv_n, s

    s_final, s_prev = lax.scan(step, s0.astype(jnp.float32),
                               (jnp.moveaxis(decay, 1, 0), jnp.moveaxis(kv, 1, 0)))
    o_inter = jnp.einsum('bnihd,nbhdv->bnihv', q_dec, s_prev)
    return (o_intra + o_inter).reshape(b, t, h, dv), s_final


def gla_final_state(k, v, log_f):
    cum = jnp.cumsum(log_f, axis=1)
    return jnp.einsum('bthd,bthv->bhdv', k * jnp.exp(cum[:, -1:] - cum), v)


def hgrn2_mix(q, i, zf_f, zf_b, g, lb_f, lb_b, s0_f, s0_b, gain):
    b, t, _ = q.shape
    qh = q.reshape(b, t, HG_HEADS, HG_KDIM)
    vh = i.reshape(b, t, HG_HEADS, HG_VDIM)
    logf_f, k_f = hgrn2_gates(zf_f, lb_f)
    logf_b, k_b = hgrn2_gates(zf_b, lb_b)
    rev = lambda a: jnp.flip(a, axis=1)
    o_f, s_f = gla_chunked(qh, k_f, vh, logf_f, s0_f)
    o_b, s_b = gla_chunked(rev(qh), rev(k_b), rev(vh), rev(logf_b), s0_b)
    o = (o_f + rev(o_b)).astype(q.dtype)
    o = rms_norm(o, gain) * jax.nn.silu(g.reshape(b, t, HG_HEADS, HG_VDIM))
    return o.reshape(b, t, HG_WIDTH), s_f, s_b


def hgrn2_context_states(zf_f, zf_b, i, lb_f, lb_b):
    b, t, _ = i.shape
    vh = i.astype(jnp.float32).reshape(b, t, HG_HEADS, HG_VDIM)
    logf_f, k_f = hgrn2_gates(zf_f, lb_f)
    logf_b, k_b = hgrn2_gates(zf_b, lb_b)
    s_f = gla_final_state(k_f, vh, logf_f)
    s_b = gla_final_state(jnp.flip(k_b, 1), jnp.flip(vh, 1), jnp.flip(logf_b, 1))
    return s_f, s_b


def token_mix(h, w_in_l, w_out_l, lb_f, lb_b, hg_gain, conv_l, s0_f, s0_b, grid_rows):
    b, t, _ = h.shape
    q, zf_f, zf_b, i, g, gate_b, gate_c, v = split_proj(h @ w_in_l)
    o_hg, s_f, s_b = hgrn2_mix(q, i, zf_f, zf_b, g, lb_f, lb_b, s0_f, s0_b, hg_gain)
    u = gate_c * v
    if grid_rows is None:
        y = dwconv3(u, conv_l)
    else:
        y = dwconv3(u.reshape(b, grid_rows, GRID_W, SC_WIDTH), conv_l).reshape(b, t, SC_WIDTH)
    y = gate_b * y
    return jnp.concatenate([o_hg, y], axis=-1) @ w_out_l, s_f, s_b


def expert_choice_ffn(h, w_router, w_gate, w_up, w_down):
    b, t, d = h.shape
    cap = EC_CAPACITY * t // N_EXPERTS
    aff = jax.nn.softmax(jnp.einsum('btd,de->bte', h, w_router).astype(jnp.float32), axis=-1)
    gate, idx = lax.top_k(jnp.swapaxes(aff, 1, 2), cap)
    xs = jax.vmap(lambda hb, ib: hb[ib])(h, idx)
    hid = jax.nn.silu(jnp.einsum('becd,edf->becf', xs, w_gate)) * jnp.einsum('becd,edf->becf', xs, w_up)
    ys = jnp.einsum('becf,efd->becd', hid, w_down) * gate[..., None].astype(h.dtype)
    return jax.vmap(lambda ib, yb: jnp.zeros((t, d), yb.dtype).at[ib.reshape(-1)].add(yb.reshape(-1, d)))(idx, ys)


def setup_inputs(seed: int = 0) -> dict:
    key = jax.random.key(seed)
    ks = jax.random.split(key, 20)
    nrm = lambda k, shape, s: jax.random.normal(k, shape, jnp.float32) * s
    return {
        'x': nrm(ks[0], (BATCH, SEQ, D_MODEL), 1.0),
        'c': nrm(ks[1], (BATCH, D_MODEL), 1.0),
        'ctx': nrm(ks[2], (BATCH, CTX_LEN, D_MODEL), 1.0),
        'c_ctx': nrm(ks[3], (D_MODEL,), 1.0),
        'w_ada': nrm(ks[4], (DEPTH, D_MODEL, N_MOD * D_MODEL), 0.5 * D_MODEL ** -0.5),
        'b_ada': nrm(ks[5], (DEPTH, N_MOD * D_MODEL), 0.02),
        'norm_mix': 1.0 + nrm(ks[6], (DEPTH, D_MODEL), 0.02),
        'norm_ffn': 1.0 + nrm(ks[7], (DEPTH, D_MODEL), 0.02),
        'w_in': nrm(ks[8], (DEPTH, D_MODEL, IN_COLS), D_MODEL ** -0.5),
        'lb_logits': nrm(ks[9], (DEPTH + 1, 2, HG_KEYS), 0.3),
        'hg_norm': 1.0 + nrm(ks[10], (DEPTH, HG_VDIM), 0.02),
        'conv_w': nrm(ks[11], (DEPTH, CONV_W, SC_WIDTH), CONV_W ** -0.5),
        'w_out': nrm(ks[12], (DEPTH, MIX_WIDTH, D_MODEL), MIX_WIDTH ** -0.5),
        'w_router': nrm(ks[13], (DEPTH, D_MODEL, N_EXPERTS), D_MODEL ** -0.5),
        'w_gate': nrm(ks[14], (DEPTH, N_EXPERTS, D_MODEL, EXPERT_DFF), D_MODEL ** -0.5),
        'w_up': nrm(ks[15], (DEPTH, N_EXPERTS, D_MODEL, EXPERT_DFF), D_MODEL ** -0.5),
        'w_down': nrm(ks[16], (DEPTH, N_EXPERTS, EXPERT_DFF, D_MODEL), EXPERT_DFF ** -0.5),
        'norm_final': 1.0 + nrm(ks[17], (D_MODEL,), 0.02),
    }


def reference(x, c, ctx, c_ctx, w_ada, b_ada, norm_mix, norm_ffn, w_in, lb_logits, hg_norm, conv_w,
              w_out, w_router, w_gate, w_up, w_down, norm_final):
    b = x.shape[0]
    rows = x.shape[1] // GRID_W
    lower_bounds = jnp.cumsum(jax.nn.softmax(lb_logits.astype(jnp.float32), axis=0), axis=0)
    cx = ctx
    for l in range(DEPTH):
        sh1, sc1, g1, sh2, sc2, g2 = [m[:, None] for m in adaln(c, w_ada[l], b_ada[l])]
        csh1, csc1, cg1, csh2, csc2, cg2 = adaln(c_ctx, w_ada[l], b_ada[l])
        lb_f, lb_b = lower_bounds[l, 0], lower_bounds[l, 1]
        hc = modulate(rms_norm(cx, norm_mix[l]), csh1, csc1)
        if l == DEPTH - 1:
            zf_f, zf_b, i_c = jnp.split(hc @ w_in[l][:, HG_KEYS:3 * HG_KEYS + HG_WIDTH],
                                        [HG_KEYS, 2 * HG_KEYS], axis=-1)
            s_f, s_b = hgrn2_context_states(zf_f, zf_b, i_c, lb_f, lb_b)
        else:
            zero = jnp.zeros((b, HG_HEADS, HG_KDIM, HG_VDIM), jnp.float32)
            mc, s_f, s_b = token_mix(hc, w_in[l], w_out[l], lb_f, lb_b, hg_norm[l], conv_w[l],
                                     zero, zero, None)
            cx = cx + cg1 * mc
            hc2 = modulate(rms_norm(cx, norm_ffn[l]), csh2, csc2)
            cx = cx + cg2 * expert_choice_ffn(hc2, w_router[l], w_gate[l], w_up[l], w_down[l])
        hx = modulate(rms_norm(x, norm_mix[l]), sh1, sc1)
        mx, _, _ = token_mix(hx, w_in[l], w_out[l], lb_f, lb_b, hg_norm[l], conv_w[l], s_f, s_b, rows)
        x = x + g1 * mx
        hx2 = modulate(rms_norm(x, norm_ffn[l]), sh2, sc2)
        x = x + g2 * expert_choice_ffn(hx2, w_router[l], w_gate[l], w_up[l], w_down[l])
    return rms_norm(x, norm_final)
```

```python
import numpy as np
import concourse.bass as bass
import concourse.mybir as mybir
from concourse.bass_utils import run_bass_kernel_spmd

F32 = mybir.dt.float32
BF16 = mybir.dt.bfloat16
I32 = mybir.dt.int32
F32R = mybir.dt.float32r
AF = mybir.ActivationFunctionType
ALU = mybir.AluOpType
AX = mybir.AxisListType

ENGS = ("pe", "act", "dve", "pool", "sp")


class Counter:
    def __init__(self, nc, name, step, epoch):
        self.nc, self.name, self.step, self.epoch = nc, name, step, epoch
        self.n = 0
        self.sems = []

    def next(self):
        self.n += 1
        e = (self.n - 1) // self.epoch
        while len(self.sems) <= e:
            self.sems.append(self.nc.alloc_semaphore(f"{self.name}_e{len(self.sems)}"))
        return self.n

    def sem_of(self, n):
        e = (n - 1) // self.epoch
        return self.sems[e], (n - e * self.epoch) * self.step


class Buf:
    __slots__ = ("name", "w", "r", "ctr")

    def __init__(self, name=""):
        self.name = name
        self.w = {}
        self.r = {}
        self.ctr = None


class Sched:
    def __init__(self, nc):
        self.nc = nc
        self.q = {e: [] for e in ENGS}
        self.eng_ctr = {e: Counter(nc, f"tk_{e}", 1, 12000) for e in ("pe", "act", "dve", "pool")}
        self.waited = {e: {} for e in ENGS}
        self.finals = {}
        self.n_dma_ctr = 0
        self.nbuf = 0
        self.scopes = []
        self.all_dma_ctrs = []

    def sb(self, name, shape, dt):
        assert not self.scopes, "persistent alloc inside scope: " + name
        return self.nc.alloc_sbuf_tensor("sb_" + name, list(shape), dt).ap()

    def ps(self, name, shape, dt=F32):
        return self.nc.alloc_psum_tensor("ps_" + name, list(shape), dt).ap()

    def buf(self, name=None):
        self.nbuf += 1
        return Buf(name or f"b{self.nbuf}")

    def _ctr_for(self, b):
        if b.ctr is None:
            self.n_dma_ctr += 1
            b.ctr = Counter(self.nc, f"dq{self.n_dma_ctr}", 16, 900)
            self.all_dma_ctrs.append(b.ctr)
        return b.ctr

    def _deps(self, eng, reads, writes):
        need = {}

        def add(c, n, src_eng):
            if src_eng == eng and eng == "pe":
                return
            if need.get(c, 0) < n:
                need[c] = n

        for b in reads:
            for c, (n, se) in b.w.items():
                add(c, n, se)
        for b in writes:
            for c, (n, se) in b.w.items():
                if se == eng and se != "dma":
                    continue
                add(c, n, se)
            for c, (n, se) in b.r.items():
                if se == eng and se != "dma":
                    continue
                add(c, n, se)
        waits = []
        wd = self.waited[eng]
        for c, n in need.items():
            if wd.get(c, 0) >= n:
                continue
            wd[c] = n
            waits.append(c.sem_of(n))
        return waits

    def _record(self, c, n, src_eng, reads, writes):
        for b in reads:
            old = b.r.get(c)
            if old is None or old[0] < n:
                b.r[c] = (n, src_eng)
        for b in writes:
            b.w = {c: (n, src_eng)}
            b.r = {}

    def op(self, eng, fn, reads=(), writes=()):
        waits = self._deps(eng, reads, writes)
        c = self.eng_ctr[eng]
        n = c.next()
        sem, _ = c.sem_of(n)
        self._record(c, n, eng, reads, writes)

        def emit(e, waits=waits, fn=fn, sem=sem):
            for s, v in waits:
                e.wait_ge(s, v)
            ins = fn(e)
            ins.then_inc(sem, 1)
        self.q[eng].append(emit)

    def dma(self, qeng, out, in_, reads=(), writes=(), slot=None, out_final=False, **kw):
        self.op_dma_ind(qeng, lambda e: e.dma_start(out=out, in_=in_, **kw), reads=reads, writes=writes,
                        slot=slot, out_final=out_final)

    def op_dma_ind(self, qeng, fn, reads=(), writes=(), slot=None, out_final=False):
        if slot is None:
            slot = writes[0] if len(writes) else reads[0]
        waits = self._deps(qeng, reads, writes)
        c = self._ctr_for(slot)
        n = c.next()
        sem, _ = c.sem_of(n)
        self._record(c, n, "dma", reads, writes)
        if out_final:
            self.finals[c] = n

        def emit(e, waits=waits, fn=fn, sem=sem):
            for s, v in waits:
                e.wait_ge(s, v)
            fn(e).then_inc(sem, 16)
        self.q[qeng].append(emit)

    def scope_push(self):
        from contextlib import ExitStack
        st = ExitStack()
        self.scopes.append(st)

    def sbs(self, name, shape, dt):
        return self.scopes[-1].enter_context(self.nc.sbuf_tensor("sb_" + name, list(shape), dt)).ap()

    def scope_pop(self):
        self.barrier()
        self.scopes.pop().close()

    def barrier(self):
        ctrs = list(self.eng_ctr.values()) + self.all_dma_ctrs
        for eng in ENGS:
            waits = []
            wd = self.waited[eng]
            for c in ctrs:
                if c.n > 0 and wd.get(c, 0) < c.n:
                    wd[c] = c.n
                    waits.append(c.sem_of(c.n))

            def emit(e, waits=waits):
                for s_, v in waits:
                    e.wait_ge(s_, v)
            self.q[eng].append(emit)

    def finish(self):
        nc = self.nc
        finals = [c.sem_of(n) for c, n in self.finals.items()]
        q = self.q
        with nc.Block() as block:
            @block.sync
            def _(e):
                for f in q["sp"]:
                    f(e)
                for s, v in finals:
                    e.wait_ge(s, v)

            @block.tensor
            def _(e):
                for f in q["pe"]:
                    f(e)

            @block.scalar
            def _(e):
                for f in q["act"]:
                    f(e)

            @block.vector
            def _(e):
                for f in q["dve"]:
                    f(e)

            @block.gpsimd
            def _(e):
                for f in q["pool"]:
                    f(e)


def _const_layout():
    lay = {}
    off = 0
    for name, n in (("ident", 128), ("maskf", 128), ("maskb", 128), ("mgt", 128), ("mlt", 128), ("ones", 128),
                    ("rsf", 512), ("rsb", 512), ("cind", 4), ("iota", 512), ("tokab", 64), ("bones", 128),
                    ("wcomb", 2), ("segpre", 128), ("tokidx", 32)):
        lay[name] = (off, n)
        off += n
    return lay, off


CL, NCONST = _const_layout()


def make_consts():
    c = np.zeros((128, NCONST), np.float32)

    def put(name, arr):
        o, n = CL[name]
        c[:arr.shape[0], o:o + n] = arr
    p = np.arange(128)
    put("ident", np.eye(128, dtype=np.float32))
    j = p[:, None]; i = p[None, :]
    same = (j // 32) == (i // 32)
    mf = (same & (j <= i)).astype(np.float32)
    mb = (same & (j >= i)).astype(np.float32)
    put("maskf", mf); put("maskb", mb)
    put("mgt", (j > i).astype(np.float32))
    put("mlt", (j < i).astype(np.float32))
    put("ones", np.ones((128, 128), np.float32))
    t = np.arange(512)
    put("rsf", np.broadcast_to((t % 32 != 0).astype(np.float32), (128, 512)))
    put("rsb", np.broadcast_to((t % 32 != 31).astype(np.float32), (128, 512)))
    put("cind", (p[:, None] // 32 == np.arange(4)[None, :]).astype(np.float32))
    put("iota", np.broadcast_to(t.astype(np.float32), (128, 512)))
    tok = (np.arange(32)[None, :] * 128 + p[:, None])
    ab = np.stack([tok // 64, tok % 64], axis=-1).reshape(128, 64).astype(np.float32)
    put("tokab", ab)
    put("bones", ((p[:, None] // 8) == (p[None, :] // 8)).astype(np.float32))
    wc = np.zeros((128, 2), np.float32)
    wc[0, 0] = 64.0; wc[1, 0] = 1.0; wc[2, 1] = 1.0; wc[3, 1] = 1.0; wc[4, 1] = 1.0
    put("wcomb", wc)
    put("segpre", (((p[:, None] // 8) == (p[None, :] // 8)) & ((p[:, None] % 8) < (p[None, :] % 8))).astype(np.float32))
    cs = np.arange(32)
    put("tokidx", ((cs[None, :] % 8) * 512 + (cs[None, :] // 8) * 128 + p[:, None]).astype(np.float32))
    return c


T_SEQ, D_MODEL, N_EXP, CAP, DFF = 4096, 1024, 16, 512, 2048
HAS_MOE = True
EPS = 1e-6


def build_program(stage=99, dbg=False):
    nc = bass.Bass("TRN2", target_bir_lowering=False)
    nc.dge_precook = False
    S = Sched(nc)

    def din(name, shape, dt=F32):
        return nc.dram_tensor(name, list(shape), dt, kind="ExternalInput").ap()

    def dscr(name, shape, dt=F32, out=False):
        if out:
            return nc.dram_tensor(name, list(shape), dt, kind="ExternalOutput").ap()
        return nc.dram_tensor(name, list(shape), dt).ap()

    x_d = din("x", [T_SEQ, D_MODEL]); ctx_d = din("ctx", [256, D_MODEL]); cc_d = din("ccT", [128, 16])
    wada_d = din("w_ada", [1024, 6144], F32R); bada_d = din("b_ada", [1, 6144])
    nmix_d = din("nmix_fm", [128, 8]); nffn_d = din("nffn_bc", [128, 1024]); nfin_d = din("nfin_bc", [128, 1024])
    win_d = din("w_in", [1024, 4096], F32R)
    lblfm_d = din("lbl_fm", [128, 16]); lblbc_d = din("lbl_bc", [128, 2048])
    hgn_d = din("hgn_fm", [128, 1]); convw_d = din("convw_fm", [128, 12])
    wout_d = din("w_out", [1024, 1024], F32R); wr_d = din("w_r_fm", [128, 128])
    if stage >= 4 and HAS_MOE:
        wg_d = din("w_gate", [N_EXP, 1024, DFF], F32R); wu_d = din("w_up", [N_EXP, 1024, DFF], F32R)
        wd_d = din("w_down", [N_EXP, DFF, 1024], F32R)
    consts_d = din("consts", [128, NCONST])
    out_d = nc.dram_tensor("out", [T_SEQ, D_MODEL], F32, kind="ExternalOutput").ap()
    x2_d = dscr("x2", [T_SEQ, D_MODEL], F32, out=dbg)
    hx2_d = dscr("hx2", [T_SEQ, D_MODEL], F32, out=dbg)
    st_qd = dscr("st_qd", [8, 128, 4, 512], BF16); st_ki = dscr("st_ki", [8, 128, 4, 512], BF16)
    st_ke = dscr("st_ke", [8, 128, 4, 4, 128], BF16); st_v = dscr("st_v", [8, 128, 4, 512], BF16)
    st_el = dscr("st_el", [8, 128, 4, 16], F32); st_of = dscr("st_of", [8, 128, 4, 512], F32)
    st_sg = dscr("st_sg", [8, 128, 4, 512], BF16); st_yb = dscr("st_yb", [8, 128, 4, 512], F32R)
    dbg_d = {}
    if dbg:
        dbg_d["s0"] = dscr("dbg_s0", [8, 128, 128], F32, out=True)
        dbg_d["aff"] = dscr("dbg_aff", [16, T_SEQ], F32, out=True)
        dbg_d["of"] = dscr("dbg_of", [8, 128, 4, 512], F32, out=True)
        dbg_d["mod"] = dscr("dbg_mod", [2, 6144], F32, out=True)

    def act(out, in_, func, rd, wr, **kw):
        S.op("act", lambda e: e.activation(out, in_, func, **kw), reads=rd, writes=wr)

    def tt(eng, out, a, b, op, rd, wr):
        S.op(eng, lambda e: e.tensor_tensor(out, a, b, op), reads=rd, writes=wr)

    def ts(eng, out, a, s1, s2, op0, op1, rd, wr):
        if s2 is None:
            S.op(eng, lambda e: e.tensor_scalar(out, a, s1, None, op0), reads=rd, writes=wr)
        else:
            S.op(eng, lambda e: e.tensor_scalar(out, a, s1, s2, op0, op1), reads=rd, writes=wr)

    def stt(out, a, s, b, op0, op1, rd, wr):
        S.op("dve", lambda e: e.scalar_tensor_tensor(out, a, s, b, op0, op1), reads=rd, writes=wr)

    def cp(eng, out, in_, rd, wr):
        if eng == "act":
            S.op("act", lambda e: e.activation(out, in_, AF.Copy), reads=rd, writes=wr)
        else:
            S.op(eng, lambda e: e.tensor_copy(out, in_), reads=rd, writes=wr)

    def mm(lst, rd, wr):
        def f(e, lst=lst):
            ins = None
            for (o, l, r, st, sp) in lst:
                ins = e.matmul(o, lhsT=l, rhs=r, start=st, stop=sp)
            return ins
        S.op("pe", f, reads=rd, writes=wr)

    def trs(lst, rd, wr):
        def f(e, lst=lst):
            ins = None
            for (o, i, idn) in lst:
                ins = e.transpose(o, i, idn)
            return ins
        S.op("pe", f, reads=rd, writes=wr)

    ipb = [(S.ps(f"ip{i}", [128, 512], F32), S.buf(f"ip{i}")) for i in range(4)]
    ipn = [0]

    def bank():
        r = ipb[ipn[0] % 4]
        ipn[0] += 1
        return r
    sc_ps, Bsc = S.ps("sc", [128, 512], F32), S.buf("sc")
    kv_ps, Bkv = S.ps("kv", [128, 512], F32), S.buf("kv")
    o_ps, Bo = S.ps("o", [128, 512], F32), S.buf("o")
    trb_ps, Btrb = S.ps("trb", [128, 512], F32), S.buf("trb")

    consts = S.sb("consts", [128, NCONST], F32); Bc = S.buf("consts")
    S.dma("sp", consts, consts_d, writes=[Bc])

    def C(name, rows=128, lo=0, hi=None):
        o, n = CL[name]
        return consts[0:rows, o + lo: o + (n if hi is None else hi)]
    ident = C("ident")
    identb = None; Bib = S.buf()
    onesr = S.sb("onesr", [128, 128], F32R); Bor = S.buf()
    cp("dve", onesr, C("ones"), [Bc], [Bor])
    mhalf = S.sb("mhalf", [128, 1], F32); Bmh = S.buf()
    S.op("pool", lambda e: e.memset(mhalf, -0.5), writes=[Bmh])

    def small_in(name, src, shape):
        t = S.sb(name, shape, F32); b = S.buf(name)
        S.dma("sp", t, src, writes=[b])
        return t, b
    ccT, Bcc = small_in("ccT", cc_d, [128, 16])
    nmix, Bnm = small_in("nmix", nmix_d, [128, 8])
    lblfm, Blf = small_in("lblfm", lblfm_d, [128, 16])
    hgn, Bhg = small_in("hgn", hgn_d, [128, 1])
    convw, Bcw = small_in("convw", convw_d, [128, 12])
    wr, Bwr = small_in("wr", wr_d, [128, 128])
    nfin, Bnf = small_in("nfin", nfin_d, [128, 1024])
    lbfm = S.sb("lbfm", [128, 8], F32); omlfm = S.sb("omlfm", [128, 8], F32); nomlfm = S.sb("nomlfm", [128, 8], F32)
    tmp8 = S.sb("tmp8", [128, 8], F32); Bl = S.buf(); Bt8 = S.buf()
    tt("dve", tmp8, lblfm[:, 0:8], lblfm[:, 8:16], ALU.subtract, [Blf], [Bt8])
    act(lbfm, tmp8, AF.Sigmoid, [Bt8], [Bl])
    ts("dve", omlfm, lbfm, -1.0, 1.0, ALU.mult, ALU.add, [Bl], [Bl])
    ts("dve", nomlfm, lbfm, 1.0, -1.0, ALU.mult, ALU.add, [Bl], [Bl])

    S32 = S.sb("S32", [128, 8, 128], F32)
    S16 = S.sb("S16", [128, 2, 8, 128], F32R)
    BS32 = [S.buf(f"S32_{i}") for i in range(8)]
    BS16 = [[S.buf(f"S16_{v}_{i}") for i in range(8)] for v in range(2)]
    sver = [0] * 8

    A1 = S.sb("A1", [128, 8], F32); B1 = S.sb("B1", [128, 8], F32)
    Ac = S.sb("Acx", [128, 8], F32); Bcx = S.sb("Bcx", [128, 8], F32); Bab = S.buf("A1B1")
    g1bc = S.sb("g1bc", [128, 1024], F32); A2bc = S.sb("A2bc", [128, 1024], F32)
    B2bc = S.sb("B2bc", [128, 1024], F32); g2bc = S.sb("g2bc", [128, 1024], F32); Bbc = S.buf("bcs")

    sc = S.sb("silu_c", [128, 16], F32R); Bscx = S.buf()
    modT = S.sb("modT", [128, 32], F32); BmT = S.buf()
    ss_t = S.sb("ss_t", [128, 4], F32); Bss = [S.buf() for _ in range(4)]
    S.scope_push()
    wsl = [(S.sbs(f"wsl{i}", [128, 8, 512], F32R), S.buf(f"wsl{i}")) for i in range(2)]
    wsn = [0]

    def wslot():
        r = wsl[wsn[0] % 2]
        wsn[0] += 1
        return r
    junk = S.sbs("junk", [128, 1024], F32); Bjunk = S.buf()
    xsl = [(S.sbs(f"xsl{i}", [128, 1024], F32), S.buf(f"xsl{i}")) for i in range(2)]
    xn = S.sbs("xn", [128, 1024], F32); Bxn = S.buf()
    hxT = S.sbs("hxT", [128, 8, 512], F32R); BhxT = S.buf("hxT")
    S.scope_push()
    wada_v = wada_d.rearrange("(k p) n -> p k n", p=128)
    win_v = win_d.rearrange("(k p) n -> p k n", p=128)

    act(sc, ccT, AF.Silu, [Bcc], [Bscx])
    sc3 = sc.rearrange("p (k j) -> p k j", j=2)
    nffn = S.sbs("nffn", [128, 1024], F32); Bnff = S.buf()
    S.dma("sp", nffn, nffn_d, writes=[Bnff])
    mrow = [(S.sbs(f"mrow{i}", [2, 512], F32), S.sbs(f"brow{i}", [2, 512], F32), S.buf(), S.buf()) for i in range(2)]
    psT, BpT = trb_ps, Btrb
    bcdst = {4: (g1bc, 0, 0), 5: (g1bc, 1, 0), 6: (B2bc, 0, 0), 7: (B2bc, 1, 0), 8: (A2bc, 0, 1), 9: (A2bc, 1, 1),
             10: (g2bc, 0, 0), 11: (g2bc, 1, 0)}
    for j in range(12):
        wt, Bw = wslot()
        S.dma("sp", wt, wada_v[:, :, j * 512:(j + 1) * 512], writes=[Bw])
        mr, br, Bmr, Bbr = mrow[j % 2]
        S.dma("sp", br[0:1, :], bada_d[:, j * 512:(j + 1) * 512], writes=[Bbr])
        S.dma("sp", br[1:2, :], bada_d[:, j * 512:(j + 1) * 512], writes=[Bbr])
        ps, Bp = bank()
        mm([(ps[0:2, :], sc3[:, k, :], wt[:, k, :], k == 0, k == 7) for k in range(8)], [Bscx, Bw], [Bp])
        tt("dve", mr, ps[0:2, :], br, ALU.add, [Bp, Bbr], [Bmr])
        if dbg:
            S.dma("pool", dbg_d["mod"][:, j * 512:(j + 1) * 512], mr, reads=[Bmr], slot=Bmr, out_final=True)
        if j < 4:
            trs([(psT[:, (j * 4 + c) * 2:(j * 4 + c + 1) * 2], mr[0:2, c * 128:(c + 1) * 128], ident[0:2, 0:2])
                 for c in range(4)], [Bmr, Bc], [BpT])
        else:
            dst, hf, kind = bcdst[j]
            ps2, Bp2 = bank()
            mm([(ps2, C("ones", rows=1), mr[0:1, :], True, True)], [Bmr, Bc], [Bp2])
            if kind == 0:
                cp("act", dst[:, hf * 512:(hf + 1) * 512], ps2, [Bp2], [Bbc])
            else:
                stt(dst[:, hf * 512:(hf + 1) * 512], ps2, 1.0, nffn[:, hf * 512:(hf + 1) * 512], ALU.add, ALU.mult,
                    [Bp2, Bnff], [Bbc])
        if j == 3:
            cp("dve", modT, psT[:, 0:32], [BpT], [BmT])
            modT3 = modT.rearrange("p (c j) -> p c j", j=2)
            stt(A1, modT3[:, 8:16, 0], 1.0, nmix, ALU.add, ALU.mult, [BmT, Bnm], [Bab])
            cp("dve", B1, modT3[:, 0:8, 0], [BmT], [Bab])
            stt(Ac, modT3[:, 8:16, 1], 1.0, nmix, ALU.add, ALU.mult, [BmT, Bnm], [Bab])
            cp("dve", Bcx, modT3[:, 0:8, 1], [BmT], [Bab])

    if stage == 0:
        S.dma("pool", out_d[0:128, :], nfin, reads=[Bnf], slot=Bnf, out_final=True)
        S.finish()
        return nc
    ssn = [0]

    def rstd_of(xt, Bx):
        i = ssn[0] % 4
        ssn[0] += 1
        col = ss_t[:, i:i + 1]
        act(junk, xt, AF.Square, [Bx], [Bjunk])
        S.op("dve", lambda e: e.reduce_sum(col, junk, AX.X), reads=[Bjunk], writes=[Bss[i]])
        ts("dve", col, col, 1.0 / D_MODEL, EPS, ALU.mult, ALU.add, [Bss[i]], [Bss[i]])
        act(col, col, AF.Sqrt, [Bss[i]], [Bss[i]])
        S.op("dve", lambda e: e.reciprocal(col, col), reads=[Bss[i]], writes=[Bss[i]])
        return col, Bss[i]

    def prep_tile(src_rows, An, Bn, col0, xi):
        xt, Bx = xsl[xi % 2]
        S.dma("sp", xt, src_rows, writes=[Bx])
        rs, Brs = rstd_of(xt, Bx)
        act(xn, xt, AF.Copy, [Bx, Brs], [Bxn], scale=rs)
        for half in range(2):
            ps, Bp = bank()
            trs([(ps[:, q * 128:(q + 1) * 128], xn[:, (half * 4 + q) * 128:(half * 4 + q + 1) * 128], ident)
                 for q in range(4)], [Bxn, Bc], [Bp])
            for q in range(4):
                k = half * 4 + q
                act(hxT[:, k, col0:col0 + 128], ps[:, q * 128:(q + 1) * 128], AF.Identity, [Bp, Bab], [BhxT],
                    scale=An[:, k:k + 1], bias=Bn[:, k:k + 1])

    lblbc = S.sbs("lblbc", [128, 2048], F32); Blb = S.buf()
    S.dma("sp", lblbc, lblbc_d, writes=[Blb])
    lbbc = S.sbs("lbbc", [128, 1024], F32); omlbc = S.sbs("omlbc", [128, 1024], F32); Blbb = S.buf()
    tt("dve", omlbc, lblbc[:, 0:1024], lblbc[:, 1024:2048], ALU.subtract, [Blb], [Blbb])
    act(lbbc, omlbc, AF.Sigmoid, [Blbb], [Blbb])
    ts("dve", omlbc, lbbc, -1.0, 1.0, ALU.mult, ALU.add, [Blbb], [Blbb])
    clogf = S.sbs("clogf", [128, 2, 2, 512], F32R); ck = S.sbs("ck", [128, 2, 2, 512], F32)
    cv16 = S.sbs("cv16", [128, 2, 512], F32R); ckd16 = S.sbs("ckd16", [128, 2, 2, 512], F32R)
    Bclf = [[S.buf() for _ in range(2)] for _ in range(2)]; Bck = [[S.buf() for _ in range(2)] for _ in range(2)]
    Bcv = [S.buf() for _ in range(2)]; Bckd = [[S.buf() for _ in range(2)] for _ in range(2)]
    csig = S.sbs("csig", [128, 512], F32); Bcs = S.buf()
    cf = S.sbs("cf", [128, 512], F32); Bcf = S.buf()
    import os
    KCUT = int(os.environ.get("KCUT", "0"))
    for i in range(2):
        prep_tile(ctx_d[i * 128:(i + 1) * 128, :], Ac, Bcx, i * 128, i)
    if KCUT == 1:
        S.dma("pool", out_d[0:128, :], nfin, reads=[Bnf], slot=Bnf, out_final=True)
        S.scope_pop(); S.scope_pop()
        S.finish()
        return nc
    for j in (1, 2, 3):
        wt, Bw = wslot()
        S.dma("sp", wt, win_v[:, :, j * 512:(j + 1) * 512], writes=[Bw])
        for i in range(2):
            ps, Bp = bank()
            mm([(ps, hxT[:, k, i * 128:(i + 1) * 128], wt[:, k, :], k == 0, k == 7) for k in range(8)],
               [BhxT, Bw], [Bp])
            if j == 3:
                cp("act", cv16[:, i, :], ps, [Bp], [Bcv[i]])
            else:
                d = j - 1
                act(csig, ps, AF.Sigmoid, [Bp], [Bcs])
                tt("dve", cf, csig, omlbc[:, d * 512:(d + 1) * 512], ALU.mult, [Bcs, Blbb], [Bcf])
                tt("dve", cf, cf, lbbc[:, d * 512:(d + 1) * 512], ALU.add, [Bcf, Blbb], [Bcf])
                act(clogf[:, d, i, :], cf, AF.Ln, [Bcf], [Bclf[d][i]])
                ts("dve", ck[:, d, i, :], cf, -1.0, 1.0, ALU.mult, ALU.add, [Bcf], [Bck[d][i]])
    if KCUT == 2:
        S.dma("pool", out_d[0:128, :], nfin, reads=[Bnf], slot=Bnf, out_final=True)
        S.scope_pop(); S.scope_pop()
        S.finish()
        return nc
    ones_f = onesr
    trir = S.sbs("trir", [128, 256], F32R); Btri = S.buf()
    cp("dve", trir[:, 0:128], C("mgt"), [Bc], [Btri])
    cp("dve", trir[:, 128:256], C("mlt"), [Bc], [Btri])
    for d in range(2):
        for i in range(2):
            ps, Bp = bank()
            tri = trir[:, 0:128] if d == 0 else trir[:, 128:256]
            other = 1 - i
            lst = [(ps, tri, clogf[:, d, i, :], True, False)]
            if (d == 0 and i == 0) or (d == 1 and i == 1):
                lst.append((ps, ones_f, clogf[:, d, other, :], False, True))
                rd = [Bclf[d][0], Bclf[d][1], Btri, Bor]
            else:
                lst[0] = (ps, tri, clogf[:, d, i, :], True, True)
                rd = [Bclf[d][i], Btri]
            mm(lst, rd, [Bp])
            act(csig, ps, AF.Exp, [Bp], [Bcs])
            tt("dve", ckd16[:, d, i, :], ck[:, d, i, :], csig, ALU.mult, [Bck[d][i], Bcs], [Bckd[d][i]])
    if KCUT == 3:
        S.dma("pool", out_d[0:128, :], nfin, reads=[Bnf], slot=Bnf, out_final=True)
        S.scope_pop(); S.scope_pop()
        S.finish()
        return nc
    for d in range(2):
        for h in range(4):
            ps, Bp = bank()
            hs = slice(h * 128, (h + 1) * 128)
            mm([(ps[:, 0:128], ckd16[:, d, i, hs], cv16[:, i, hs], i == 0, i == 1) for i in range(2)],
               [Bckd[d][0], Bckd[d][1], Bcv[0], Bcv[1]], [Bp])
            cp("dve", S32[:, d * 4 + h, :], ps[:, 0:128], [Bp], [BS32[d * 4 + h]])
            cp("act", S16[:, 0, d * 4 + h, :], S32[:, d * 4 + h, :], [BS32[d * 4 + h]], [BS16[0][d * 4 + h]])
    if dbg and stage == 1:
        for nm, tns, shp, bufs in (("clogf", clogf, [128, 2048], [x for y in Bclf for x in y]), ("ck", ck, [128, 2048], [x for y in Bck for x in y]),
                                   ("ckd", ckd16, [128, 2048], [x for y in Bckd for x in y]), ("cv", cv16, [128, 1024], Bcv),
                                   )[:int(os.environ.get("NDBG", "4"))]:
            dd_ = dscr("dbg_" + nm, shp, F32, out=True)
            flat = tns.bitcast(F32) if nm != "ck" else tns
            if flat.ndim == 4:
                flat = flat.rearrange("p a b c -> p (a b c)")
            elif flat.ndim == 3:
                flat = flat.rearrange("p a b -> p (a b)")
            S.dma("sp", dd_, flat, reads=bufs, slot=S.buf(), out_final=True)
    if dbg and stage == 1:
        for hh in range(2):
            dd_ = dscr(f"dbg_hcT{hh}", [128, 2048], F32, out=True)
            S.dma("sp", dd_, hxT.bitcast(F32)[:, hh * 4:(hh + 1) * 4, :].rearrange("p a b -> p (a b)"), reads=[BhxT], slot=S.buf(), out_final=True)
        dd_ = dscr("dbg_xn", [128, 1024], F32, out=True)
        S.dma("sp", dd_, xn, reads=[Bxn], slot=S.buf(), out_final=True)
        dd_ = dscr("dbg_ss", [128, 4], F32, out=True)
        S.dma("sp", dd_, ss_t, reads=Bss, slot=S.buf(), out_final=True)
    if KCUT == 4:
        S.dma("pool", out_d[0:128, :], nfin, reads=[Bnf], slot=Bnf, out_final=True)
        S.scope_pop(); S.scope_pop()
        S.finish()
        return nc
    if dbg:
        S.dma("pool", dbg_d["s0"].rearrange("s p v -> p s v"), S32, reads=BS32, slot=BS32[0], out_final=True)
    S.scope_pop()
    if stage == 1:
        S.dma("pool", out_d[0:128, :], nfin, reads=[Bnf], slot=Bnf, out_final=True)
        S.finish()
        return nc
    return _build_rest(nc, S, locals(), stage, dbg)


def _host_layouts(inputs):
    f = lambda a: np.ascontiguousarray(np.asarray(a, dtype=np.float32))
    fm = lambda v: f(np.asarray(v).reshape(-1, 128).T)
    sh = {}
    sh["w_ada"] = f(inputs["w_ada"][0]); sh["b_ada"] = f(inputs["b_ada"][0]).reshape(1, 6144)
    sh["nmix_fm"] = fm(inputs["norm_mix"][0])
    sh["nffn_bc"] = f(np.broadcast_to(np.asarray(inputs["norm_ffn"][0])[None, :], (128, 1024)))
    sh["nfin_bc"] = f(np.broadcast_to(np.asarray(inputs["norm_final"])[None, :], (128, 1024)))
    sh["w_in"] = f(inputs["w_in"][0])
    lbl = np.asarray(inputs["lb_logits"], dtype=np.float32)
    sh["lbl_fm"] = f(np.concatenate([fm(lbl[0].reshape(-1)), fm(lbl[1].reshape(-1))], axis=1))
    sh["lbl_bc"] = f(np.broadcast_to(lbl.reshape(1, 2048), (128, 2048)))
    sh["hgn_fm"] = f(np.asarray(inputs["hg_norm"][0]).reshape(128, 1))
    cw = np.asarray(inputs["conv_w"][0], dtype=np.float32)
    sh["convw_fm"] = f(cw.reshape(3, 4, 128).transpose(2, 1, 0).reshape(128, 12))
    sh["w_out"] = f(inputs["w_out"][0])
    wr = np.asarray(inputs["w_router"][0], dtype=np.float32)
    sh["w_r_fm"] = f(wr.reshape(8, 128, 16).transpose(1, 0, 2).reshape(128, 128))
    sh["w_gate"] = f(inputs["w_gate"][0]); sh["w_up"] = f(inputs["w_up"][0]); sh["w_down"] = f(inputs["w_down"][0])
    sh["consts"] = make_consts()
    return sh


def _core_inputs(inputs, sh, b):
    f = lambda a: np.ascontiguousarray(np.asarray(a, dtype=np.float32))
    m = dict(sh)
    m["x"] = f(inputs["x"][b]); m["ctx"] = f(inputs["ctx"][b])
    cc = np.stack([np.asarray(inputs["c"][b]).reshape(8, 128).T, np.asarray(inputs["c_ctx"]).reshape(8, 128).T],
                  axis=-1)
    m["ccT"] = f(cc.reshape(128, 16))
    return m


_PROG = {}


def kernel(**inputs):
    if "full" not in _PROG:
        _PROG["full"] = build_program()
    nc = _PROG["full"]
    sh = _host_layouts(inputs)
    in_maps = [_core_inputs(inputs, sh, b) for b in range(8)]
    if not HAS_MOE:
        for m in in_maps:
            for k in ("w_gate", "w_up", "w_down"):
                m.pop(k, None)
    res = run_bass_kernel_spmd(nc, in_maps, core_ids=list(range(8)))
    return np.stack([np.asarray(r["out"], dtype=np.float32) for r in res.results], axis=0)


def _build_rest(nc, S, L, stage, dbg):
    g = lambda n: L[n]
    (act, tt, ts, stt, cp, mm, trs, bank, C, wslot, prep_tile, rstd_of) = [g(n) for n in (
        "act", "tt", "ts", "stt", "cp", "mm", "trs", "bank", "C", "wslot", "prep_tile", "rstd_of")]
    (x_d, out_d, x2_d, hx2_d, win_v, wout_d, dbg_d, st_of) = [g(n) for n in (
        "x_d", "out_d", "x2_d", "hx2_d", "win_v", "wout_d", "dbg_d", "st_of")]
    (ident, identb, onesr, Bc, Bib, Bor, lbfm, omlfm, nomlfm, Bl, S32, S16, BS32, BS16, sver, A1, B1, Bab,
     g1bc, A2bc, B2bc, g2bc, Bbc, hxT, BhxT, xsl, xn, Bxn, junk, Bjunk, hgn, Bhg, convw, Bcw, wr, Bwr, nfin, Bnf,
     sc_ps, Bsc, kv_ps, Bkv, o_ps, Bo, trb_ps, Btrb) = [g(n) for n in (
         "ident", "identb", "onesr", "Bc", "Bib", "Bor", "lbfm", "omlfm", "nomlfm", "Bl", "S32", "S16", "BS32",
         "BS16", "sver", "A1", "B1", "Bab", "g1bc", "A2bc", "B2bc", "g2bc", "Bbc", "hxT", "BhxT", "xsl", "xn",
         "Bxn", "junk", "Bjunk", "hgn", "Bhg", "convw", "Bcw", "wr", "Bwr", "nfin", "Bnf",
         "sc_ps", "Bsc", "kv_ps", "Bkv", "o_ps", "Bo", "trb_ps", "Btrb")]
    aff_d = nc.dram_tensor("aff_scr", [16, T_SEQ], F32, kind=("ExternalOutput" if dbg else "Internal")).ap()
    wout_v = wout_d.rearrange("(k p) n -> p k n", p=128)
    xcnt = [0]

    S.scope_push()
    Ta = S.sbs("Ta", [128, 512], F32); Tb = S.sbs("Tb", [128, 512], F32)
    Tc = S.sbs("Tc", [128, 512], F32); Td = S.sbs("Td", [128, 512], F32)
    BTa, BTb, BTc, BTd = S.buf(), S.buf(), S.buf(), S.buf()
    kend16 = S.sbs("kend16", [128, 512], F32); Bke16 = S.buf()
    Eall = S.sbs("Eall", [128, 4, 512], F32); BE = [S.buf() for _ in range(4)]
    QD = S.sbs("QD", [128, 4, 512], F32R); BQD = [S.buf() for _ in range(4)]
    KI = S.sbs("KI", [128, 4, 512], F32R); BKI = [S.buf() for _ in range(4)]
    KE = S.sbs("KE", [128, 4, 4, 128], F32R); BKE = [S.buf() for _ in range(4)]
    EL = S.sbs("EL", [128, 4, 16], F32); BEL = [S.buf() for _ in range(4)]
    V16 = S.sbs("V16", [128, 4, 512], F32R); BV = [S.buf() for _ in range(4)]
    VM = S.sbs("VM", [128, 4, 128], F32R); BVM = S.buf()
    AT16 = S.sbs("AT16", [128, 512], F32R); BAT = S.buf()
    OB = S.sbs("OB", [128, 4, 512], F32); BOB = [S.buf() for _ in range(4)]
    SG = S.sbs("SG", [128, 4, 512], BF16); BSG = [S.buf() for _ in range(4)]
    cvs = S.sbs("cvs", [128, 4, 512], F32); Bcvs = [S.buf() for _ in range(4)]
    MIXT = S.sbs("MIXT", [128, 8, 512], F32R); BMX = [S.buf() for _ in range(8)]
    MIXTf = MIXT.bitcast(F32)
    hx2T = S.sbs("hx2T", [128, 8, 128], F32); Bh2T = S.buf()
    osq = S.sbs("osq", [128, 512], F32R); Bosq = S.buf()
    affsb = S.sbs("affsb", [16, 128], F32); Baf = S.buf()
    eT = S.sbs("eT", [16, 128], F32); BeT = S.buf()
    Bstash = [S.buf(f"stash{i}") for i in range(8)]

    def gate_prep(d, h, ps, Bp):
        col = d * 4 + h
        act(Ta, ps, AF.Sigmoid, [Bp], [BTa])
        act(Tb, Ta, AF.Ln, [BTa, Bl], [BTb], scale=omlfm[:, col:col + 1], bias=lbfm[:, col:col + 1])
        ts("dve", Tc, Ta, nomlfm[:, col:col + 1], omlfm[:, col:col + 1], ALU.mult, ALU.add, [BTa, Bl], [BTc])
        if d == 0:
            S.op("dve", lambda e: e.tensor_tensor_scan(Td, C("rsf"), Tb, 0.0, ALU.mult, ALU.add),
                 reads=[BTb, Bc], writes=[BTd])
        else:
            S.op("dve", lambda e: e.tensor_tensor_scan(Td[:, ::-1], C("rsb")[:, ::-1], Tb[:, ::-1], 0.0,
                                                       ALU.mult, ALU.add), reads=[BTb, Bc], writes=[BTd])
        act(Eall[:, h, :], Td, AF.Exp, [BTd], [BE[h]])
        act(Ta, Td, AF.Exp, [BTd], [BTa], scale=-1.0)
        tt("dve", Tc, Tc, Ta, ALU.mult, [BTc, BTa], [BTc])
        cp("act", KI[:, h, :], Tc, [BTc], [BKI[h]])
        E3 = Eall[:, h, :].rearrange("p (c k) -> p c k", k=32)
        cp("pool", EL[:, h, :], E3[:, :, 31] if d == 0 else E3[:, :, 0], [BE[h]], [BEL[h]])
        elb = EL[:, h, :].rearrange("p (c o) -> p c o", o=1).to_broadcast([128, 16, 32])
        tt("dve", kend16.rearrange("p (c k) -> p c k", k=32), Tc.rearrange("p (c k) -> p c k", k=32), elb,
           ALU.mult, [BTc, BEL[h]], [Bke16])
        trs([(trb_ps[:, t * 128:(t + 1) * 128], kend16[:, t * 128:(t + 1) * 128], ident) for t in range(4)],
            [Bke16, Bc], [Btrb])
        cp("act", KE[:, h, :, :].rearrange("p t d -> p (t d)"), trb_ps[:, 0:512], [Btrb], [BKE[h]])

    def gla_head(d, h):
        col = d * 4 + h
        mm([(sc_ps[:, t * 128:(t + 1) * 128], KI[:, h, t * 128:(t + 1) * 128], QD[:, h, t * 128:(t + 1) * 128],
             True, True) for t in range(4)], [BKI[h], BQD[h]], [Bsc])
        mk = (C("maskf") if d == 0 else C("maskb")).rearrange("p (o q) -> p o q", o=1).to_broadcast([128, 4, 128])
        tt("dve", AT16.rearrange("p (t q) -> p t q", q=128), sc_ps.rearrange("p (t q) -> p t q", q=128), mk, ALU.mult,
           [Bsc, Bc], [BAT])
        order = range(4) if d == 0 else range(3, -1, -1)
        hs = slice(h * 128, (h + 1) * 128)
        for t in order:
            for cc in range(4):
                ts("pool", VM[:, cc, :], V16.bitcast(F32)[:, t, hs], C("cind")[:, cc:cc + 1], None, ALU.mult, None,
                   [BV[t], Bc], [BVM])
            mm([(kv_ps[:, cc * 128:(cc + 1) * 128], KE[:, h, t, :], VM[:, cc, :], True, True) for cc in range(4)],
               [BKE[h], BVM], [Bkv])
            tc0 = t * 128
            mm([(o_ps[:, tc0:tc0 + 128], V16[:, t, hs], AT16[:, tc0:tc0 + 128], True, False)],
               [BV[t], BAT], [Bo])
            for ci, cc in enumerate(order):
                c0 = tc0 + cc * 32
                v = sver[col]
                mm([(o_ps[:, c0:c0 + 32], S16[:, v, col, :], QD[:, h, c0:c0 + 32], False, ci == 3)],
                   [BS16[v][col], BQD[h]], [Bo])
                chunk = t * 4 + cc
                stt(S32[:, col, :], S32[:, col, :], EL[:, h, chunk:chunk + 1], kv_ps[:, cc * 128:(cc + 1) * 128],
                    ALU.mult, ALU.add, [BS32[col], BEL[h], Bkv], [BS32[col]])
                cp("act", S16[:, 1 - v, col, :], S32[:, col, :], [BS32[col]], [BS16[1 - v][col]])
                sver[col] = 1 - v

    def sweep(d):
        groups = range(8) if d == 0 else range(7, -1, -1)
        pieces = [1, 0, 3, 4, 7, 6, 5] if d == 0 else [2, 0, 3]
        for gi in groups:
            for t in range(4):
                r0 = (gi * 4 + t) * 128
                prep_tile(x_d[r0:r0 + 128, :], A1, B1, t * 128, xcnt[0])
                xcnt[0] += 1
            if d == 0:
                S.dma("sp", OB, st_of[gi], reads=[Bstash[gi]], writes=BOB, slot=BOB[0])
            for j in pieces:
                wt, Bw = wslot()
                S.dma("sp", wt, win_v[:, :, j * 512:(j + 1) * 512], writes=[Bw])
                for q in range(4):
                    ps, Bp = bank()
                    if j == 3:
                        mm([(ps, hxT[:, k, q * 128:(q + 1) * 128], wt[:, k, :], k == 0, k == 7) for k in range(8)],
                           [BhxT, Bw], [Bp])
                        cp("act", V16[:, q, :], ps, [Bp], [BV[q]])
                        continue
                    mm([(ps, wt[:, k, q * 128:(q + 1) * 128], hxT[:, k, :], k == 0, k == 7) for k in range(8)],
                       [BhxT, Bw], [Bp])
                    if j in (1, 2):
                        gate_prep(d, q, ps, Bp)
                    elif j == 0:
                        tt("dve", QD[:, q, :], ps, Eall[:, q, :], ALU.mult, [Bp, BE[q]], [BQD[q]])
                    elif j == 4:
                        act(SG[:, q, :], ps, AF.Silu, [Bp], [BSG[q]])
                    elif j == 7:
                        cp("act", cvs[:, q, :], ps, [Bp], [Bcvs[q]])
                    elif j == 6:
                        u = cvs[:, q, :]
                        y = Eall[:, q, :]
                        tt("dve", u, ps, u, ALU.mult, [Bp, Bcvs[q]], [Bcvs[q]])
                        ts("dve", y, u, convw[:, q * 3 + 1:q * 3 + 2], None, ALU.mult, None, [Bcvs[q], Bcw], [BE[q]])
                        u3 = u.rearrange("p (r w) -> p r w", w=64); y3 = y.rearrange("p (r w) -> p r w", w=64)
                        stt(y3[:, :, 1:64], u3[:, :, 0:63], convw[:, q * 3:q * 3 + 1], y3[:, :, 1:64], ALU.mult, ALU.add,
                            [Bcvs[q], Bcw, BE[q]], [BE[q]])
                        stt(y3[:, :, 0:63], u3[:, :, 1:64], convw[:, q * 3 + 2:q * 3 + 3], y3[:, :, 0:63], ALU.mult,
                            ALU.add, [Bcvs[q], Bcw, BE[q]], [BE[q]])
                    elif j == 5:
                        tt("dve", MIXT[:, 4 + q, :], ps, Eall[:, q, :], ALU.mult, [Bp, BE[q]], [BMX[4 + q]])
            for h in range(4):
                gla_head(d, h)
                if d == 1:
                    cp("act", OB[:, h, :], o_ps, [Bo], [BOB[h]])
                else:
                    tt("dve", Tb, o_ps, OB[:, h, :], ALU.add, [Bo, BOB[h]], [BTb])
                    act(osq, Tb, AF.Square, [BTb], [Bosq])
                    ps, Bp = bank()
                    mm([(ps, onesr, osq, True, True)], [Bosq, Bor], [Bp])
                    ts("dve", Td, ps, 1.0 / 128.0, EPS, ALU.mult, ALU.add, [Bp], [BTd])
                    act(Td, Td, AF.Sqrt, [BTd], [BTd])
                    S.op("dve", lambda e: e.reciprocal(Td, Td), reads=[BTd], writes=[BTd])
                    tt("dve", Tb, Tb, Td, ALU.mult, [BTb, BTd], [BTb])
                    stt(MIXT[:, h, :], Tb, hgn[:, 0:1], SG[:, h, :], ALU.mult, ALU.mult, [BTb, Bhg, BSG[h]], [BMX[h]])
            if d == 1:
                S.dma("pool", st_of[gi], OB, reads=BOB, writes=[Bstash[gi]], slot=BOB[0])
                if dbg:
                    S.dma("pool", dbg_d["of"][gi], OB, reads=BOB, slot=BOB[0], out_final=True)
                continue
            wo = []
            for hf in range(2):
                wt, Bw = wslot()
                S.dma("sp", wt, wout_v[:, :, hf * 512:(hf + 1) * 512], writes=[Bw])
                wo.append((wt, Bw))
            for t in range(4):
                r0 = (gi * 4 + t) * 128
                xt, Bx = xsl[xcnt[0] % 2]
                xcnt[0] += 1
                S.dma("sp", xt, x_d[r0:r0 + 128, :], writes=[Bx])
                for hf in range(2):
                    ps, Bp = bank()
                    wt, Bw = wo[hf]
                    mm([(ps, MIXT[:, kc, t * 128:(t + 1) * 128], wt[:, kc, :], kc == 0, kc == 7) for kc in range(8)],
                       BMX + [Bw], [Bp])
                    hsl = slice(hf * 512, (hf + 1) * 512)
                    tt("dve", xn[:, hsl], ps, g1bc[:, hsl], ALU.mult, [Bp, Bbc], [Bxn])
                    tt("pool", xn[:, hsl], xn[:, hsl], xt[:, hsl], ALU.add, [Bxn, Bx], [Bxn])
                S.dma("pool", x2_d[r0:r0 + 128, :], xn, reads=[Bxn], slot=Bxn, out_final=dbg)
                rs, Brs = rstd_of(xn, Bxn)
                stt(junk, xn, rs, A2bc, ALU.mult, ALU.mult, [Bxn, Brs, Bbc], [Bjunk])
                tt("pool", junk, junk, B2bc, ALU.add, [Bjunk, Bbc], [Bjunk])
                S.dma("pool", hx2_d[r0:r0 + 128, :], junk, reads=[Bjunk], slot=Bjunk, out_final=dbg)
                for half in range(2):
                    ps, Bp = bank()
                    trs([(ps[:, q * 128:(q + 1) * 128], junk[:, (half * 4 + q) * 128:(half * 4 + q + 1) * 128], ident)
                         for q in range(4)], [Bjunk, Bc], [Bp])
                    cp("act", hx2T[:, half * 4:(half + 1) * 4, :].rearrange("p k t -> p (k t)"), ps, [Bp], [Bh2T])
                ps, Bp = bank()
                mm([(ps[0:16, 0:128], wr[:, k * 16:(k + 1) * 16], hx2T[:, k, :], k == 0, k == 7) for k in range(8)],
                   [Bwr, Bh2T], [Bp])
                act(eT, ps[0:16, 0:128], AF.Exp, [Bp], [BeT])
                ps2, Bp2 = bank()
                mm([(ps2[0:16, 0:128], C("ones", rows=16, hi=16), eT, True, True)], [BeT, Bc], [Bp2])
                S.op("dve", lambda e, ps2=ps2: e.reciprocal(affsb, ps2[0:16, 0:128]), reads=[Bp2], writes=[Baf])
                tt("dve", affsb, affsb, eT, ALU.mult, [Baf, BeT], [Baf])
                S.dma("pool", aff_d[:, r0:r0 + 128], affsb, reads=[Baf], slot=Baf, out_final=dbg)

    sweep(1)
    if stage == 2:
        S.dma("pool", out_d[0:128, :], nfin, reads=[Bnf], slot=Bnf, out_final=True)
        S.scope_pop(); S.scope_pop()
        S.finish()
        return nc
    sweep(0)
    S.scope_pop()
    S.scope_pop()
    if stage == 3:
        S.dma("pool", out_d[0:128, :], nfin, reads=[Bnf], slot=Bnf, out_final=True)
        S.finish()
        return nc
    return _build_moe(nc, S, L, locals(), stage, dbg)


def _build_moe(nc, S, L, L2, stage, dbg):
    import concourse.bass as bass_
    act, tt, ts, stt, cp, mm, trs, C = [L[n] for n in ("act", "tt", "ts", "stt", "cp", "mm", "trs", "C")]
    x2_d, hx2_d, out_d, nfin, Bnf, g2bc, Bbc, Bc, ident, ipb = [L[n] for n in (
        "x2_d", "hx2_d", "out_d", "nfin", "Bnf", "g2bc", "Bbc", "Bc", "ident", "ipb")]
    wg_d, wu_d, wd_d = L["wg_d"], L["wu_d"], L["wd_d"]
    aff_d = L2["aff_d"]
    yacc = [(L["sc_ps"], L["Bsc"]), (L["kv_ps"], L["Bkv"]), (L["o_ps"], L["Bo"]), (L["trb_ps"], L["Btrb"])]
    R_ps, BR = ipb[3]
    rot = ipb[0:3]
    rn = [0]

    def bank3():
        r = rot[rn[0] % 3]
        rn[0] += 1
        return r
    Bx2all = S.buf("x2all")
    Bhx2all = S.buf("hx2all")

    S.scope_push()
    slotT = S.sbs("slotT", [128, 4, 128], F32); BslT = S.buf()
    affT = S.sbs("affT", [128, 4, 128], F32); BafT = S.buf()
    vals = S.sbs("vals", [128, 4, 128, 3], F32R); Bvals = S.buf()
    Rsb = S.sbs("Rsb", [3, 512], F32); BRsb = S.buf()
    IG = S.sbs("IG", [128, 12], F32); BIG = S.buf()
    idxs = [(S.sbs(f"idx{i}", [128, 4], I32), S.buf(f"idx{i}")) for i in range(2)]
    gates = [(S.sbs(f"gate{i}", [128, 4], F32), S.buf(f"gate{i}")) for i in range(2)]
    sm = S.sbs("smalls", [128, 16], F32); Bsm = S.buf()
    lo, hi, mid, cnt, ge, dl, nge, off = [sm[:, i:i + 1] for i in range(8)]
    S.scope_push()
    aff128 = S.sbs("aff128", [128, 512], F32); Ba128 = S.buf()
    msk = S.sbs("msk", [128, 512], F32); Bmsk = S.buf()
    cum = S.sbs("cum", [128, 512], F32); Bcum = S.buf()

    S.dma("sp", aff128, aff_d.rearrange("e (s t) -> (e s) t", t=512), writes=[Ba128])
    S.op("dve", lambda e: e.memset(sm, 0.0), writes=[Bsm])
    S.op("dve", lambda e: e.memset(hi, 2.0), reads=[Bsm], writes=[Bsm])
    for it in range(34):
        tt("dve", mid, lo, hi, ALU.add, [Bsm], [Bsm])
        ts("dve", mid, mid, 0.5, None, ALU.mult, None, [Bsm], [Bsm])
        ts("dve", msk, aff128, mid, None, ALU.is_ge, None, [Ba128, Bsm], [Bmsk])
        S.op("dve", lambda e: e.reduce_sum(cnt, msk, AX.X), reads=[Bmsk], writes=[Bsm])
        ps, Bp = bank3()
        mm([(ps[:, 0:1], C("bones"), cnt, True, True)], [Bc, Bsm], [Bp])
        ts("dve", ge, ps[:, 0:1], float(CAP), None, ALU.is_ge, None, [Bp], [Bsm])
        tt("dve", dl, mid, lo, ALU.subtract, [Bsm], [Bsm])
        stt(lo, dl, ge, lo, ALU.mult, ALU.add, [Bsm], [Bsm])
        tt("dve", dl, hi, mid, ALU.subtract, [Bsm], [Bsm])
        stt(hi, dl, ge, mid, ALU.mult, ALU.add, [Bsm], [Bsm])
    import os
    MOECUT = int(os.environ.get("MOECUT", "0"))
    MOEN = int(os.environ.get("MOEN", str(N_EXP)))
    ones512 = S.sbs("ones512", [128, 512], F32); Bo512 = S.buf()
    S.op("pool", lambda e: e.memset(ones512, 1.0), writes=[Bo512])
    ts("dve", msk, aff128, lo, None, ALU.is_ge, None, [Ba128, Bsm], [Bmsk])
    S.op("dve", lambda e: e.reduce_sum(cnt, msk, AX.X), reads=[Bmsk], writes=[Bsm])
    ps, Bp = bank3()
    mm([(ps[:, 0:1], C("segpre"), cnt, True, True)], [Bc, Bsm], [Bp])
    cp("dve", off, ps[:, 0:1], [Bp], [Bsm])
    S.op("dve", lambda e: e.tensor_tensor_scan(cum, ones512, msk, off, ALU.mult, ALU.add),
         reads=[Bmsk, Bsm, Bo512], writes=[Bcum])
    tt("dve", cum, cum, msk, ALU.mult, [Bcum, Bmsk], [Bcum])
    ts("dve", cum, cum, -1.0, None, ALU.add, None, [Bcum], [Bcum])
    for (src, Bsrc, dst, Bdst) in ((cum, Bcum, slotT, BslT), (aff128, Ba128, affT, BafT)):
        ps, Bp = bank3()
        trs([(ps[:, c * 128:(c + 1) * 128], src[:, c * 128:(c + 1) * 128], ident) for c in range(4)], [Bsrc, Bc], [Bp])
        cp("act", dst.rearrange("p c q -> p (c q)"), ps, [Bp], [Bdst])
    valsf = vals.bitcast(F32)
    cp("dve", vals[:, :, :, 1], affT, [BafT], [Bvals])
    tt("dve", vals[:, :, :, 2], affT, valsf[:, :, :, 1], ALU.subtract, [BafT, Bvals], [Bvals])
    tki = C("tokidx").rearrange("p (c o s) -> p c o s", c=4, o=1)
    for c in range(4):
        cp("dve", vals[:, c, :, 0].rearrange("p (e s) -> p e s", s=8), tki[:, c, :, :].to_broadcast([128, 16, 8]),
           [Bc], [Bvals])

    S.scope_pop()
    S.scope_push()
    wsl = [(S.sbs(f"mw{i}", [128, 8, 512], F32R), S.buf(f"mw{i}")) for i in range(3)]
    wn = [0]

    def wslot():
        r = wsl[wn[0] % 3]
        wn[0] += 1
        return r
    xsT = [(S.sbs(f"xsT{i}", [128, 8, 512], F32R), S.buf(f"xsT{i}")) for i in range(2)]
    hidT = S.sbs("hidT", [128, 16, 512], F32R); Bhid = [S.buf() for _ in range(16)]
    xstok = [(S.sbs(f"xstok{i}", [128, 1024], F32), S.buf(f"xstok{i}")) for i in range(2)]
    ysb = [(S.sbs(f"ysb{i}", [128, 1024], F32), S.buf(f"ysb{i}")) for i in range(4)]
    sgt = [(S.sbs(f"sgt{i}", [128, 512], F32), S.buf(f"sgt{i}")) for i in range(2)]
    Qb = [(S.sbs(f"Qb{i}", [128, 512], F32R), S.buf(f"Qb{i}")) for i in range(3)]

    if dbg:
        dbg_lo = nc.dram_tensor("dbg_lo", [128, 16], F32, kind="ExternalOutput").ap()
        dbg_idx = nc.dram_tensor("dbg_idx", [16, 128, 4], I32, kind="ExternalOutput").ap()
        dbg_gate = nc.dram_tensor("dbg_gate", [16, 128, 4], F32, kind="ExternalOutput").ap()
        dbg_slot = nc.dram_tensor("dbg_slot", [128, 512], F32, kind="ExternalOutput").ap()
        S.dma("sp", dbg_lo, sm, reads=[Bsm], slot=S.buf(), out_final=True)
        S.dma("sp", dbg_slot, slotT.rearrange("p c q -> p (c q)"), reads=[BslT], slot=S.buf(), out_final=True)

    def prep(e):
        xT, BxT = xsT[e % 2]
        idx, Bidx = idxs[e % 2]
        gat, Bgat = gates[e % 2]
        for i in range(32):
            s_, c = i // 4, i % 4
            col = e * 8 + s_
            Q, BQ = Qb[i % 3]
            ts("pool" if i % 2 == 0 else "dve", Q, C("iota"), slotT[:, c, col:col + 1], None, ALU.is_equal, None,
               [Bc, BslT], [BQ])
            mm([(R_ps[0:3, :], vals[:, c, col, :], Q, i == 0, i == 31)], [Bvals, BQ], [BR])
            yield
        cp("act", Rsb, R_ps[0:3, :], [BR], [BRsb])
        ps, Bp = bank3()
        trs([(ps[:, b * 3:(b + 1) * 3], Rsb[0:3, b * 128:(b + 1) * 128], ident[0:3, 0:3]) for b in range(4)],
            [BRsb, Bc], [Bp])
        cp("dve", IG, ps[:, 0:12], [Bp], [BIG])
        IG3 = IG.rearrange("p (b j) -> p b j", j=3)
        cp("dve", idx, IG3[:, :, 0], [BIG], [Bidx])
        tt("dve", gat, IG3[:, :, 1], IG3[:, :, 2], ALU.add, [BIG], [Bgat])
        if dbg:
            S.dma("sp", dbg_idx[e], idx, reads=[Bidx], slot=S.buf(), out_final=True)
            S.dma("sp", dbg_gate[e], gat, reads=[Bgat], slot=S.buf(), out_final=True)
        yield
        for b in range(4):
            xt, Bx = xstok[b % 2]
            S.op_dma_ind("pool", lambda en, xt=xt, b=b: en.indirect_dma_start(
                out=xt, out_offset=None, in_=hx2_d, in_offset=bass_.IndirectOffsetOnAxis(ap=idx[:, b:b + 1], axis=0)),
                reads=[Bidx, Bhx2all], writes=[Bx])
            for half in range(2):
                ps, Bp = bank3()
                trs([(ps[:, q * 128:(q + 1) * 128], xt[:, (half * 4 + q) * 128:(half * 4 + q + 1) * 128], ident)
                     for q in range(4)], [Bx, Bc], [Bp])
                for q in range(4):
                    cp("act", xT[:, half * 4 + q, b * 128:(b + 1) * 128], ps[:, q * 128:(q + 1) * 128], [Bp], [BxT])
            yield

    def drain(gen):
        for _ in gen:
            pass

    def tick(gen):
        if gen is not None:
            next(gen, None)

    if MOECUT != 2:
        drain(prep(0))
    for e in range(MOEN if MOECUT not in (2, 3) else 0):
        gen = prep(e + 1) if e + 1 < MOEN else None
        xT, BxT = xsT[e % 2]
        idx, Bidx = idxs[e % 2]
        gat, Bgat = gates[e % 2]
        wgv = wg_d[e].rearrange("(k p) f -> p k f", p=128)
        wuv = wu_d[e].rearrange("(k p) f -> p k f", p=128)
        wdv = wd_d[e].rearrange("(g c p) d -> g p c d", p=128, c=4)
        for fg in range(4):
            wgt, Bwg = wslot()
            S.dma("sp", wgt, wgv[:, :, fg * 512:(fg + 1) * 512], writes=[Bwg])
            wut, Bwu = wslot()
            S.dma("sp", wut, wuv[:, :, fg * 512:(fg + 1) * 512], writes=[Bwu])
            for fc in range(4):
                f = fg * 4 + fc
                sg, Bsg = sgt[f % 2]
                ps, Bp = bank3()
                mm([(ps, wgt[:, k, fc * 128:(fc + 1) * 128], xT[:, k, :], k == 0, k == 7) for k in range(8)],
                   [Bwg, BxT], [Bp])
                act(sg, ps, AF.Silu, [Bp], [Bsg])
                tick(gen)
                ps2, Bp2 = bank3()
                mm([(ps2, wut[:, k, fc * 128:(fc + 1) * 128], xT[:, k, :], k == 0, k == 7) for k in range(8)],
                   [Bwu, BxT], [Bp2])
                tt("dve", hidT[:, f, :], ps2, sg, ALU.mult, [Bp2, Bsg], [Bhid[f]])
                tick(gen)
        for dh in range(2):
            for fg in range(4):
                wt, Bw = wslot()
                wt4 = wt.rearrange("p (a c) d -> p a c d", a=2)[:, 0, :, :]
                S.dma("sp", wt4, wdv[fg][:, :, dh * 512:(dh + 1) * 512], writes=[Bw])
                for t4 in range(4):
                    ya, Bya = yacc[t4]
                    mm([(ya, hidT[:, fg * 4 + fc, t4 * 128:(t4 + 1) * 128], wt4[:, fc, :], fg == 0 and fc == 0,
                         fg == 3 and fc == 3) for fc in range(4)], [Bhid[fg * 4 + fc] for fc in range(4)] + [Bw], [Bya])
                    tick(gen)
            for t4 in range(4):
                ya, Bya = yacc[t4]
                yt, By = ysb[t4]
                hsl = slice(dh * 512, (dh + 1) * 512)
                stt(yt[:, hsl], ya, gat[:, t4:t4 + 1], g2bc[:, hsl], ALU.mult, ALU.mult, [Bya, Bgat, Bbc], [By])
        for t4 in range(4):
            yt, By = ysb[t4]
            S.op_dma_ind("pool", lambda en, yt=yt, t4=t4, idx=idx: en.indirect_dma_start(
                out=x2_d, out_offset=bass_.IndirectOffsetOnAxis(ap=idx[:, t4:t4 + 1], axis=0), in_=yt, in_offset=None,
                compute_op=ALU.add), reads=[Bidx, By, Bx2all], writes=[Bx2all], slot=By)
        if gen is not None:
            drain(gen)
    S.scope_pop()
    S.scope_pop()

    S.scope_push()
    xs = [(S.sbs(f"fx{i}", [128, 1024], F32), S.buf()) for i in range(2)]
    ys = [(S.sbs(f"fy{i}", [128, 1024], F32), S.buf()) for i in range(2)]
    jk = S.sbs("fjunk", [128, 1024], F32); Bjk = S.buf()
    ss = S.sbs("fss", [128, 4], F32); Bs4 = [S.buf() for _ in range(4)]
    for t in range(32):
        xt, Bx = xs[t % 2]; yt, By = ys[t % 2]
        S.dma("sp", xt, x2_d[t * 128:(t + 1) * 128, :], reads=[Bx2all], writes=[Bx])
        col = ss[:, t % 4:t % 4 + 1]; Bcol = Bs4[t % 4]
        act(jk, xt, AF.Square, [Bx], [Bjk])
        S.op("dve", lambda en, col=col: en.reduce_sum(col, jk, AX.X), reads=[Bjk], writes=[Bcol])
        ts("dve", col, col, 1.0 / D_MODEL, EPS, ALU.mult, ALU.add, [Bcol], [Bcol])
        act(col, col, AF.Sqrt, [Bcol], [Bcol])
        S.op("dve", lambda en, col=col: en.reciprocal(col, col), reads=[Bcol], writes=[Bcol])
        stt(yt, xt, col, nfin, ALU.mult, ALU.mult, [Bx, Bcol, Bnf], [By])
        S.dma("pool", out_d[t * 128:(t + 1) * 128, :], yt, reads=[By], slot=By, out_final=True)
    S.scope_pop()
    S.finish()
    return nc
```

```python
import numpy as np
import concourse.bass as bass
import concourse.mybir as mybir
from concourse.bass_utils import run_bass_kernel_spmd

F32 = mybir.dt.float32
BF16 = mybir.dt.bfloat16
I32 = mybir.dt.int32
F32R = mybir.dt.float32r
AF = mybir.ActivationFunctionType
ALU = mybir.AluOpType
AX = mybir.AxisListType

ENGS = ("pe", "act", "dve", "pool", "sp")


class Counter:
    def __init__(self, nc, name, step, epoch):
        self.nc, self.name, self.step, self.epoch = nc, name, step, epoch
        self.n = 0
        self.sems = []

    def next(self):
        self.n += 1
        e = (self.n - 1) // self.epoch
        while len(self.sems) <= e:
            self.sems.append(self.nc.alloc_semaphore(f"{self.name}_e{len(self.sems)}"))
        return self.n

    def sem_of(self, n):
        e = (n - 1) // self.epoch
        return self.sems[e], (n - e * self.epoch) * self.step


class Buf:
    __slots__ = ("name", "w", "r", "ctr")

    def __init__(self, name=""):
        self.name = name
        self.w = {}
        self.r = {}
        self.ctr = None


class Sched:
    def __init__(self, nc):
        self.nc = nc
        self.q = {e: [] for e in ENGS}
        self.eng_ctr = {e: Counter(nc, f"tk_{e}", 1, 12000) for e in ("pe", "act", "dve", "pool")}
        self.waited = {e: {} for e in ENGS}
        self.finals = {}
        self.n_dma_ctr = 0
        self.nbuf = 0
        self.scopes = []
        self.all_dma_ctrs = []

    def sb(self, name, shape, dt):
        assert not self.scopes, "persistent alloc inside scope: " + name
        return self.nc.alloc_sbuf_tensor("sb_" + name, list(shape), dt).ap()

    def ps(self, name, shape, dt=F32):
        return self.nc.alloc_psum_tensor("ps_" + name, list(shape), dt).ap()

    def buf(self, name=None):
        self.nbuf += 1
        return Buf(name or f"b{self.nbuf}")

    def _ctr_for(self, b):
        if b.ctr is None:
            self.n_dma_ctr += 1
            b.ctr = Counter(self.nc, f"dq{self.n_dma_ctr}", 16, 900)
            self.all_dma_ctrs.append(b.ctr)
        return b.ctr

    def _deps(self, eng, reads, writes):
        need = {}

        def add(c, n, src_eng):
            if src_eng == eng and eng == "pe":
                return
            if need.get(c, 0) < n:
                need[c] = n

        for b in reads:
            for c, (n, se) in b.w.items():
                add(c, n, se)
        for b in writes:
            for c, (n, se) in b.w.items():
                if se == eng and se != "dma":
                    continue
                add(c, n, se)
            for c, (n, se) in b.r.items():
                if se == eng and se != "dma":
                    continue
                add(c, n, se)
        waits = []
        wd = self.waited[eng]
        for c, n in need.items():
            if wd.get(c, 0) >= n:
                continue
            wd[c] = n
            waits.append(c.sem_of(n))
        return waits

    def _record(self, c, n, src_eng, reads, writes):
        for b in reads:
            old = b.r.get(c)
            if old is None or old[0] < n:
                b.r[c] = (n, src_eng)
        for b in writes:
            b.w = {c: (n, src_eng)}
            b.r = {}

    def op(self, eng, fn, reads=(), writes=()):
        waits = self._deps(eng, reads, writes)
        c = self.eng_ctr[eng]
        n = c.next()
        sem, _ = c.sem_of(n)
        self._record(c, n, eng, reads, writes)

        def emit(e, waits=waits, fn=fn, sem=sem):
            for s, v in waits:
                e.wait_ge(s, v)
            ins = fn(e)
            ins.then_inc(sem, 1)
        self.q[eng].append(emit)

    def dma(self, qeng, out, in_, reads=(), writes=(), slot=None, out_final=False, **kw):
        self.op_dma_ind(qeng, lambda e: e.dma_start(out=out, in_=in_, **kw), reads=reads, writes=writes,
                        slot=slot, out_final=out_final)

    def op_dma_ind(self, qeng, fn, reads=(), writes=(), slot=None, out_final=False):
        if slot is None:
            slot = writes[0] if len(writes) else reads[0]
        waits = self._deps(qeng, reads, writes)
        c = self._ctr_for(slot)
        n = c.next()
        sem, _ = c.sem_of(n)
        self._record(c, n, "dma", reads, writes)
        if out_final:
            self.finals[c] = n

        def emit(e, waits=waits, fn=fn, sem=sem):
            for s, v in waits:
                e.wait_ge(s, v)
            fn(e).then_inc(sem, 16)
        self.q[qeng].append(emit)

    def scope_push(self):
        from contextlib import ExitStack
        st = ExitStack()
        self.scopes.append(st)

    def sbs(self, name, shape, dt):
        return self.scopes[-1].enter_context(self.nc.sbuf_tensor("sb_" + name, list(shape), dt)).ap()

    def scope_pop(self):
        self.barrier()
        self.scopes.pop().close()

    def barrier(self):
        ctrs = list(self.eng_ctr.values()) + self.all_dma_ctrs
        for eng in ENGS:
            waits = []
            wd = self.waited[eng]
            for c in ctrs:
                if c.n > 0 and wd.get(c, 0) < c.n:
                    wd[c] = c.n
                    waits.append(c.sem_of(c.n))

            def emit(e, waits=waits):
                for s_, v in waits:
                    e.wait_ge(s_, v)
            self.q[eng].append(emit)

    def finish(self):
        nc = self.nc
        finals = [c.sem_of(n) for c, n in self.finals.items()]
        q = self.q
        with nc.Block() as block:
            @block.sync
            def _(e):
                for f in q["sp"]:
                    f(e)
                for s, v in finals:
                    e.wait_ge(s, v)

            @block.tensor
            def _(e):
                for f in q["pe"]:
                    f(e)

            @block.scalar
            def _(e):
                for f in q["act"]:
                    f(e)

            @block.vector
            def _(e):
                for f in q["dve"]:
                    f(e)

            @block.gpsimd
            def _(e):
                for f in q["pool"]:
                    f(e)


def _const_layout():
    lay = {}
    off = 0
    for name, n in (("ident", 128), ("maskf", 128), ("maskb", 128), ("mgt", 128), ("mlt", 128), ("ones", 128),
                    ("rsf", 512), ("rsb", 512), ("cind", 4), ("iota", 512), ("tokab", 64), ("bones", 128),
                    ("wcomb", 2), ("segpre", 128), ("tokidx", 32)):
        lay[name] = (off, n)
        off += n
    return lay, off


CL, NCONST = _const_layout()


def make_consts():
    c = np.zeros((128, NCONST), np.float32)

    def put(name, arr):
        o, n = CL[name]
        c[:arr.shape[0], o:o + n] = arr
    p = np.arange(128)
    put("ident", np.eye(128, dtype=np.float32))
    j = p[:, None]; i = p[None, :]
    same = (j // 32) == (i // 32)
    mf = (same & (j <= i)).astype(np.float32)
    mb = (same & (j >= i)).astype(np.float32)
    put("maskf", mf); put("maskb", mb)
    put("mgt", (j > i).astype(np.float32))
    put("mlt", (j < i).astype(np.float32))
    put("ones", np.ones((128, 128), np.float32))
    t = np.arange(512)
    put("rsf", np.broadcast_to((t % 32 != 0).astype(np.float32), (128, 512)))
    put("rsb", np.broadcast_to((t % 32 != 31).astype(np.float32), (128, 512)))
    put("cind", (p[:, None] // 32 == np.arange(4)[None, :]).astype(np.float32))
    put("iota", np.broadcast_to(t.astype(np.float32), (128, 512)))
    tok = (np.arange(32)[None, :] * 128 + p[:, None])
    ab = np.stack([tok // 64, tok % 64], axis=-1).reshape(128, 64).astype(np.float32)
    put("tokab", ab)
    put("bones", ((p[:, None] // 8) == (p[None, :] // 8)).astype(np.float32))
    wc = np.zeros((128, 2), np.float32)
    wc[0, 0] = 64.0; wc[1, 0] = 1.0; wc[2, 1] = 1.0; wc[3, 1] = 1.0; wc[4, 1] = 1.0
    put("wcomb", wc)
    put("segpre", (((p[:, None] // 8) == (p[None, :] // 8)) & ((p[:, None] % 8) < (p[None, :] % 8))).astype(np.float32))
    cs = np.arange(32)
    put("tokidx", ((cs[None, :] % 8) * 512 + (cs[None, :] // 8) * 128 + p[:, None]).astype(np.float32))
    return c


T_SEQ, D_MODEL, N_EXP, CAP, DFF = 4096, 1024, 16, 512, 2048
HAS_MOE = True
EPS = 1e-6


def build_program(stage=99, dbg=False):
    nc = bass.Bass("TRN2", target_bir_lowering=False)
    nc.dge_precook = False
    S = Sched(nc)

    def din(name, shape, dt=F32):
        return nc.dram_tensor(name, list(shape), dt, kind="ExternalInput").ap()

    def dscr(name, shape, dt=F32, out=False):
        if out:
            return nc.dram_tensor(name, list(shape), dt, kind="ExternalOutput").ap()
        return nc.dram_tensor(name, list(shape), dt).ap()

    x_d = din("x", [T_SEQ, D_MODEL]); ctx_d = din("ctx", [256, D_MODEL]); cc_d = din("ccT", [128, 16])
    wada_d = din("w_ada", [1024, 6144], F32R); bada_d = din("b_ada", [1, 6144])
    nmix_d = din("nmix_fm", [128, 8]); nffn_d = din("nffn_bc", [128, 1024]); nfin_d = din("nfin_bc", [128, 1024])
    win_d = din("w_in", [1024, 4096], F32R)
    lblfm_d = din("lbl_fm", [128, 16]); lblbc_d = din("lbl_bc", [128, 2048])
    hgn_d = din("hgn_fm", [128, 1]); convw_d = din("convw_fm", [128, 12])
    wout_d = din("w_out", [1024, 1024], F32R); wr_d = din("w_r_fm", [128, 128])
    if stage >= 4 and HAS_MOE:
        wg_d = din("w_gate", [N_EXP, 1024, DFF], F32R); wu_d = din("w_up", [N_EXP, 1024, DFF], F32R)
        wd_d = din("w_down", [N_EXP, DFF, 1024], F32R)
    consts_d = din("consts", [128, NCONST])
    out_d = nc.dram_tensor("out", [T_SEQ, D_MODEL], F32, kind="ExternalOutput").ap()
    x2_d = dscr("x2", [T_SEQ, D_MODEL], F32, out=dbg)
    hx2_d = dscr("hx2", [T_SEQ, D_MODEL], F32, out=dbg)
    st_qd = dscr("st_qd", [8, 128, 4, 512], BF16); st_ki = dscr("st_ki", [8, 128, 4, 512], BF16)
    st_ke = dscr("st_ke", [8, 128, 4, 4, 128], BF16); st_v = dscr("st_v", [8, 128, 4, 512], BF16)
    st_el = dscr("st_el", [8, 128, 4, 16], F32); st_of = dscr("st_of", [8, 128, 4, 512], F32)
    st_sg = dscr("st_sg", [8, 128, 4, 512], BF16); st_yb = dscr("st_yb", [8, 128, 4, 512], F32R)
    dbg_d = {}
    if dbg:
        dbg_d["s0"] = dscr("dbg_s0", [8, 128, 128], F32, out=True)
        dbg_d["aff"] = dscr("dbg_aff", [16, T_SEQ], F32, out=True)
        dbg_d["of"] = dscr("dbg_of", [8, 128, 4, 512], F32, out=True)
        dbg_d["mod"] = dscr("dbg_mod", [2, 6144], F32, out=True)

    def act(out, in_, func, rd, wr, **kw):
        S.op("act", lambda e: e.activation(out, in_, func, **kw), reads=rd, writes=wr)

    def tt(eng, out, a, b, op, rd, wr):
        S.op(eng, lambda e: e.tensor_tensor(out, a, b, op), reads=rd, writes=wr)

    def ts(eng, out, a, s1, s2, op0, op1, rd, wr):
        if s2 is None:
            S.op(eng, lambda e: e.tensor_scalar(out, a, s1, None, op0), reads=rd, writes=wr)
        else:
            S.op(eng, lambda e: e.tensor_scalar(out, a, s1, s2, op0, op1), reads=rd, writes=wr)

    def stt(out, a, s, b, op0, op1, rd, wr):
        S.op("dve", lambda e: e.scalar_tensor_tensor(out, a, s, b, op0, op1), reads=rd, writes=wr)

    def cp(eng, out, in_, rd, wr):
        if eng == "act":
            S.op("act", lambda e: e.activation(out, in_, AF.Copy), reads=rd, writes=wr)
        else:
            S.op(eng, lambda e: e.tensor_copy(out, in_), reads=rd, writes=wr)

    def mm(lst, rd, wr):
        def f(e, lst=lst):
            ins = None
            for (o, l, r, st, sp) in lst:
                ins = e.matmul(o, lhsT=l, rhs=r, start=st, stop=sp)
            return ins
        S.op("pe", f, reads=rd, writes=wr)

    def trs(lst, rd, wr):
        def f(e, lst=lst):
            ins = None
            for (o, i, idn) in lst:
                ins = e.transpose(o, i, idn)
            return ins
        S.op("pe", f, reads=rd, writes=wr)

    ipb = [(S.ps(f"ip{i}", [128, 512], F32), S.buf(f"ip{i}")) for i in range(4)]
    ipn = [0]

    def bank():
        r = ipb[ipn[0] % 4]
        ipn[0] += 1
        return r
    sc_ps, Bsc = S.ps("sc", [128, 512], F32), S.buf("sc")
    kv_ps, Bkv = S.ps("kv", [128, 512], F32), S.buf("kv")
    o_ps, Bo = S.ps("o", [128, 512], F32), S.buf("o")
    trb_ps, Btrb = S.ps("trb", [128, 512], F32), S.buf("trb")

    consts = S.sb("consts", [128, NCONST], F32); Bc = S.buf("consts")
    S.dma("sp", consts, consts_d, writes=[Bc])

    def C(name, rows=128, lo=0, hi=None):
        o, n = CL[name]
        return consts[0:rows, o + lo: o + (n if hi is None else hi)]
    ident = C("ident")
    identb = None; Bib = S.buf()
    onesr = S.sb("onesr", [128, 128], F32R); Bor = S.buf()
    cp("dve", onesr, C("ones"), [Bc], [Bor])
    mhalf = S.sb("mhalf", [128, 1], F32); Bmh = S.buf()
    S.op("pool", lambda e: e.memset(mhalf, -0.5), writes=[Bmh])

    def small_in(name, src, shape):
        t = S.sb(name, shape, F32); b = S.buf(name)
        S.dma("sp", t, src, writes=[b])
        return t, b
    ccT, Bcc = small_in("ccT", cc_d, [128, 16])
    nmix, Bnm = small_in("nmix", nmix_d, [128, 8])
    lblfm, Blf = small_in("lblfm", lblfm_d, [128, 16])
    hgn, Bhg = small_in("hgn", hgn_d, [128, 1])
    convw, Bcw = small_in("convw", convw_d, [128, 12])
    wr, Bwr = small_in("wr", wr_d, [128, 128])
    nfin, Bnf = small_in("nfin", nfin_d, [128, 1024])
    lbfm = S.sb("lbfm", [128, 8], F32); omlfm = S.sb("omlfm", [128, 8], F32); nomlfm = S.sb("nomlfm", [128, 8], F32)
    tmp8 = S.sb("tmp8", [128, 8], F32); Bl = S.buf(); Bt8 = S.buf()
    tt("dve", tmp8, lblfm[:, 0:8], lblfm[:, 8:16], ALU.subtract, [Blf], [Bt8])
    act(lbfm, tmp8, AF.Sigmoid, [Bt8], [Bl])
    ts("dve", omlfm, lbfm, -1.0, 1.0, ALU.mult, ALU.add, [Bl], [Bl])
    ts("dve", nomlfm, lbfm, 1.0, -1.0, ALU.mult, ALU.add, [Bl], [Bl])

    S32 = S.sb("S32", [128, 8, 128], F32R)
    S16 = None
    BS32 = [S.buf(f"S32_{i}") for i in range(8)]
    BS16 = [[S.buf(f"S16_{v}_{i}") for i in range(8)] for v in range(2)]
    sver = [0] * 8

    A1 = S.sb("A1", [128, 8], F32); B1 = S.sb("B1", [128, 8], F32)
    Ac = S.sb("Acx", [128, 8], F32); Bcx = S.sb("Bcx", [128, 8], F32); Bab = S.buf("A1B1")
    g1bc = S.sb("g1bc", [128, 1024], F32); A2bc = S.sb("A2bc", [128, 1024], F32)
    B2bc = S.sb("B2bc", [128, 1024], F32); g2bc = S.sb("g2bc", [128, 1024], F32); Bbc = S.buf("bcs")

    sc = S.sb("silu_c", [128, 16], F32R); Bscx = S.buf()
    modT = S.sb("modT", [128, 32], F32); BmT = S.buf()
    ss_t = S.sb("ss_t", [128, 4], F32); Bss = [S.buf() for _ in range(4)]
    S.scope_push()
    wsl = [(S.sbs(f"wsl{i}", [128, 8, 512], F32R), S.buf(f"wsl{i}")) for i in range(2)]
    wsn = [0]

    def wslot():
        r = wsl[wsn[0] % 2]
        wsn[0] += 1
        return r
    junk = S.sbs("junk", [128, 1024], F32); Bjunk = S.buf()
    xsl = [(S.sbs(f"xsl{i}", [128, 1024], F32), S.buf(f"xsl{i}")) for i in range(2)]
    xn = S.sbs("xn", [128, 1024], F32); Bxn = S.buf()
    hxT = S.sbs("hxT", [128, 8, 512], F32R); BhxT = S.buf("hxT")
    S.scope_push()
    wada_v = wada_d.rearrange("(k p) n -> p k n", p=128)
    win_v = win_d.rearrange("(k p) n -> p k n", p=128)

    act(sc, ccT, AF.Silu, [Bcc], [Bscx])
    sc3 = sc.rearrange("p (k j) -> p k j", j=2)
    nffn = S.sbs("nffn", [128, 1024], F32); Bnff = S.buf()
    S.dma("sp", nffn, nffn_d, writes=[Bnff])
    mrow = [(S.sbs(f"mrow{i}", [2, 512], F32), S.sbs(f"brow{i}", [2, 512], F32), S.buf(), S.buf()) for i in range(2)]
    psT, BpT = trb_ps, Btrb
    bcdst = {4: (g1bc, 0, 0), 5: (g1bc, 1, 0), 6: (B2bc, 0, 0), 7: (B2bc, 1, 0), 8: (A2bc, 0, 1), 9: (A2bc, 1, 1),
             10: (g2bc, 0, 0), 11: (g2bc, 1, 0)}
    for j in range(12):
        wt, Bw = wslot()
        S.dma("sp", wt, wada_v[:, :, j * 512:(j + 1) * 512], writes=[Bw])
        mr, br, Bmr, Bbr = mrow[j % 2]
        S.dma("sp", br[0:1, :], bada_d[:, j * 512:(j + 1) * 512], writes=[Bbr])
        S.dma("sp", br[1:2, :], bada_d[:, j * 512:(j + 1) * 512], writes=[Bbr])
        ps, Bp = bank()
        mm([(ps[0:2, :], sc3[:, k, :], wt[:, k, :], k == 0, k == 7) for k in range(8)], [Bscx, Bw], [Bp])
        tt("dve", mr, ps[0:2, :], br, ALU.add, [Bp, Bbr], [Bmr])
        if dbg:
            S.dma("act", dbg_d["mod"][:, j * 512:(j + 1) * 512], mr, reads=[Bmr], slot=Bmr, out_final=True)
        if j < 4:
            trs([(psT[:, (j * 4 + c) * 2:(j * 4 + c + 1) * 2], mr[0:2, c * 128:(c + 1) * 128], ident[0:2, 0:2])
                 for c in range(4)], [Bmr, Bc], [BpT])
        else:
            dst, hf, kind = bcdst[j]
            ps2, Bp2 = bank()
            mm([(ps2, C("ones", rows=1), mr[0:1, :], True, True)], [Bmr, Bc], [Bp2])
            if kind == 0:
                cp("act", dst[:, hf * 512:(hf + 1) * 512], ps2, [Bp2], [Bbc])
            else:
                stt(dst[:, hf * 512:(hf + 1) * 512], ps2, 1.0, nffn[:, hf * 512:(hf + 1) * 512], ALU.add, ALU.mult,
                    [Bp2, Bnff], [Bbc])
        if j == 3:
            cp("dve", modT, psT[:, 0:32], [BpT], [BmT])
            modT3 = modT.rearrange("p (c j) -> p c j", j=2)
            stt(A1, modT3[:, 8:16, 0], 1.0, nmix, ALU.add, ALU.mult, [BmT, Bnm], [Bab])
            cp("dve", B1, modT3[:, 0:8, 0], [BmT], [Bab])
            stt(Ac, modT3[:, 8:16, 1], 1.0, nmix, ALU.add, ALU.mult, [BmT, Bnm], [Bab])
            cp("dve", Bcx, modT3[:, 0:8, 1], [BmT], [Bab])

    if stage == 0:
        S.dma("act", out_d[0:128, :], nfin, reads=[Bnf], slot=Bnf, out_final=True)
        S.finish()
        return nc
    ssn = [0]

    def rstd_of(xt, Bx):
        i = ssn[0] % 4
        ssn[0] += 1
        col = ss_t[:, i:i + 1]
        act(junk, xt, AF.Square, [Bx], [Bjunk])
        S.op("dve", lambda e: e.reduce_sum(col, junk, AX.X), reads=[Bjunk], writes=[Bss[i]])
        ts("dve", col, col, 1.0 / D_MODEL, EPS, ALU.mult, ALU.add, [Bss[i]], [Bss[i]])
        act(col, col, AF.Sqrt, [Bss[i]], [Bss[i]])
        S.op("dve", lambda e: e.reciprocal(col, col), reads=[Bss[i]], writes=[Bss[i]])
        return col, Bss[i]

    def prep_tile(src_rows, An, Bn, col0, xi):
        xt, Bx = xsl[xi % 2]
        S.dma("sp", xt, src_rows, writes=[Bx])
        rs, Brs = rstd_of(xt, Bx)
        act(xn, xt, AF.Copy, [Bx, Brs], [Bxn], scale=rs)
        for half in range(2):
            ps, Bp = bank()
            trs([(ps[:, q * 128:(q + 1) * 128], xn[:, (half * 4 + q) * 128:(half * 4 + q + 1) * 128], ident)
                 for q in range(4)], [Bxn, Bc], [Bp])
            for q in range(4):
                k = half * 4 + q
                act(hxT[:, k, col0:col0 + 128], ps[:, q * 128:(q + 1) * 128], AF.Identity, [Bp, Bab], [BhxT],
                    scale=An[:, k:k + 1], bias=Bn[:, k:k + 1])

    lblbc = S.sbs("lblbc", [128, 2048], F32); Blb = S.buf()
    S.dma("sp", lblbc, lblbc_d, writes=[Blb])
    lbbc = S.sbs("lbbc", [128, 1024], F32); omlbc = S.sbs("omlbc", [128, 1024], F32); Blbb = S.buf()
    tt("dve", omlbc, lblbc[:, 0:1024], lblbc[:, 1024:2048], ALU.subtract, [Blb], [Blbb])
    act(lbbc, omlbc, AF.Sigmoid, [Blbb], [Blbb])
    ts("dve", omlbc, lbbc, -1.0, 1.0, ALU.mult, ALU.add, [Blbb], [Blbb])
    clogf = S.sbs("clogf", [128, 2, 2, 512], F32R); ck = S.sbs("ck", [128, 2, 2, 512], F32)
    cv16 = S.sbs("cv16", [128, 2, 512], F32R); ckd16 = S.sbs("ckd16", [128, 2, 2, 512], F32R)
    Bclf = [[S.buf() for _ in range(2)] for _ in range(2)]; Bck = [[S.buf() for _ in range(2)] for _ in range(2)]
    Bcv = [S.buf() for _ in range(2)]; Bckd = [[S.buf() for _ in range(2)] for _ in range(2)]
    csig = S.sbs("csig", [128, 512], F32); Bcs = S.buf()
    cf = S.sbs("cf", [128, 512], F32); Bcf = S.buf()
    import os
    KCUT = int(os.environ.get("KCUT", "0"))
    for i in range(2):
        prep_tile(ctx_d[i * 128:(i + 1) * 128, :], Ac, Bcx, i * 128, i)
    if KCUT == 1:
        S.dma("act", out_d[0:128, :], nfin, reads=[Bnf], slot=Bnf, out_final=True)
        S.scope_pop(); S.scope_pop()
        S.finish()
        return nc
    for j in (1, 2, 3):
        wt, Bw = wslot()
        S.dma("sp", wt, win_v[:, :, j * 512:(j + 1) * 512], writes=[Bw])
        for i in range(2):
            ps, Bp = bank()
            mm([(ps, hxT[:, k, i * 128:(i + 1) * 128], wt[:, k, :], k == 0, k == 7) for k in range(8)],
               [BhxT, Bw], [Bp])
            if j == 3:
                cp("act", cv16[:, i, :], ps, [Bp], [Bcv[i]])
            else:
                d = j - 1
                act(csig, ps, AF.Sigmoid, [Bp], [Bcs])
                tt("dve", cf, csig, omlbc[:, d * 512:(d + 1) * 512], ALU.mult, [Bcs, Blbb], [Bcf])
                tt("dve", cf, cf, lbbc[:, d * 512:(d + 1) * 512], ALU.add, [Bcf, Blbb], [Bcf])
                act(clogf[:, d, i, :], cf, AF.Ln, [Bcf], [Bclf[d][i]])
                ts("dve", ck[:, d, i, :], cf, -1.0, 1.0, ALU.mult, ALU.add, [Bcf], [Bck[d][i]])
    if KCUT == 2:
        S.dma("act", out_d[0:128, :], nfin, reads=[Bnf], slot=Bnf, out_final=True)
        S.scope_pop(); S.scope_pop()
        S.finish()
        return nc
    ones_f = onesr
    trir = S.sbs("trir", [128, 256], F32R); Btri = S.buf()
    cp("dve", trir[:, 0:128], C("mgt"), [Bc], [Btri])
    cp("dve", trir[:, 128:256], C("mlt"), [Bc], [Btri])
    for d in range(2):
        for i in range(2):
            ps, Bp = bank()
            tri = trir[:, 0:128] if d == 0 else trir[:, 128:256]
            other = 1 - i
            lst = [(ps, tri, clogf[:, d, i, :], True, False)]
            if (d == 0 and i == 0) or (d == 1 and i == 1):
                lst.append((ps, ones_f, clogf[:, d, other, :], False, True))
                rd = [Bclf[d][0], Bclf[d][1], Btri, Bor]
            else:
                lst[0] = (ps, tri, clogf[:, d, i, :], True, True)
                rd = [Bclf[d][i], Btri]
            mm(lst, rd, [Bp])
            act(csig, ps, AF.Exp, [Bp], [Bcs])
            tt("dve", ckd16[:, d, i, :], ck[:, d, i, :], csig, ALU.mult, [Bck[d][i], Bcs], [Bckd[d][i]])
    if KCUT == 3:
        S.dma("act", out_d[0:128, :], nfin, reads=[Bnf], slot=Bnf, out_final=True)
        S.scope_pop(); S.scope_pop()
        S.finish()
        return nc
    for d in range(2):
        for h in range(4):
            ps, Bp = bank()
            hs = slice(h * 128, (h + 1) * 128)
            mm([(ps[:, 0:128], ckd16[:, d, i, hs], cv16[:, i, hs], i == 0, i == 1) for i in range(2)],
               [Bckd[d][0], Bckd[d][1], Bcv[0], Bcv[1]], [Bp])
            cp("dve", S32[:, d * 4 + h, :], ps[:, 0:128], [Bp], [BS32[d * 4 + h]])
    if dbg and stage == 1:
        for nm, tns, shp, bufs in (("clogf", clogf, [128, 2048], [x for y in Bclf for x in y]), ("ck", ck, [128, 2048], [x for y in Bck for x in y]),
                                   ("ckd", ckd16, [128, 2048], [x for y in Bckd for x in y]), ("cv", cv16, [128, 1024], Bcv),
                                   )[:int(os.environ.get("NDBG", "4"))]:
            dd_ = dscr("dbg_" + nm, shp, F32, out=True)
            flat = tns.bitcast(F32) if nm != "ck" else tns
            if flat.ndim == 4:
                flat = flat.rearrange("p a b c -> p (a b c)")
            elif flat.ndim == 3:
                flat = flat.rearrange("p a b -> p (a b)")
            S.dma("sp", dd_, flat, reads=bufs, slot=S.buf(), out_final=True)
    if dbg and stage == 1:
        for hh in range(2):
            dd_ = dscr(f"dbg_hcT{hh}", [128, 2048], F32, out=True)
            S.dma("sp", dd_, hxT.bitcast(F32)[:, hh * 4:(hh + 1) * 4, :].rearrange("p a b -> p (a b)"), reads=[BhxT], slot=S.buf(), out_final=True)
        dd_ = dscr("dbg_xn", [128, 1024], F32, out=True)
        S.dma("sp", dd_, xn, reads=[Bxn], slot=S.buf(), out_final=True)
        dd_ = dscr("dbg_ss", [128, 4], F32, out=True)
        S.dma("sp", dd_, ss_t, reads=Bss, slot=S.buf(), out_final=True)
    if KCUT == 4:
        S.dma("act", out_d[0:128, :], nfin, reads=[Bnf], slot=Bnf, out_final=True)
        S.scope_pop(); S.scope_pop()
        S.finish()
        return nc
    if dbg:
        S.dma("act", dbg_d["s0"].rearrange("s p v -> p s v"), S32.bitcast(F32), reads=BS32, slot=BS32[0], out_final=True)
    S.scope_pop()
    if stage == 1:
        S.dma("act", out_d[0:128, :], nfin, reads=[Bnf], slot=Bnf, out_final=True)
        S.finish()
        return nc
    return _build_rest(nc, S, locals(), stage, dbg)


def _host_layouts(inputs):
    f = lambda a: np.ascontiguousarray(np.asarray(a, dtype=np.float32))
    fm = lambda v: f(np.asarray(v).reshape(-1, 128).T)
    sh = {}
    sh["w_ada"] = f(inputs["w_ada"][0]); sh["b_ada"] = f(inputs["b_ada"][0]).reshape(1, 6144)
    sh["nmix_fm"] = fm(inputs["norm_mix"][0])
    sh["nffn_bc"] = f(np.broadcast_to(np.asarray(inputs["norm_ffn"][0])[None, :], (128, 1024)))
    sh["nfin_bc"] = f(np.broadcast_to(np.asarray(inputs["norm_final"])[None, :], (128, 1024)))
    sh["w_in"] = f(inputs["w_in"][0])
    lbl = np.asarray(inputs["lb_logits"], dtype=np.float32)
    sh["lbl_fm"] = f(np.concatenate([fm(lbl[0].reshape(-1)), fm(lbl[1].reshape(-1))], axis=1))
    sh["lbl_bc"] = f(np.broadcast_to(lbl.reshape(1, 2048), (128, 2048)))
    sh["hgn_fm"] = f(np.asarray(inputs["hg_norm"][0]).reshape(128, 1))
    cw = np.asarray(inputs["conv_w"][0], dtype=np.float32)
    sh["convw_fm"] = f(cw.reshape(3, 4, 128).transpose(2, 1, 0).reshape(128, 12))
    sh["w_out"] = f(inputs["w_out"][0])
    wr = np.asarray(inputs["w_router"][0], dtype=np.float32)
    sh["w_r_fm"] = f(wr.reshape(8, 128, 16).transpose(1, 0, 2).reshape(128, 128))
    sh["w_gate"] = f(inputs["w_gate"][0]); sh["w_up"] = f(inputs["w_up"][0]); sh["w_down"] = f(inputs["w_down"][0])
    sh["consts"] = make_consts()
    return sh


def _core_inputs(inputs, sh, b):
    f = lambda a: np.ascontiguousarray(np.asarray(a, dtype=np.float32))
    m = dict(sh)
    m["x"] = f(inputs["x"][b]); m["ctx"] = f(inputs["ctx"][b])
    cc = np.stack([np.asarray(inputs["c"][b]).reshape(8, 128).T, np.asarray(inputs["c_ctx"]).reshape(8, 128).T],
                  axis=-1)
    m["ccT"] = f(cc.reshape(128, 16))
    return m


_PROG = {}


def kernel(**inputs):
    if "full" not in _PROG:
        _PROG["full"] = build_program()
    nc = _PROG["full"]
    sh = _host_layouts(inputs)
    in_maps = [_core_inputs(inputs, sh, b) for b in range(8)]
    if not HAS_MOE:
        for m in in_maps:
            for k in ("w_gate", "w_up", "w_down"):
                m.pop(k, None)
    res = run_bass_kernel_spmd(nc, in_maps, core_ids=list(range(8)))
    return np.stack([np.asarray(r["out"], dtype=np.float32) for r in res.results], axis=0)


def _build_rest(nc, S, L, stage, dbg):
    g = lambda n: L[n]
    (act, tt, ts, stt, cp, mm, trs, bank, C, wslot, prep_tile, rstd_of) = [g(n) for n in (
        "act", "tt", "ts", "stt", "cp", "mm", "trs", "bank", "C", "wslot", "prep_tile", "rstd_of")]
    (x_d, out_d, x2_d, hx2_d, win_v, wout_d, dbg_d, st_of) = [g(n) for n in (
        "x_d", "out_d", "x2_d", "hx2_d", "win_v", "wout_d", "dbg_d", "st_of")]
    (ident, identb, onesr, Bc, Bib, Bor, lbfm, omlfm, nomlfm, Bl, S32, S16, BS32, BS16, sver, A1, B1, Bab,
     g1bc, A2bc, B2bc, g2bc, Bbc, hxT, BhxT, xsl, xn, Bxn, junk, Bjunk, hgn, Bhg, convw, Bcw, wr, Bwr, nfin, Bnf,
     sc_ps, Bsc, kv_ps, Bkv, o_ps, Bo, trb_ps, Btrb) = [g(n) for n in (
         "ident", "identb", "onesr", "Bc", "Bib", "Bor", "lbfm", "omlfm", "nomlfm", "Bl", "S32", "S16", "BS32",
         "BS16", "sver", "A1", "B1", "Bab", "g1bc", "A2bc", "B2bc", "g2bc", "Bbc", "hxT", "BhxT", "xsl", "xn",
         "Bxn", "junk", "Bjunk", "hgn", "Bhg", "convw", "Bcw", "wr", "Bwr", "nfin", "Bnf",
         "sc_ps", "Bsc", "kv_ps", "Bkv", "o_ps", "Bo", "trb_ps", "Btrb")]
    aff_d = nc.dram_tensor("aff_scr", [16, T_SEQ], F32, kind=("ExternalOutput" if dbg else "Internal")).ap()
    wout_v = wout_d.rearrange("(k p) n -> p k n", p=128)
    xcnt = [0]

    S.scope_push()
    Ta = S.sbs("Ta", [128, 512], F32); Tb = S.sbs("Tb", [128, 512], F32)
    Tc = S.sbs("Tc", [128, 512], F32); Td = S.sbs("Td", [128, 512], F32)
    BTa, BTb, BTc, BTd = S.buf(), S.buf(), S.buf(), S.buf()
    kend16 = S.sbs("kend16", [128, 512], F32); Bke16 = S.buf()
    Eall = S.sbs("Eall", [128, 4, 512], F32); BE = [S.buf() for _ in range(4)]
    QD = S.sbs("QD", [128, 4, 512], F32R); BQD = [S.buf() for _ in range(4)]
    KI = S.sbs("KI", [128, 4, 512], F32R); BKI = [S.buf() for _ in range(4)]
    KE = S.sbs("KE", [128, 4, 4, 128], F32R); BKE = [S.buf() for _ in range(4)]
    EL = S.sbs("EL", [128, 4, 16], F32); BEL = [S.buf() for _ in range(4)]
    V16 = S.sbs("V16", [128, 4, 512], F32R); BV = [S.buf() for _ in range(4)]
    VMs = [(S.sbs(f"VM{i}", [128, 512], F32R), S.buf(f"VM{i}")) for i in range(2)]
    vmn = [0]
    AT16 = S.sbs("AT16", [128, 512], F32R); BAT = S.buf()
    OB = S.sbs("OB", [128, 4, 512], F32); BOB = [S.buf() for _ in range(4)]
    SG = S.sbs("SG", [128, 4, 512], BF16); BSG = [S.buf() for _ in range(4)]
    cvs = S.sbs("cvs", [128, 4, 512], F32); Bcvs = [S.buf() for _ in range(4)]
    MIXT = S.sbs("MIXT", [128, 8, 512], F32R); BMX = [S.buf() for _ in range(8)]
    MIXTf = MIXT.bitcast(F32)
    hx2T = S.sbs("hx2T", [128, 8, 128], F32); Bh2T = S.buf()
    osq = S.sbs("osq", [128, 512], F32R); Bosq = S.buf()
    affsb = S.sbs("affsb", [16, 128], F32); Baf = S.buf()
    eT = S.sbs("eT", [16, 128], F32); BeT = S.buf()
    Bstash = [S.buf(f"stash{i}") for i in range(8)]

    def gate_prep(d, h, ps, Bp):
        col = d * 4 + h
        act(Ta, ps, AF.Sigmoid, [Bp], [BTa])
        act(Tb, Ta, AF.Ln, [BTa, Bl], [BTb], scale=omlfm[:, col:col + 1], bias=lbfm[:, col:col + 1])
        ts("dve", Tc, Ta, nomlfm[:, col:col + 1], omlfm[:, col:col + 1], ALU.mult, ALU.add, [BTa, Bl], [BTc])
        if d == 0:
            S.op("dve", lambda e: e.tensor_tensor_scan(Td, C("rsf"), Tb, 0.0, ALU.mult, ALU.add),
                 reads=[BTb, Bc], writes=[BTd])
        else:
            S.op("dve", lambda e: e.tensor_tensor_scan(Td[:, ::-1], C("rsb")[:, ::-1], Tb[:, ::-1], 0.0,
                                                       ALU.mult, ALU.add), reads=[BTb, Bc], writes=[BTd])
        act(Eall[:, h, :], Td, AF.Exp, [BTd], [BE[h]])
        act(Ta, Td, AF.Exp, [BTd], [BTa], scale=-1.0)
        tt("dve", Tc, Tc, Ta, ALU.mult, [BTc, BTa], [BTc])
        cp("act", KI[:, h, :], Tc, [BTc], [BKI[h]])
        E3 = Eall[:, h, :].rearrange("p (c k) -> p c k", k=32)
        cp("pool", EL[:, h, :], E3[:, :, 31] if d == 0 else E3[:, :, 0], [BE[h]], [BEL[h]])
        elb = EL[:, h, :].rearrange("p (c o) -> p c o", o=1).to_broadcast([128, 16, 32])
        tt("dve", kend16.rearrange("p (c k) -> p c k", k=32), Tc.rearrange("p (c k) -> p c k", k=32), elb,
           ALU.mult, [BTc, BEL[h]], [Bke16])
        trs([(trb_ps[:, t * 128:(t + 1) * 128], kend16[:, t * 128:(t + 1) * 128], ident) for t in range(4)],
            [Bke16, Bc], [Btrb])
        cp("act", KE[:, h, :, :].rearrange("p t d -> p (t d)"), trb_ps[:, 0:512], [Btrb], [BKE[h]])

    kvb = [(kv_ps, Bkv), (trb_ps, Btrb)]
    S32f = S32.bitcast(F32)
    V16f = V16.bitcast(F32)

    def gla_group(d):
        order = list(range(4)) if d == 0 else [3, 2, 1, 0]
        mk = (C("maskf") if d == 0 else C("maskb")).rearrange("p (o q) -> p o q", o=1).to_broadcast([128, 4, 128])
        hsl = [slice(h * 128, (h + 1) * 128) for h in range(4)]
        Bst = [BS32[d * 4 + h] for h in range(4)]
        for t in order:
            tcol = slice(t * 128, (t + 1) * 128)
            mm([(sc_ps[:, hsl[h]], KI[:, h, tcol], QD[:, h, tcol], True, True) for h in range(4)], BKI + BQD, [Bsc])
            tt("dve", AT16.rearrange("p (t q) -> p t q", q=128), sc_ps.rearrange("p (t q) -> p t q", q=128), mk, ALU.mult,
               [Bsc, Bc], [BAT])
            mm([(o_ps[:, hsl[h]], V16[:, t, hsl[h]], AT16[:, hsl[h]], h == 0, False) for h in range(4)], [BV[t], BAT], [Bo])
            for ci, cc in enumerate(order):
                VMb, BVMb = VMs[vmn[0] % 2]
                kvp, Bkvp = kvb[vmn[0] % 2]
                vmn[0] += 1
                act(VMb, V16f[:, t, :], AF.Copy, [BV[t], Bc], [BVMb], scale=C("cind")[:, cc:cc + 1])
                mm([(kvp[:, hsl[h]], KE[:, h, t, :], VMb[:, hsl[h]], True, True) for h in range(4)], BKE + [BVMb], [Bkvp])
                mm([(o_ps[:, h * 128 + cc * 32: h * 128 + cc * 32 + 32], S32[:, d * 4 + h, :],
                     QD[:, h, t * 128 + cc * 32: t * 128 + cc * 32 + 32], False, ci == 3) for h in range(4)],
                   Bst + BQD, [Bo])
                chunk = t * 4 + cc

                def upd(e, kvp=kvp, chunk=chunk):
                    ins = None
                    for h in range(4):
                        col = d * 4 + h
                        ins = e.scalar_tensor_tensor(S32[:, col, :], S32f[:, col, :], EL[:, h, chunk:chunk + 1],
                                                     kvp[:, hsl[h]], ALU.mult, ALU.add)
                    return ins
                S.op("dve", upd, reads=Bst + BEL + [Bkvp], writes=Bst)
            o3 = o_ps.rearrange("p (h q) -> p h q", q=128)
            if d == 1:
                cp("act", OB[:, :, tcol], o3, [Bo], BOB)
            else:
                tt("dve", OB[:, :, tcol], o3, OB[:, :, tcol], ALU.add, [Bo] + BOB, BOB)

    def sweep(d):
        groups = range(8) if d == 0 else range(7, -1, -1)
        pieces = [1, 0, 3, 4, 7, 6, 5] if d == 0 else [2, 0, 3]
        for gi in groups:
            for t in range(4):
                r0 = (gi * 4 + t) * 128
                prep_tile(x_d[r0:r0 + 128, :], A1, B1, t * 128, xcnt[0])
                xcnt[0] += 1
            if d == 0:
                S.dma("sp", OB, st_of[gi], reads=[Bstash[gi]], writes=BOB, slot=BOB[0])
            for j in pieces:
                wt, Bw = wslot()
                S.dma("sp", wt, win_v[:, :, j * 512:(j + 1) * 512], writes=[Bw])
                for q in range(4):
                    ps, Bp = bank()
                    if j == 3:
                        mm([(ps, hxT[:, k, q * 128:(q + 1) * 128], wt[:, k, :], k == 0, k == 7) for k in range(8)],
                           [BhxT, Bw], [Bp])
                        cp("act", V16[:, q, :], ps, [Bp], [BV[q]])
                        continue
                    mm([(ps, wt[:, k, q * 128:(q + 1) * 128], hxT[:, k, :], k == 0, k == 7) for k in range(8)],
                       [BhxT, Bw], [Bp])
                    if j in (1, 2):
                        gate_prep(d, q, ps, Bp)
                    elif j == 0:
                        tt("dve", QD[:, q, :], ps, Eall[:, q, :], ALU.mult, [Bp, BE[q]], [BQD[q]])
                    elif j == 4:
                        act(SG[:, q, :], ps, AF.Silu, [Bp], [BSG[q]])
                    elif j == 7:
                        cp("act", cvs[:, q, :], ps, [Bp], [Bcvs[q]])
                    elif j == 6:
                        u = cvs[:, q, :]
                        y = Eall[:, q, :]
                        tt("dve", u, ps, u, ALU.mult, [Bp, Bcvs[q]], [Bcvs[q]])
                        ts("dve", y, u, convw[:, q * 3 + 1:q * 3 + 2], None, ALU.mult, None, [Bcvs[q], Bcw], [BE[q]])
                        u3 = u.rearrange("p (r w) -> p r w", w=64); y3 = y.rearrange("p (r w) -> p r w", w=64)
                        stt(y3[:, :, 1:64], u3[:, :, 0:63], convw[:, q * 3:q * 3 + 1], y3[:, :, 1:64], ALU.mult, ALU.add,
                            [Bcvs[q], Bcw, BE[q]], [BE[q]])
                        stt(y3[:, :, 0:63], u3[:, :, 1:64], convw[:, q * 3 + 2:q * 3 + 3], y3[:, :, 0:63], ALU.mult,
                            ALU.add, [Bcvs[q], Bcw, BE[q]], [BE[q]])
                    elif j == 5:
                        tt("dve", MIXT[:, 4 + q, :], ps, Eall[:, q, :], ALU.mult, [Bp, BE[q]], [BMX[4 + q]])
            gla_group(d)
            if d == 0:
                for h in range(4):
                    osum = OB[:, h, :]
                    act(osq, osum, AF.Square, [BOB[h]], [Bosq])
                    ps, Bp = bank()
                    mm([(ps, onesr, osq, True, True)], [Bosq, Bor], [Bp])
                    ts("dve", Td, ps, 1.0 / 128.0, EPS, ALU.mult, ALU.add, [Bp], [BTd])
                    act(Td, Td, AF.Sqrt, [BTd], [BTd])
                    S.op("dve", lambda e: e.reciprocal(Td, Td), reads=[BTd], writes=[BTd])
                    tt("dve", Tb, osum, Td, ALU.mult, [BOB[h], BTd], [BTb])
                    stt(MIXT[:, h, :], Tb, hgn[:, 0:1], SG[:, h, :], ALU.mult, ALU.mult, [BTb, Bhg, BSG[h]], [BMX[h]])
            if d == 1:
                S.dma("act", st_of[gi], OB, reads=BOB, writes=[Bstash[gi]], slot=BOB[0])
                if dbg:
                    S.dma("act", dbg_d["of"][gi], OB, reads=BOB, slot=BOB[0], out_final=True)
                continue
            wo = []
            for hf in range(2):
                wt, Bw = wslot()
                S.dma("sp", wt, wout_v[:, :, hf * 512:(hf + 1) * 512], writes=[Bw])
                wo.append((wt, Bw))
            for t in range(4):
                r0 = (gi * 4 + t) * 128
                xt, Bx = xsl[xcnt[0] % 2]
                xcnt[0] += 1
                S.dma("sp", xt, x_d[r0:r0 + 128, :], writes=[Bx])
                for hf in range(2):
                    ps, Bp = bank()
                    wt, Bw = wo[hf]
                    mm([(ps, MIXT[:, kc, t * 128:(t + 1) * 128], wt[:, kc, :], kc == 0, kc == 7) for kc in range(8)],
                       BMX + [Bw], [Bp])
                    hsl = slice(hf * 512, (hf + 1) * 512)
                    tt("dve", xn[:, hsl], ps, g1bc[:, hsl], ALU.mult, [Bp, Bbc], [Bxn])
                    tt("pool", xn[:, hsl], xn[:, hsl], xt[:, hsl], ALU.add, [Bxn, Bx], [Bxn])
                S.dma("act", x2_d[r0:r0 + 128, :], xn, reads=[Bxn], slot=Bxn, out_final=dbg)
                rs, Brs = rstd_of(xn, Bxn)
                stt(junk, xn, rs, A2bc, ALU.mult, ALU.mult, [Bxn, Brs, Bbc], [Bjunk])
                tt("pool", junk, junk, B2bc, ALU.add, [Bjunk, Bbc], [Bjunk])
                S.dma("act", hx2_d[r0:r0 + 128, :], junk, reads=[Bjunk], slot=Bjunk, out_final=dbg)
                for half in range(2):
                    ps, Bp = bank()
                    trs([(ps[:, q * 128:(q + 1) * 128], junk[:, (half * 4 + q) * 128:(half * 4 + q + 1) * 128], ident)
                         for q in range(4)], [Bjunk, Bc], [Bp])
                    cp("act", hx2T[:, half * 4:(half + 1) * 4, :].rearrange("p k t -> p (k t)"), ps, [Bp], [Bh2T])
                ps, Bp = bank()
                mm([(ps[0:16, 0:128], wr[:, k * 16:(k + 1) * 16], hx2T[:, k, :], k == 0, k == 7) for k in range(8)],
                   [Bwr, Bh2T], [Bp])
                act(eT, ps[0:16, 0:128], AF.Exp, [Bp], [BeT])
                ps2, Bp2 = bank()
                mm([(ps2[0:16, 0:128], C("ones", rows=16, hi=16), eT, True, True)], [BeT, Bc], [Bp2])
                S.op("dve", lambda e, ps2=ps2: e.reciprocal(affsb, ps2[0:16, 0:128]), reads=[Bp2], writes=[Baf])
                tt("dve", affsb, affsb, eT, ALU.mult, [Baf, BeT], [Baf])
                S.dma("act", aff_d[:, r0:r0 + 128], affsb, reads=[Baf], slot=Baf, out_final=dbg)

    sweep(1)
    if stage == 2:
        S.dma("act", out_d[0:128, :], nfin, reads=[Bnf], slot=Bnf, out_final=True)
        S.scope_pop(); S.scope_pop()
        S.finish()
        return nc
    sweep(0)
    S.scope_pop()
    S.scope_pop()
    if stage == 3:
        S.dma("act", out_d[0:128, :], nfin, reads=[Bnf], slot=Bnf, out_final=True)
        S.finish()
        return nc
    return _build_moe(nc, S, L, locals(), stage, dbg)


def _build_moe(nc, S, L, L2, stage, dbg):
    import concourse.bass as bass_
    act, tt, ts, stt, cp, mm, trs, C = [L[n] for n in ("act", "tt", "ts", "stt", "cp", "mm", "trs", "C")]
    x2_d, hx2_d, out_d, nfin, Bnf, g2bc, Bbc, Bc, ident, ipb = [L[n] for n in (
        "x2_d", "hx2_d", "out_d", "nfin", "Bnf", "g2bc", "Bbc", "Bc", "ident", "ipb")]
    wg_d, wu_d, wd_d = L["wg_d"], L["wu_d"], L["wd_d"]
    aff_d = L2["aff_d"]
    yacc = [(L["sc_ps"], L["Bsc"]), (L["kv_ps"], L["Bkv"]), (L["o_ps"], L["Bo"]), (L["trb_ps"], L["Btrb"])]
    R_ps, BR = ipb[3]
    rot = ipb[0:3]
    rn = [0]

    def bank3():
        r = rot[rn[0] % 3]
        rn[0] += 1
        return r
    Bx2all = S.buf("x2all")
    Bhx2all = S.buf("hx2all")

    S.scope_push()
    slotT = S.sbs("slotT", [128, 4, 128], F32); BslT = S.buf()
    affT = S.sbs("affT", [128, 4, 128], F32); BafT = S.buf()
    vals = S.sbs("vals", [128, 4, 128, 3], F32R); Bvals = S.buf()
    Rsb = S.sbs("Rsb", [3, 512], F32); BRsb = S.buf()
    IG = S.sbs("IG", [128, 12], F32); BIG = S.buf()
    idxs = [(S.sbs(f"idx{i}", [128, 4], I32), S.buf(f"idx{i}")) for i in range(2)]
    gates = [(S.sbs(f"gate{i}", [128, 4], F32), S.buf(f"gate{i}")) for i in range(2)]
    sm = S.sbs("smalls", [128, 16], F32); Bsm = S.buf()
    lo, hi, mid, cnt, ge, dl, nge, off = [sm[:, i:i + 1] for i in range(8)]
    S.scope_push()
    aff128 = S.sbs("aff128", [128, 512], F32); Ba128 = S.buf()
    msk = S.sbs("msk", [128, 512], F32); Bmsk = S.buf()
    cum = S.sbs("cum", [128, 512], F32); Bcum = S.buf()

    S.dma("sp", aff128, aff_d.rearrange("e (s t) -> (e s) t", t=512), writes=[Ba128])
    S.op("dve", lambda e: e.memset(sm, 0.0), writes=[Bsm])
    S.op("dve", lambda e: e.memset(hi, 2.0), reads=[Bsm], writes=[Bsm])
    for it in range(34):
        tt("dve", mid, lo, hi, ALU.add, [Bsm], [Bsm])
        ts("dve", mid, mid, 0.5, None, ALU.mult, None, [Bsm], [Bsm])
        ts("dve", msk, aff128, mid, None, ALU.is_ge, None, [Ba128, Bsm], [Bmsk])
        S.op("dve", lambda e: e.reduce_sum(cnt, msk, AX.X), reads=[Bmsk], writes=[Bsm])
        ps, Bp = bank3()
        mm([(ps[:, 0:1], C("bones"), cnt, True, True)], [Bc, Bsm], [Bp])
        ts("dve", ge, ps[:, 0:1], float(CAP), None, ALU.is_ge, None, [Bp], [Bsm])
        tt("dve", dl, mid, lo, ALU.subtract, [Bsm], [Bsm])
        stt(lo, dl, ge, lo, ALU.mult, ALU.add, [Bsm], [Bsm])
        tt("dve", dl, hi, mid, ALU.subtract, [Bsm], [Bsm])
        stt(hi, dl, ge, mid, ALU.mult, ALU.add, [Bsm], [Bsm])
    import os
    MOECUT = int(os.environ.get("MOECUT", "0"))
    MOEN = int(os.environ.get("MOEN", str(N_EXP)))
    ones512 = S.sbs("ones512", [128, 512], F32); Bo512 = S.buf()
    S.op("pool", lambda e: e.memset(ones512, 1.0), writes=[Bo512])
    ts("dve", msk, aff128, lo, None, ALU.is_ge, None, [Ba128, Bsm], [Bmsk])
    S.op("dve", lambda e: e.reduce_sum(cnt, msk, AX.X), reads=[Bmsk], writes=[Bsm])
    ps, Bp = bank3()
    mm([(ps[:, 0:1], C("segpre"), cnt, True, True)], [Bc, Bsm], [Bp])
    cp("dve", off, ps[:, 0:1], [Bp], [Bsm])
    S.op("dve", lambda e: e.tensor_tensor_scan(cum, ones512, msk, off, ALU.mult, ALU.add),
         reads=[Bmsk, Bsm, Bo512], writes=[Bcum])
    tt("dve", cum, cum, msk, ALU.mult, [Bcum, Bmsk], [Bcum])
    ts("dve", cum, cum, -1.0, None, ALU.add, None, [Bcum], [Bcum])
    for (src, Bsrc, dst, Bdst) in ((cum, Bcum, slotT, BslT), (aff128, Ba128, affT, BafT)):
        ps, Bp = bank3()
        trs([(ps[:, c * 128:(c + 1) * 128], src[:, c * 128:(c + 1) * 128], ident) for c in range(4)], [Bsrc, Bc], [Bp])
        cp("act", dst.rearrange("p c q -> p (c q)"), ps, [Bp], [Bdst])
    valsf = vals.bitcast(F32)
    cp("dve", vals[:, :, :, 1], affT, [BafT], [Bvals])
    tt("dve", vals[:, :, :, 2], affT, valsf[:, :, :, 1], ALU.subtract, [BafT, Bvals], [Bvals])
    tki = C("tokidx").rearrange("p (c o s) -> p c o s", c=4, o=1)
    for c in range(4):
        cp("dve", vals[:, c, :, 0].rearrange("p (e s) -> p e s", s=8), tki[:, c, :, :].to_broadcast([128, 16, 8]),
           [Bc], [Bvals])

    S.scope_pop()
    S.scope_push()
    wsl = [(S.sbs(f"mw{i}", [128, 8, 512], F32R), S.buf(f"mw{i}")) for i in range(3)]
    wn = [0]

    def wslot():
        r = wsl[wn[0] % 3]
        wn[0] += 1
        return r
    xsT = [(S.sbs(f"xsT{i}", [128, 8, 512], F32R), S.buf(f"xsT{i}")) for i in range(2)]
    hidT = S.sbs("hidT", [128, 16, 512], F32R); Bhid = [S.buf() for _ in range(16)]
    xstok = [(S.sbs(f"xstok{i}", [128, 1024], F32), S.buf(f"xstok{i}")) for i in range(2)]
    ysb = [(S.sbs(f"ysb{i}", [128, 1024], F32), S.buf(f"ysb{i}")) for i in range(4)]
    sgt = [(S.sbs(f"sgt{i}", [128, 512], F32), S.buf(f"sgt{i}")) for i in range(2)]
    Qb = [(S.sbs(f"Qb{i}", [128, 512], F32R), S.buf(f"Qb{i}")) for i in range(3)]

    if dbg:
        dbg_lo = nc.dram_tensor("dbg_lo", [128, 16], F32, kind="ExternalOutput").ap()
        dbg_idx = nc.dram_tensor("dbg_idx", [16, 128, 4], I32, kind="ExternalOutput").ap()
        dbg_gate = nc.dram_tensor("dbg_gate", [16, 128, 4], F32, kind="ExternalOutput").ap()
        dbg_slot = nc.dram_tensor("dbg_slot", [128, 512], F32, kind="ExternalOutput").ap()
        S.dma("sp", dbg_lo, sm, reads=[Bsm], slot=S.buf(), out_final=True)
        S.dma("sp", dbg_slot, slotT.rearrange("p c q -> p (c q)"), reads=[BslT], slot=S.buf(), out_final=True)

    def prep(e):
        xT, BxT = xsT[e % 2]
        idx, Bidx = idxs[e % 2]
        gat, Bgat = gates[e % 2]
        for i in range(32):
            s_, c = i // 4, i % 4
            col = e * 8 + s_
            Q, BQ = Qb[i % 3]
            ts("dve", Q, C("iota"), slotT[:, c, col:col + 1], None, ALU.is_equal, None,
               [Bc, BslT], [BQ])
            mm([(R_ps[0:3, :], vals[:, c, col, :], Q, i == 0, i == 31)], [Bvals, BQ], [BR])
            yield
        cp("act", Rsb, R_ps[0:3, :], [BR], [BRsb])
        ps, Bp = bank3()
        trs([(ps[:, b * 3:(b + 1) * 3], Rsb[0:3, b * 128:(b + 1) * 128], ident[0:3, 0:3]) for b in range(4)],
            [BRsb, Bc], [Bp])
        cp("dve", IG, ps[:, 0:12], [Bp], [BIG])
        IG3 = IG.rearrange("p (b j) -> p b j", j=3)
        cp("dve", idx, IG3[:, :, 0], [BIG], [Bidx])
        tt("dve", gat, IG3[:, :, 1], IG3[:, :, 2], ALU.add, [BIG], [Bgat])
        if dbg:
            S.dma("sp", dbg_idx[e], idx, reads=[Bidx], slot=S.buf(), out_final=True)
            S.dma("sp", dbg_gate[e], gat, reads=[Bgat], slot=S.buf(), out_final=True)
        yield
        for b in range(4):
            xt, Bx = xstok[b % 2]
            S.op_dma_ind("pool", lambda en, xt=xt, b=b: en.indirect_dma_start(
                out=xt, out_offset=None, in_=hx2_d, in_offset=bass_.IndirectOffsetOnAxis(ap=idx[:, b:b + 1], axis=0)),
                reads=[Bidx, Bhx2all], writes=[Bx])
            for half in range(2):
                ps, Bp = bank3()
                trs([(ps[:, q * 128:(q + 1) * 128], xt[:, (half * 4 + q) * 128:(half * 4 + q + 1) * 128], ident)
                     for q in range(4)], [Bx, Bc], [Bp])
                for q in range(4):
                    cp("act", xT[:, half * 4 + q, b * 128:(b + 1) * 128], ps[:, q * 128:(q + 1) * 128], [Bp], [BxT])
            yield

    def drain(gen):
        for _ in gen:
            pass

    def tick(gen):
        if gen is not None:
            next(gen, None)

    if MOECUT != 2:
        drain(prep(0))
    for e in range(MOEN if MOECUT not in (2, 3) else 0):
        gen = prep(e + 1) if e + 1 < MOEN else None
        xT, BxT = xsT[e % 2]
        idx, Bidx = idxs[e % 2]
        gat, Bgat = gates[e % 2]
        wgv = wg_d[e].rearrange("(k p) f -> p k f", p=128)
        wuv = wu_d[e].rearrange("(k p) f -> p k f", p=128)
        wdv = wd_d[e].rearrange("(g c p) d -> g p c d", p=128, c=4)
        for fg in range(4):
            wgt, Bwg = wslot()
            S.dma("sp", wgt, wgv[:, :, fg * 512:(fg + 1) * 512], writes=[Bwg])
            wut, Bwu = wslot()
            S.dma("sp", wut, wuv[:, :, fg * 512:(fg + 1) * 512], writes=[Bwu])
            for fc in range(4):
                f = fg * 4 + fc
                sg, Bsg = sgt[f % 2]
                ps, Bp = bank3()
                mm([(ps, wgt[:, k, fc * 128:(fc + 1) * 128], xT[:, k, :], k == 0, k == 7) for k in range(8)],
                   [Bwg, BxT], [Bp])
                act(sg, ps, AF.Silu, [Bp], [Bsg])
                tick(gen)
                ps2, Bp2 = bank3()
                mm([(ps2, wut[:, k, fc * 128:(fc + 1) * 128], xT[:, k, :], k == 0, k == 7) for k in range(8)],
                   [Bwu, BxT], [Bp2])
                tt("dve", hidT[:, f, :], ps2, sg, ALU.mult, [Bp2, Bsg], [Bhid[f]])
                tick(gen)
        for dh in range(2):
            for fg in range(4):
                wt, Bw = wslot()
                wt4 = wt.rearrange("p (a c) d -> p a c d", a=2)[:, 0, :, :]
                S.dma("sp", wt4, wdv[fg][:, :, dh * 512:(dh + 1) * 512], writes=[Bw])
                for t4 in range(4):
                    ya, Bya = yacc[t4]
                    mm([(ya, hidT[:, fg * 4 + fc, t4 * 128:(t4 + 1) * 128], wt4[:, fc, :], fg == 0 and fc == 0,
                         fg == 3 and fc == 3) for fc in range(4)], [Bhid[fg * 4 + fc] for fc in range(4)] + [Bw], [Bya])
                    tick(gen)
            for t4 in range(4):
                ya, Bya = yacc[t4]
                yt, By = ysb[t4]
                hsl = slice(dh * 512, (dh + 1) * 512)
                stt(yt[:, hsl], ya, gat[:, t4:t4 + 1], g2bc[:, hsl], ALU.mult, ALU.mult, [Bya, Bgat, Bbc], [By])
        for t4 in range(4):
            yt, By = ysb[t4]
            S.op_dma_ind("pool", lambda en, yt=yt, t4=t4, idx=idx: en.indirect_dma_start(
                out=x2_d, out_offset=bass_.IndirectOffsetOnAxis(ap=idx[:, t4:t4 + 1], axis=0), in_=yt, in_offset=None,
                compute_op=ALU.add), reads=[Bidx, By, Bx2all], writes=[Bx2all], slot=By)
        if gen is not None:
            drain(gen)
    S.scope_pop()
    S.scope_pop()

    S.scope_push()
    xs = [(S.sbs(f"fx{i}", [128, 1024], F32), S.buf()) for i in range(2)]
    ys = [(S.sbs(f"fy{i}", [128, 1024], F32), S.buf()) for i in range(2)]
    jk = S.sbs("fjunk", [128, 1024], F32); Bjk = S.buf()
    ss = S.sbs("fss", [128, 4], F32); Bs4 = [S.buf() for _ in range(4)]
    for t in range(32):
        xt, Bx = xs[t % 2]; yt, By = ys[t % 2]
        S.dma("sp", xt, x2_d[t * 128:(t + 1) * 128, :], reads=[Bx2all], writes=[Bx])
        col = ss[:, t % 4:t % 4 + 1]; Bcol = Bs4[t % 4]
        act(jk, xt, AF.Square, [Bx], [Bjk])
        S.op("dve", lambda en, col=col: en.reduce_sum(col, jk, AX.X), reads=[Bjk], writes=[Bcol])
        ts("dve", col, col, 1.0 / D_MODEL, EPS, ALU.mult, ALU.add, [Bcol], [Bcol])
        act(col, col, AF.Sqrt, [Bcol], [Bcol])
        S.op("dve", lambda en, col=col: en.reciprocal(col, col), reads=[Bcol], writes=[Bcol])
        stt(yt, xt, col, nfin, ALU.mult, ALU.mult, [Bx, Bcol, Bnf], [By])
        S.dma("act", out_d[t * 128:(t + 1) * 128, :], yt, reads=[By], slot=By, out_final=True)
    S.scope_pop()
    S.finish()
    return nc
```

```python
import numpy as np
import concourse.bass as bass
import concourse.mybir as mybir
from concourse.bass_utils import run_bass_kernel_spmd

F32 = mybir.dt.float32
BF16 = mybir.dt.bfloat16
I32 = mybir.dt.int32
F32R = mybir.dt.float32r
AF = mybir.ActivationFunctionType
ALU = mybir.AluOpType
AX = mybir.AxisListType

ENGS = ("pe", "act", "dve", "pool", "sp")


class Counter:
    def __init__(self, nc, name, step, epoch):
        self.nc, self.name, self.step, self.epoch = nc, name, step, epoch
        self.n = 0
        self.sems = []

    def next(self):
        self.n += 1
        e = (self.n - 1) // self.epoch
        while len(self.sems) <= e:
            self.sems.append(self.nc.alloc_semaphore(f"{self.name}_e{len(self.sems)}"))
        return self.n

    def sem_of(self, n):
        e = (n - 1) // self.epoch
        return self.sems[e], (n - e * self.epoch) * self.step


class Buf:
    __slots__ = ("name", "w", "r", "ctr")

    def __init__(self, name=""):
        self.name = name
        self.w = {}
        self.r = {}
        self.ctr = None


class Sched:
    def __init__(self, nc):
        self.nc = nc
        self.q = {e: [] for e in ENGS}
        self.eng_ctr = {e: Counter(nc, f"tk_{e}", 1, 12000) for e in ("pe", "act", "dve", "pool")}
        self.waited = {e: {} for e in ENGS}
        self.finals = {}
        self.n_dma_ctr = 0
        self.nbuf = 0
        self.scopes = []
        self.all_dma_ctrs = []

    def sb(self, name, shape, dt):
        assert not self.scopes, "persistent alloc inside scope: " + name
        return self.nc.alloc_sbuf_tensor("sb_" + name, list(shape), dt).ap()

    def ps(self, name, shape, dt=F32):
        return self.nc.alloc_psum_tensor("ps_" + name, list(shape), dt).ap()

    def buf(self, name=None):
        self.nbuf += 1
        return Buf(name or f"b{self.nbuf}")

    def _ctr_for(self, b):
        if b.ctr is None:
            self.n_dma_ctr += 1
            b.ctr = Counter(self.nc, f"dq{self.n_dma_ctr}", 16, 900)
            self.all_dma_ctrs.append(b.ctr)
        return b.ctr

    def _deps(self, eng, reads, writes):
        need = {}

        def add(c, n, src_eng):
            if src_eng == eng and eng == "pe":
                return
            if need.get(c, 0) < n:
                need[c] = n

        for b in reads:
            for c, (n, se) in b.w.items():
                add(c, n, se)
        for b in writes:
            for c, (n, se) in b.w.items():
                if se == eng and se != "dma":
                    continue
                add(c, n, se)
            for c, (n, se) in b.r.items():
                if se == eng and se != "dma":
                    continue
                add(c, n, se)
        waits = []
        wd = self.waited[eng]
        for c, n in need.items():
            if wd.get(c, 0) >= n:
                continue
            wd[c] = n
            waits.append(c.sem_of(n))
        return waits

    def _record(self, c, n, src_eng, reads, writes):
        for b in reads:
            old = b.r.get(c)
            if old is None or old[0] < n:
                b.r[c] = (n, src_eng)
        for b in writes:
            b.w = {c: (n, src_eng)}
            b.r = {}

    def op(self, eng, fn, reads=(), writes=()):
        waits = self._deps(eng, reads, writes)
        c = self.eng_ctr[eng]
        n = c.next()
        sem, _ = c.sem_of(n)
        self._record(c, n, eng, reads, writes)

        def emit(e, waits=waits, fn=fn, sem=sem):
            for s, v in waits:
                e.wait_ge(s, v)
            ins = fn(e)
            ins.then_inc(sem, 1)
        self.q[eng].append(emit)

    def dma(self, qeng, out, in_, reads=(), writes=(), slot=None, out_final=False, **kw):
        self.op_dma_ind(qeng, lambda e: e.dma_start(out=out, in_=in_, **kw), reads=reads, writes=writes,
                        slot=slot, out_final=out_final)

    def op_dma_ind(self, qeng, fn, reads=(), writes=(), slot=None, out_final=False):
        if slot is None:
            slot = writes[0] if len(writes) else reads[0]
        waits = self._deps(qeng, reads, writes)
        c = self._ctr_for(slot)
        n = c.next()
        sem, _ = c.sem_of(n)
        self._record(c, n, "dma", reads, writes)
        if out_final:
            self.finals[c] = n

        def emit(e, waits=waits, fn=fn, sem=sem):
            for s, v in waits:
                e.wait_ge(s, v)
            fn(e).then_inc(sem, 16)
        self.q[qeng].append(emit)

    def scope_push(self):
        from contextlib import ExitStack
        st = ExitStack()
        self.scopes.append(st)

    def sbs(self, name, shape, dt):
        return self.scopes[-1].enter_context(self.nc.sbuf_tensor("sb_" + name, list(shape), dt)).ap()

    def scope_pop(self):
        self.barrier()
        self.scopes.pop().close()

    def barrier(self):
        ctrs = list(self.eng_ctr.values()) + self.all_dma_ctrs
        for eng in ENGS:
            waits = []
            wd = self.waited[eng]
            for c in ctrs:
                if c.n > 0 and wd.get(c, 0) < c.n:
                    wd[c] = c.n
                    waits.append(c.sem_of(c.n))

            def emit(e, waits=waits):
                for s_, v in waits:
                    e.wait_ge(s_, v)
            self.q[eng].append(emit)

    def finish(self):
        nc = self.nc
        finals = [c.sem_of(n) for c, n in self.finals.items()]
        q = self.q
        with nc.Block() as block:
            @block.sync
            def _(e):
                for f in q["sp"]:
                    f(e)
                for s, v in finals:
                    e.wait_ge(s, v)

            @block.tensor
            def _(e):
                for f in q["pe"]:
                    f(e)

            @block.scalar
            def _(e):
                for f in q["act"]:
                    f(e)

            @block.vector
            def _(e):
                for f in q["dve"]:
                    f(e)

            @block.gpsimd
            def _(e):
                for f in q["pool"]:
                    f(e)


def _const_layout():
    lay = {}
    off = 0
    for name, n in (("ident", 128), ("maskf", 128), ("maskb", 128), ("mgt", 128), ("mlt", 128), ("ones", 128),
                    ("rsf", 512), ("rsb", 512), ("cind", 4), ("iota", 512), ("tokab", 64), ("bones", 128),
                    ("wcomb", 2), ("segpre", 128), ("tokidx", 32)):
        lay[name] = (off, n)
        off += n
    return lay, off


CL, NCONST = _const_layout()


def make_consts():
    c = np.zeros((128, NCONST), np.float32)

    def put(name, arr):
        o, n = CL[name]
        c[:arr.shape[0], o:o + n] = arr
    p = np.arange(128)
    put("ident", np.eye(128, dtype=np.float32))
    j = p[:, None]; i = p[None, :]
    same = (j // 32) == (i // 32)
    mf = (same & (j <= i)).astype(np.float32)
    mb = (same & (j >= i)).astype(np.float32)
    put("maskf", mf); put("maskb", mb)
    put("mgt", (j > i).astype(np.float32))
    put("mlt", (j < i).astype(np.float32))
    put("ones", np.ones((128, 128), np.float32))
    t = np.arange(512)
    put("rsf", np.broadcast_to((t % 32 != 0).astype(np.float32), (128, 512)))
    put("rsb", np.broadcast_to((t % 32 != 31).astype(np.float32), (128, 512)))
    put("cind", (p[:, None] // 32 == np.arange(4)[None, :]).astype(np.float32))
    put("iota", np.broadcast_to(t.astype(np.float32), (128, 512)))
    tok = (np.arange(32)[None, :] * 128 + p[:, None])
    ab = np.stack([tok // 64, tok % 64], axis=-1).reshape(128, 64).astype(np.float32)
    put("tokab", ab)
    put("bones", ((p[:, None] // 8) == (p[None, :] // 8)).astype(np.float32))
    wc = np.zeros((128, 2), np.float32)
    wc[0, 0] = 64.0; wc[1, 0] = 1.0; wc[2, 1] = 1.0; wc[3, 1] = 1.0; wc[4, 1] = 1.0
    put("wcomb", wc)
    put("segpre", (((p[:, None] // 8) == (p[None, :] // 8)) & ((p[:, None] % 8) < (p[None, :] % 8))).astype(np.float32))
    cs = np.arange(32)
    put("tokidx", ((cs[None, :] % 8) * 512 + (cs[None, :] // 8) * 128 + p[:, None]).astype(np.float32))
    return c


T_SEQ, D_MODEL, N_EXP, CAP, DFF = 4096, 1024, 16, 512, 2048
HAS_MOE = True
EPS = 1e-6


def build_program(stage=99, dbg=False):
    nc = bass.Bass("TRN2", target_bir_lowering=False)
    nc.dge_precook = False
    S = Sched(nc)

    def din(name, shape, dt=F32):
        return nc.dram_tensor(name, list(shape), dt, kind="ExternalInput").ap()

    def dscr(name, shape, dt=F32, out=False):
        if out:
            return nc.dram_tensor(name, list(shape), dt, kind="ExternalOutput").ap()
        return nc.dram_tensor(name, list(shape), dt).ap()

    x_d = din("x", [T_SEQ, D_MODEL]); ctx_d = din("ctx", [256, D_MODEL]); cc_d = din("ccT", [128, 16])
    wada_d = din("w_ada", [1024, 6144], F32R); bada_d = din("b_ada", [1, 6144])
    nmix_d = din("nmix_fm", [128, 8]); nffn_d = din("nffn_bc", [128, 1024]); nfin_d = din("nfin_bc", [128, 1024])
    win_d = din("w_in", [1024, 4096], F32R)
    lblfm_d = din("lbl_fm", [128, 16]); lblbc_d = din("lbl_bc", [128, 2048])
    hgn_d = din("hgn_fm", [128, 1]); convw_d = din("convw_fm", [128, 12])
    wout_d = din("w_out", [1024, 1024], F32R); wr_d = din("w_r_fm", [128, 128])
    if stage >= 4 and HAS_MOE:
        wg_d = din("w_gate", [N_EXP, 1024, DFF], F32R); wu_d = din("w_up", [N_EXP, 1024, DFF], F32R)
        wd_d = din("w_down", [N_EXP, DFF, 1024], F32R)
    consts_d = din("consts", [128, NCONST])
    out_d = nc.dram_tensor("out", [T_SEQ, D_MODEL], F32, kind="ExternalOutput").ap()
    x2_d = dscr("x2", [T_SEQ, D_MODEL], F32, out=dbg)
    hx2_d = dscr("hx2", [T_SEQ, D_MODEL], F32, out=dbg)
    st_qd = dscr("st_qd", [8, 128, 4, 512], BF16); st_ki = dscr("st_ki", [8, 128, 4, 512], BF16)
    st_ke = dscr("st_ke", [8, 128, 4, 4, 128], BF16); st_v = dscr("st_v", [8, 128, 4, 512], BF16)
    st_el = dscr("st_el", [8, 128, 4, 16], F32); st_of = dscr("st_of", [8, 128, 4, 512], F32)
    st_sg = dscr("st_sg", [8, 128, 4, 512], BF16); st_yb = dscr("st_yb", [8, 128, 4, 512], F32R)
    dbg_d = {}
    if dbg:
        dbg_d["s0"] = dscr("dbg_s0", [8, 128, 128], F32, out=True)
        dbg_d["aff"] = dscr("dbg_aff", [16, T_SEQ], F32, out=True)
        dbg_d["of"] = dscr("dbg_of", [8, 128, 4, 512], F32, out=True)
        dbg_d["mod"] = dscr("dbg_mod", [2, 6144], F32, out=True)

    def act(out, in_, func, rd, wr, **kw):
        S.op("act", lambda e: e.activation(out, in_, func, **kw), reads=rd, writes=wr)

    def tt(eng, out, a, b, op, rd, wr):
        S.op(eng, lambda e: e.tensor_tensor(out, a, b, op), reads=rd, writes=wr)

    def ts(eng, out, a, s1, s2, op0, op1, rd, wr):
        if s2 is None:
            S.op(eng, lambda e: e.tensor_scalar(out, a, s1, None, op0), reads=rd, writes=wr)
        else:
            S.op(eng, lambda e: e.tensor_scalar(out, a, s1, s2, op0, op1), reads=rd, writes=wr)

    def stt(out, a, s, b, op0, op1, rd, wr):
        S.op("dve", lambda e: e.scalar_tensor_tensor(out, a, s, b, op0, op1), reads=rd, writes=wr)

    def cp(eng, out, in_, rd, wr):
        if eng == "act":
            S.op("act", lambda e: e.activation(out, in_, AF.Copy), reads=rd, writes=wr)
        else:
            S.op(eng, lambda e: e.tensor_copy(out, in_), reads=rd, writes=wr)

    def mm(lst, rd, wr):
        def f(e, lst=lst):
            ins = None
            for (o, l, r, st, sp) in lst:
                ins = e.matmul(o, lhsT=l, rhs=r, start=st, stop=sp)
            return ins
        S.op("pe", f, reads=rd, writes=wr)

    def trs(lst, rd, wr):
        def f(e, lst=lst):
            ins = None
            for (o, i, idn) in lst:
                ins = e.transpose(o, i, idn)
            return ins
        S.op("pe", f, reads=rd, writes=wr)

    ipb = [(S.ps(f"ip{i}", [128, 512], F32), S.buf(f"ip{i}")) for i in range(4)]
    ipn = [0]

    def bank():
        r = ipb[ipn[0] % 4]
        ipn[0] += 1
        return r
    sc_ps, Bsc = S.ps("sc", [128, 512], F32), S.buf("sc")
    kv_ps, Bkv = S.ps("kv", [128, 512], F32), S.buf("kv")
    o_ps, Bo = S.ps("o", [128, 512], F32), S.buf("o")
    trb_ps, Btrb = S.ps("trb", [128, 512], F32), S.buf("trb")

    consts = S.sb("consts", [128, NCONST], F32); Bc = S.buf("consts")
    S.dma("sp", consts, consts_d, writes=[Bc])

    def C(name, rows=128, lo=0, hi=None):
        o, n = CL[name]
        return consts[0:rows, o + lo: o + (n if hi is None else hi)]
    ident = C("ident")
    identb = None; Bib = S.buf()
    onesr = S.sb("onesr", [128, 128], F32R); Bor = S.buf()
    cp("dve", onesr, C("ones"), [Bc], [Bor])
    mhalf = S.sb("mhalf", [128, 1], F32); Bmh = S.buf()
    S.op("pool", lambda e: e.memset(mhalf, -0.5), writes=[Bmh])

    def small_in(name, src, shape):
        t = S.sb(name, shape, F32); b = S.buf(name)
        S.dma("sp", t, src, writes=[b])
        return t, b
    ccT, Bcc = small_in("ccT", cc_d, [128, 16])
    nmix, Bnm = small_in("nmix", nmix_d, [128, 8])
    lblfm, Blf = small_in("lblfm", lblfm_d, [128, 16])
    hgn, Bhg = small_in("hgn", hgn_d, [128, 1])
    convw, Bcw = small_in("convw", convw_d, [128, 12])
    wr, Bwr = small_in("wr", wr_d, [128, 128])
    nfin, Bnf = small_in("nfin", nfin_d, [128, 1024])
    lbfm = S.sb("lbfm", [128, 8], F32); omlfm = S.sb("omlfm", [128, 8], F32); nomlfm = S.sb("nomlfm", [128, 8], F32)
    tmp8 = S.sb("tmp8", [128, 8], F32); Bl = S.buf(); Bt8 = S.buf()
    tt("dve", tmp8, lblfm[:, 0:8], lblfm[:, 8:16], ALU.subtract, [Blf], [Bt8])
    act(lbfm, tmp8, AF.Sigmoid, [Bt8], [Bl])
    ts("dve", omlfm, lbfm, -1.0, 1.0, ALU.mult, ALU.add, [Bl], [Bl])
    ts("dve", nomlfm, lbfm, 1.0, -1.0, ALU.mult, ALU.add, [Bl], [Bl])

    S32 = S.sb("S32", [128, 2, 8, 128], F32R)
    S16 = None
    BS32 = [S.buf(f"S32_{i}") for i in range(8)]
    BS32b = [[BS32[i] for i in range(8)], [S.buf(f"S32b_{i}") for i in range(8)]]
    stver = [0, 0]
    BS16 = [[S.buf(f"S16_{v}_{i}") for i in range(8)] for v in range(2)]
    sver = [0] * 8

    A1 = S.sb("A1", [128, 8], F32); B1 = S.sb("B1", [128, 8], F32)
    Ac = S.sb("Acx", [128, 8], F32); Bcx = S.sb("Bcx", [128, 8], F32); Bab = S.buf("A1B1")
    g1bc = S.sb("g1bc", [128, 1024], F32); A2bc = S.sb("A2bc", [128, 1024], F32)
    B2bc = S.sb("B2bc", [128, 1024], F32); g2bc = S.sb("g2bc", [128, 1024], F32); Bbc = S.buf("bcs")

    sc = S.sb("silu_c", [128, 16], F32R); Bscx = S.buf()
    modT = S.sb("modT", [128, 32], F32); BmT = S.buf()
    ss_t = S.sb("ss_t", [128, 4], F32); Bss = [S.buf() for _ in range(4)]
    S.scope_push()
    wsl = [(S.sbs(f"wsl{i}", [128, 8, 512], F32R), S.buf(f"wsl{i}")) for i in range(2)]
    wsn = [0]

    def wslot():
        r = wsl[wsn[0] % 2]
        wsn[0] += 1
        return r
    junk = S.sbs("junk", [128, 1024], F32); Bjunk = S.buf()
    xsl = [(S.sbs(f"xsl{i}", [128, 1024], F32), S.buf(f"xsl{i}")) for i in range(2)]
    xn = S.sbs("xn", [128, 1024], F32); Bxn = S.buf()
    hxT = S.sbs("hxT", [128, 8, 512], F32R); BhxT = S.buf("hxT")
    S.scope_push()
    wada_v = wada_d.rearrange("(k p) n -> p k n", p=128)
    win_v = win_d.rearrange("(k p) n -> p k n", p=128)

    act(sc, ccT, AF.Silu, [Bcc], [Bscx])
    sc3 = sc.rearrange("p (k j) -> p k j", j=2)
    nffn = S.sbs("nffn", [128, 1024], F32); Bnff = S.buf()
    S.dma("sp", nffn, nffn_d, writes=[Bnff])
    mrow = [(S.sbs(f"mrow{i}", [2, 512], F32), S.sbs(f"brow{i}", [2, 512], F32), S.buf(), S.buf()) for i in range(2)]
    psT, BpT = trb_ps, Btrb
    bcdst = {4: (g1bc, 0, 0), 5: (g1bc, 1, 0), 6: (B2bc, 0, 0), 7: (B2bc, 1, 0), 8: (A2bc, 0, 1), 9: (A2bc, 1, 1),
             10: (g2bc, 0, 0), 11: (g2bc, 1, 0)}
    for j in range(12):
        wt, Bw = wslot()
        S.dma("sp", wt, wada_v[:, :, j * 512:(j + 1) * 512], writes=[Bw])
        mr, br, Bmr, Bbr = mrow[j % 2]
        S.dma("sp", br[0:1, :], bada_d[:, j * 512:(j + 1) * 512], writes=[Bbr])
        S.dma("sp", br[1:2, :], bada_d[:, j * 512:(j + 1) * 512], writes=[Bbr])
        ps, Bp = bank()
        mm([(ps[0:2, :], sc3[:, k, :], wt[:, k, :], k == 0, k == 7) for k in range(8)], [Bscx, Bw], [Bp])
        tt("dve", mr, ps[0:2, :], br, ALU.add, [Bp, Bbr], [Bmr])
        if dbg:
            S.dma("act", dbg_d["mod"][:, j * 512:(j + 1) * 512], mr, reads=[Bmr], slot=Bmr, out_final=True)
        if j < 4:
            trs([(psT[:, (j * 4 + c) * 2:(j * 4 + c + 1) * 2], mr[0:2, c * 128:(c + 1) * 128], ident[0:2, 0:2])
                 for c in range(4)], [Bmr, Bc], [BpT])
        else:
            dst, hf, kind = bcdst[j]
            ps2, Bp2 = bank()
            mm([(ps2, C("ones", rows=1), mr[0:1, :], True, True)], [Bmr, Bc], [Bp2])
            if kind == 0:
                cp("act", dst[:, hf * 512:(hf + 1) * 512], ps2, [Bp2], [Bbc])
            else:
                stt(dst[:, hf * 512:(hf + 1) * 512], ps2, 1.0, nffn[:, hf * 512:(hf + 1) * 512], ALU.add, ALU.mult,
                    [Bp2, Bnff], [Bbc])
        if j == 3:
            cp("dve", modT, psT[:, 0:32], [BpT], [BmT])
            modT3 = modT.rearrange("p (c j) -> p c j", j=2)
            stt(A1, modT3[:, 8:16, 0], 1.0, nmix, ALU.add, ALU.mult, [BmT, Bnm], [Bab])
            cp("dve", B1, modT3[:, 0:8, 0], [BmT], [Bab])
            stt(Ac, modT3[:, 8:16, 1], 1.0, nmix, ALU.add, ALU.mult, [BmT, Bnm], [Bab])
            cp("dve", Bcx, modT3[:, 0:8, 1], [BmT], [Bab])

    if stage == 0:
        S.dma("act", out_d[0:128, :], nfin, reads=[Bnf], slot=Bnf, out_final=True)
        S.finish()
        return nc
    ssn = [0]

    def rstd_of(xt, Bx):
        i = ssn[0] % 4
        ssn[0] += 1
        col = ss_t[:, i:i + 1]
        act(junk, xt, AF.Square, [Bx], [Bjunk])
        S.op("dve", lambda e: e.reduce_sum(col, junk, AX.X), reads=[Bjunk], writes=[Bss[i]])
        ts("dve", col, col, 1.0 / D_MODEL, EPS, ALU.mult, ALU.add, [Bss[i]], [Bss[i]])
        act(col, col, AF.Sqrt, [Bss[i]], [Bss[i]])
        S.op("dve", lambda e: e.reciprocal(col, col), reads=[Bss[i]], writes=[Bss[i]])
        return col, Bss[i]

    def prep_tile(src_rows, An, Bn, col0, xi):
        xt, Bx = xsl[xi % 2]
        S.dma("sp", xt, src_rows, writes=[Bx])
        rs, Brs = rstd_of(xt, Bx)
        act(xn, xt, AF.Copy, [Bx, Brs], [Bxn], scale=rs)
        for half in range(2):
            ps, Bp = bank()
            trs([(ps[:, q * 128:(q + 1) * 128], xn[:, (half * 4 + q) * 128:(half * 4 + q + 1) * 128], ident)
                 for q in range(4)], [Bxn, Bc], [Bp])
            for q in range(4):
                k = half * 4 + q
                act(hxT[:, k, col0:col0 + 128], ps[:, q * 128:(q + 1) * 128], AF.Identity, [Bp, Bab], [BhxT],
                    scale=An[:, k:k + 1], bias=Bn[:, k:k + 1])

    lblbc = S.sbs("lblbc", [128, 2048], F32); Blb = S.buf()
    S.dma("sp", lblbc, lblbc_d, writes=[Blb])
    lbbc = S.sbs("lbbc", [128, 1024], F32); omlbc = S.sbs("omlbc", [128, 1024], F32); Blbb = S.buf()
    tt("dve", omlbc, lblbc[:, 0:1024], lblbc[:, 1024:2048], ALU.subtract, [Blb], [Blbb])
    act(lbbc, omlbc, AF.Sigmoid, [Blbb], [Blbb])
    ts("dve", omlbc, lbbc, -1.0, 1.0, ALU.mult, ALU.add, [Blbb], [Blbb])
    clogf = S.sbs("clogf", [128, 2, 2, 512], F32R); ck = S.sbs("ck", [128, 2, 2, 512], F32)
    cv16 = S.sbs("cv16", [128, 2, 512], F32R); ckd16 = S.sbs("ckd16", [128, 2, 2, 512], F32R)
    Bclf = [[S.buf() for _ in range(2)] for _ in range(2)]; Bck = [[S.buf() for _ in range(2)] for _ in range(2)]
    Bcv = [S.buf() for _ in range(2)]; Bckd = [[S.buf() for _ in range(2)] for _ in range(2)]
    csig = S.sbs("csig", [128, 512], F32); Bcs = S.buf()
    cf = S.sbs("cf", [128, 512], F32); Bcf = S.buf()
    import os
    KCUT = int(os.environ.get("KCUT", "0"))
    for i in range(2):
        prep_tile(ctx_d[i * 128:(i + 1) * 128, :], Ac, Bcx, i * 128, i)
    if KCUT == 1:
        S.dma("act", out_d[0:128, :], nfin, reads=[Bnf], slot=Bnf, out_final=True)
        S.scope_pop(); S.scope_pop()
        S.finish()
        return nc
    for j in (1, 2, 3):
        wt, Bw = wslot()
        S.dma("sp", wt, win_v[:, :, j * 512:(j + 1) * 512], writes=[Bw])
        for i in range(2):
            ps, Bp = bank()
            mm([(ps, hxT[:, k, i * 128:(i + 1) * 128], wt[:, k, :], k == 0, k == 7) for k in range(8)],
               [BhxT, Bw], [Bp])
            if j == 3:
                cp("act", cv16[:, i, :], ps, [Bp], [Bcv[i]])
            else:
                d = j - 1
                act(csig, ps, AF.Sigmoid, [Bp], [Bcs])
                tt("dve", cf, csig, omlbc[:, d * 512:(d + 1) * 512], ALU.mult, [Bcs, Blbb], [Bcf])
                tt("dve", cf, cf, lbbc[:, d * 512:(d + 1) * 512], ALU.add, [Bcf, Blbb], [Bcf])
                act(clogf[:, d, i, :], cf, AF.Ln, [Bcf], [Bclf[d][i]])
                ts("dve", ck[:, d, i, :], cf, -1.0, 1.0, ALU.mult, ALU.add, [Bcf], [Bck[d][i]])
    if KCUT == 2:
        S.dma("act", out_d[0:128, :], nfin, reads=[Bnf], slot=Bnf, out_final=True)
        S.scope_pop(); S.scope_pop()
        S.finish()
        return nc
    ones_f = onesr
    trir = S.sbs("trir", [128, 256], F32R); Btri = S.buf()
    cp("dve", trir[:, 0:128], C("mgt"), [Bc], [Btri])
    cp("dve", trir[:, 128:256], C("mlt"), [Bc], [Btri])
    for d in range(2):
        for i in range(2):
            ps, Bp = bank()
            tri = trir[:, 0:128] if d == 0 else trir[:, 128:256]
            other = 1 - i
            lst = [(ps, tri, clogf[:, d, i, :], True, False)]
            if (d == 0 and i == 0) or (d == 1 and i == 1):
                lst.append((ps, ones_f, clogf[:, d, other, :], False, True))
                rd = [Bclf[d][0], Bclf[d][1], Btri, Bor]
            else:
                lst[0] = (ps, tri, clogf[:, d, i, :], True, True)
                rd = [Bclf[d][i], Btri]
            mm(lst, rd, [Bp])
            act(csig, ps, AF.Exp, [Bp], [Bcs])
            tt("dve", ckd16[:, d, i, :], ck[:, d, i, :], csig, ALU.mult, [Bck[d][i], Bcs], [Bckd[d][i]])
    if KCUT == 3:
        S.dma("act", out_d[0:128, :], nfin, reads=[Bnf], slot=Bnf, out_final=True)
        S.scope_pop(); S.scope_pop()
        S.finish()
        return nc
    for d in range(2):
        for h in range(4):
            ps, Bp = bank()
            hs = slice(h * 128, (h + 1) * 128)
            mm([(ps[:, 0:128], ckd16[:, d, i, hs], cv16[:, i, hs], i == 0, i == 1) for i in range(2)],
               [Bckd[d][0], Bckd[d][1], Bcv[0], Bcv[1]], [Bp])
            cp("dve", S32[:, 0, d * 4 + h, :], ps[:, 0:128], [Bp], [BS32[d * 4 + h]])
    if dbg and stage == 1:
        for nm, tns, shp, bufs in (("clogf", clogf, [128, 2048], [x for y in Bclf for x in y]), ("ck", ck, [128, 2048], [x for y in Bck for x in y]),
                                   ("ckd", ckd16, [128, 2048], [x for y in Bckd for x in y]), ("cv", cv16, [128, 1024], Bcv),
                                   )[:int(os.environ.get("NDBG", "4"))]:
            dd_ = dscr("dbg_" + nm, shp, F32, out=True)
            flat = tns.bitcast(F32) if nm != "ck" else tns
            if flat.ndim == 4:
                flat = flat.rearrange("p a b c -> p (a b c)")
            elif flat.ndim == 3:
                flat = flat.rearrange("p a b -> p (a b)")
            S.dma("sp", dd_, flat, reads=bufs, slot=S.buf(), out_final=True)
    if dbg and stage == 1:
        for hh in range(2):
            dd_ = dscr(f"dbg_hcT{hh}", [128, 2048], F32, out=True)
            S.dma("sp", dd_, hxT.bitcast(F32)[:, hh * 4:(hh + 1) * 4, :].rearrange("p a b -> p (a b)"), reads=[BhxT], slot=S.buf(), out_final=True)
        dd_ = dscr("dbg_xn", [128, 1024], F32, out=True)
        S.dma("sp", dd_, xn, reads=[Bxn], slot=S.buf(), out_final=True)
        dd_ = dscr("dbg_ss", [128, 4], F32, out=True)
        S.dma("sp", dd_, ss_t, reads=Bss, slot=S.buf(), out_final=True)
    if KCUT == 4:
        S.dma("act", out_d[0:128, :], nfin, reads=[Bnf], slot=Bnf, out_final=True)
        S.scope_pop(); S.scope_pop()
        S.finish()
        return nc
    if dbg:
        S.dma("act", dbg_d["s0"].rearrange("s p v -> p s v"), S32.bitcast(F32)[:, 0, :, :], reads=BS32, slot=BS32[0], out_final=True)
    S.scope_pop()
    if stage == 1:
        S.dma("act", out_d[0:128, :], nfin, reads=[Bnf], slot=Bnf, out_final=True)
        S.finish()
        return nc
    return _build_rest(nc, S, locals(), stage, dbg)


def _host_layouts(inputs):
    f = lambda a: np.ascontiguousarray(np.asarray(a, dtype=np.float32))
    fm = lambda v: f(np.asarray(v).reshape(-1, 128).T)
    sh = {}
    sh["w_ada"] = f(inputs["w_ada"][0]); sh["b_ada"] = f(inputs["b_ada"][0]).reshape(1, 6144)
    sh["nmix_fm"] = fm(inputs["norm_mix"][0])
    sh["nffn_bc"] = f(np.broadcast_to(np.asarray(inputs["norm_ffn"][0])[None, :], (128, 1024)))
    sh["nfin_bc"] = f(np.broadcast_to(np.asarray(inputs["norm_final"])[None, :], (128, 1024)))
    sh["w_in"] = f(inputs["w_in"][0])
    lbl = np.asarray(inputs["lb_logits"], dtype=np.float32)
    sh["lbl_fm"] = f(np.concatenate([fm(lbl[0].reshape(-1)), fm(lbl[1].reshape(-1))], axis=1))
    sh["lbl_bc"] = f(np.broadcast_to(lbl.reshape(1, 2048), (128, 2048)))
    sh["hgn_fm"] = f(np.asarray(inputs["hg_norm"][0]).reshape(128, 1))
    cw = np.asarray(inputs["conv_w"][0], dtype=np.float32)
    sh["convw_fm"] = f(cw.reshape(3, 4, 128).transpose(2, 1, 0).reshape(128, 12))
    sh["w_out"] = f(inputs["w_out"][0])
    wr = np.asarray(inputs["w_router"][0], dtype=np.float32)
    sh["w_r_fm"] = f(wr.reshape(8, 128, 16).transpose(1, 0, 2).reshape(128, 128))
    sh["w_gate"] = f(inputs["w_gate"][0]); sh["w_up"] = f(inputs["w_up"][0]); sh["w_down"] = f(inputs["w_down"][0])
    sh["consts"] = make_consts()
    return sh


def _core_inputs(inputs, sh, b):
    f = lambda a: np.ascontiguousarray(np.asarray(a, dtype=np.float32))
    m = dict(sh)
    m["x"] = f(inputs["x"][b]); m["ctx"] = f(inputs["ctx"][b])
    cc = np.stack([np.asarray(inputs["c"][b]).reshape(8, 128).T, np.asarray(inputs["c_ctx"]).reshape(8, 128).T],
                  axis=-1)
    m["ccT"] = f(cc.reshape(128, 16))
    return m


_PROG = {}


def kernel(**inputs):
    if "full" not in _PROG:
        _PROG["full"] = build_program()
    nc = _PROG["full"]
    sh = _host_layouts(inputs)
    in_maps = [_core_inputs(inputs, sh, b) for b in range(8)]
    if not HAS_MOE:
        for m in in_maps:
            for k in ("w_gate", "w_up", "w_down"):
                m.pop(k, None)
    res = run_bass_kernel_spmd(nc, in_maps, core_ids=list(range(8)))
    return np.stack([np.asarray(r["out"], dtype=np.float32) for r in res.results], axis=0)


def _build_rest(nc, S, L, stage, dbg):
    g = lambda n: L[n]
    (act, tt, ts, stt, cp, mm, trs, bank, C, wslot, prep_tile, rstd_of) = [g(n) for n in (
        "act", "tt", "ts", "stt", "cp", "mm", "trs", "bank", "C", "wslot", "prep_tile", "rstd_of")]
    (x_d, out_d, x2_d, hx2_d, win_v, wout_d, dbg_d, st_of) = [g(n) for n in (
        "x_d", "out_d", "x2_d", "hx2_d", "win_v", "wout_d", "dbg_d", "st_of")]
    (ident, identb, onesr, Bc, Bib, Bor, lbfm, omlfm, nomlfm, Bl, S32, S16, BS32, BS16, sver, A1, B1, Bab,
     g1bc, A2bc, B2bc, g2bc, Bbc, hxT, BhxT, xsl, xn, Bxn, junk, Bjunk, hgn, Bhg, convw, Bcw, wr, Bwr, nfin, Bnf,
     sc_ps, Bsc, kv_ps, Bkv, o_ps, Bo, trb_ps, Btrb) = [g(n) for n in (
         "ident", "identb", "onesr", "Bc", "Bib", "Bor", "lbfm", "omlfm", "nomlfm", "Bl", "S32", "S16", "BS32",
         "BS16", "sver", "A1", "B1", "Bab", "g1bc", "A2bc", "B2bc", "g2bc", "Bbc", "hxT", "BhxT", "xsl", "xn",
         "Bxn", "junk", "Bjunk", "hgn", "Bhg", "convw", "Bcw", "wr", "Bwr", "nfin", "Bnf",
         "sc_ps", "Bsc", "kv_ps", "Bkv", "o_ps", "Bo", "trb_ps", "Btrb")]
    aff_d = nc.dram_tensor("aff_scr", [16, T_SEQ], F32, kind=("ExternalOutput" if dbg else "Internal")).ap()
    wout_v = wout_d.rearrange("(k p) n -> p k n", p=128)
    xcnt = [0]

    S.scope_push()
    Ta = S.sbs("Ta", [128, 512], F32); Tb = S.sbs("Tb", [128, 512], F32)
    Tc = S.sbs("Tc", [128, 512], F32); Td = S.sbs("Td", [128, 512], F32)
    BTa, BTb, BTc, BTd = S.buf(), S.buf(), S.buf(), S.buf()
    kend16 = S.sbs("kend16", [128, 512], F32); Bke16 = S.buf()
    Eall = S.sbs("Eall", [128, 4, 512], F32); BE = [S.buf() for _ in range(4)]
    QD = S.sbs("QD", [128, 4, 512], F32R); BQD = [S.buf() for _ in range(4)]
    KI = S.sbs("KI", [128, 4, 512], F32R); BKI = [S.buf() for _ in range(4)]
    KE = S.sbs("KE", [128, 4, 4, 128], F32R); BKE = [S.buf() for _ in range(4)]
    EL = S.sbs("EL", [128, 4, 16], F32); BEL = [S.buf() for _ in range(4)]
    V16 = S.sbs("V16", [128, 4, 512], F32R); BV = [S.buf() for _ in range(4)]
    VMs = [(S.sbs(f"VM{i}", [128, 512], F32R), S.buf(f"VM{i}")) for i in range(2)]
    vmn = [0]
    AT16 = S.sbs("AT16", [128, 512], F32R); BAT = S.buf()
    OB = S.sbs("OB", [128, 4, 512], F32); BOB = [S.buf() for _ in range(4)]
    SG = S.sbs("SG", [128, 4, 512], BF16); BSG = [S.buf() for _ in range(4)]
    cvs = S.sbs("cvs", [128, 4, 512], F32); Bcvs = [S.buf() for _ in range(4)]
    MIXT = S.sbs("MIXT", [128, 8, 512], F32R); BMX = [S.buf() for _ in range(8)]
    MIXTf = MIXT.bitcast(F32)
    hx2T = S.sbs("hx2T", [128, 8, 128], F32); Bh2T = S.buf()
    osq = S.sbs("osq", [128, 512], F32R); Bosq = S.buf()
    affsb = S.sbs("affsb", [16, 128], F32); Baf = S.buf()
    eT = S.sbs("eT", [16, 128], F32); BeT = S.buf()
    Bstash = [S.buf(f"stash{i}") for i in range(8)]

    def gate_prep(d, h, ps, Bp):
        col = d * 4 + h
        act(Ta, ps, AF.Sigmoid, [Bp], [BTa])
        act(Tb, Ta, AF.Ln, [BTa, Bl], [BTb], scale=omlfm[:, col:col + 1], bias=lbfm[:, col:col + 1])
        ts("dve", Tc, Ta, nomlfm[:, col:col + 1], omlfm[:, col:col + 1], ALU.mult, ALU.add, [BTa, Bl], [BTc])
        if d == 0:
            S.op("dve", lambda e: e.tensor_tensor_scan(Td, C("rsf"), Tb, 0.0, ALU.mult, ALU.add),
                 reads=[BTb, Bc], writes=[BTd])
        else:
            S.op("dve", lambda e: e.tensor_tensor_scan(Td[:, ::-1], C("rsb")[:, ::-1], Tb[:, ::-1], 0.0,
                                                       ALU.mult, ALU.add), reads=[BTb, Bc], writes=[BTd])
        act(Eall[:, h, :], Td, AF.Exp, [BTd], [BE[h]])
        act(Ta, Td, AF.Exp, [BTd], [BTa], scale=-1.0)
        tt("dve", Tc, Tc, Ta, ALU.mult, [BTc, BTa], [BTc])
        cp("act", KI[:, h, :], Tc, [BTc], [BKI[h]])
        E3 = Eall[:, h, :].rearrange("p (c k) -> p c k", k=32)
        cp("pool", EL[:, h, :], E3[:, :, 31] if d == 0 else E3[:, :, 0], [BE[h]], [BEL[h]])
        elb = EL[:, h, :].rearrange("p (c o) -> p c o", o=1).to_broadcast([128, 16, 32])
        tt("dve", kend16.rearrange("p (c k) -> p c k", k=32), Tc.rearrange("p (c k) -> p c k", k=32), elb,
           ALU.mult, [BTc, BEL[h]], [Bke16])
        trs([(trb_ps[:, t * 128:(t + 1) * 128], kend16[:, t * 128:(t + 1) * 128], ident) for t in range(4)],
            [Bke16, Bc], [Btrb])
        cp("act", KE[:, h, :, :].rearrange("p t d -> p (t d)"), trb_ps[:, 0:512], [Btrb], [BKE[h]])

    kvb = [(kv_ps, Bkv), (trb_ps, Btrb)]
    BS32b = L["BS32b"]; stver = L["stver"]
    S32f = S32.bitcast(F32)
    V16f = V16.bitcast(F32)

    def gla_group(d):
        order = list(range(4)) if d == 0 else [3, 2, 1, 0]
        mk = (C("maskf") if d == 0 else C("maskb")).rearrange("p (o q) -> p o q", o=1).to_broadcast([128, 4, 128])
        hsl = [slice(h * 128, (h + 1) * 128) for h in range(4)]
        for t in order:
            tcol = slice(t * 128, (t + 1) * 128)
            mm([(sc_ps[:, hsl[h]], KI[:, h, tcol], QD[:, h, tcol], True, True) for h in range(4)], BKI + BQD, [Bsc])
            tt("dve", AT16.rearrange("p (t q) -> p t q", q=128), sc_ps.rearrange("p (t q) -> p t q", q=128), mk, ALU.mult,
               [Bsc, Bc], [BAT])
            mm([(o_ps[:, hsl[h]], V16[:, t, hsl[h]], AT16[:, hsl[h]], h == 0, False) for h in range(4)], [BV[t], BAT], [Bo])
            for ci, cc in enumerate(order):
                VMb, BVMb = VMs[vmn[0] % 2]
                kvp, Bkvp = kvb[vmn[0] % 2]
                vmn[0] += 1
                act(VMb, V16f[:, t, :], AF.Copy, [BV[t], Bc], [BVMb], scale=C("cind")[:, cc:cc + 1])
                mm([(kvp[:, hsl[h]], KE[:, h, t, :], VMb[:, hsl[h]], True, True) for h in range(4)], BKE + [BVMb], [Bkvp])
                v = stver[d]
                Bcur = [BS32b[v][d * 4 + h] for h in range(4)]
                Bnxt = [BS32b[1 - v][d * 4 + h] for h in range(4)]
                mm([(o_ps[:, h * 128 + cc * 32: h * 128 + cc * 32 + 32], S32[:, v, d * 4 + h, :],
                     QD[:, h, t * 128 + cc * 32: t * 128 + cc * 32 + 32], False, ci == 3) for h in range(4)],
                   Bcur + BQD, [Bo])
                chunk = t * 4 + cc

                def upd(e, kvp=kvp, chunk=chunk, v=v):
                    ins = None
                    for h in range(4):
                        col = d * 4 + h
                        ins = e.scalar_tensor_tensor(S32[:, 1 - v, col, :], S32f[:, v, col, :], EL[:, h, chunk:chunk + 1],
                                                     kvp[:, hsl[h]], ALU.mult, ALU.add)
                    return ins
                S.op("dve", upd, reads=Bcur + BEL + [Bkvp], writes=Bnxt)
                stver[d] = 1 - v
            o3 = o_ps.rearrange("p (h q) -> p h q", q=128)
            if d == 1:
                cp("act", OB[:, :, tcol], o3, [Bo], BOB)
            else:
                tt("dve", OB[:, :, tcol], o3, OB[:, :, tcol], ALU.add, [Bo] + BOB, BOB)

    def sweep(d):
        groups = range(8) if d == 0 else range(7, -1, -1)
        pieces = [1, 0, 3, 4, 7, 6, 5] if d == 0 else [2, 0, 3]
        for gi in groups:
            for t in range(4):
                r0 = (gi * 4 + t) * 128
                prep_tile(x_d[r0:r0 + 128, :], A1, B1, t * 128, xcnt[0])
                xcnt[0] += 1
            if d == 0:
                S.dma("sp", OB, st_of[gi], reads=[Bstash[gi]], writes=BOB, slot=BOB[0])
            for j in pieces:
                wt, Bw = wslot()
                S.dma("sp", wt, win_v[:, :, j * 512:(j + 1) * 512], writes=[Bw])
                for q in range(4):
                    ps, Bp = bank()
                    if j == 3:
                        mm([(ps, hxT[:, k, q * 128:(q + 1) * 128], wt[:, k, :], k == 0, k == 7) for k in range(8)],
                           [BhxT, Bw], [Bp])
                        cp("act", V16[:, q, :], ps, [Bp], [BV[q]])
                        continue
                    mm([(ps, wt[:, k, q * 128:(q + 1) * 128], hxT[:, k, :], k == 0, k == 7) for k in range(8)],
                       [BhxT, Bw], [Bp])
                    if j in (1, 2):
                        gate_prep(d, q, ps, Bp)
                    elif j == 0:
                        tt("dve", QD[:, q, :], ps, Eall[:, q, :], ALU.mult, [Bp, BE[q]], [BQD[q]])
                    elif j == 4:
                        act(SG[:, q, :], ps, AF.Silu, [Bp], [BSG[q]])
                    elif j == 7:
                        cp("act", cvs[:, q, :], ps, [Bp], [Bcvs[q]])
                    elif j == 6:
                        u = cvs[:, q, :]
                        y = Eall[:, q, :]
                        tt("dve", u, ps, u, ALU.mult, [Bp, Bcvs[q]], [Bcvs[q]])
                        ts("dve", y, u, convw[:, q * 3 + 1:q * 3 + 2], None, ALU.mult, None, [Bcvs[q], Bcw], [BE[q]])
                        u3 = u.rearrange("p (r w) -> p r w", w=64); y3 = y.rearrange("p (r w) -> p r w", w=64)
                        stt(y3[:, :, 1:64], u3[:, :, 0:63], convw[:, q * 3:q * 3 + 1], y3[:, :, 1:64], ALU.mult, ALU.add,
                            [Bcvs[q], Bcw, BE[q]], [BE[q]])
                        stt(y3[:, :, 0:63], u3[:, :, 1:64], convw[:, q * 3 + 2:q * 3 + 3], y3[:, :, 0:63], ALU.mult,
                            ALU.add, [Bcvs[q], Bcw, BE[q]], [BE[q]])
                    elif j == 5:
                        tt("dve", MIXT[:, 4 + q, :], ps, Eall[:, q, :], ALU.mult, [Bp, BE[q]], [BMX[4 + q]])
            gla_group(d)
            if d == 0:
                for h in range(4):
                    osum = OB[:, h, :]
                    act(osq, osum, AF.Square, [BOB[h]], [Bosq])
                    ps, Bp = bank()
                    mm([(ps, onesr, osq, True, True)], [Bosq, Bor], [Bp])
                    ts("dve", Td, ps, 1.0 / 128.0, EPS, ALU.mult, ALU.add, [Bp], [BTd])
                    act(Td, Td, AF.Sqrt, [BTd], [BTd])
                    S.op("dve", lambda e: e.reciprocal(Td, Td), reads=[BTd], writes=[BTd])
                    tt("dve", Tb, osum, Td, ALU.mult, [BOB[h], BTd], [BTb])
                    stt(MIXT[:, h, :], Tb, hgn[:, 0:1], SG[:, h, :], ALU.mult, ALU.mult, [BTb, Bhg, BSG[h]], [BMX[h]])
            if d == 1:
                S.dma("act", st_of[gi], OB, reads=BOB, writes=[Bstash[gi]], slot=BOB[0])
                if dbg:
                    S.dma("act", dbg_d["of"][gi], OB, reads=BOB, slot=BOB[0], out_final=True)
                continue
            wo = []
            for hf in range(2):
                wt, Bw = wslot()
                S.dma("sp", wt, wout_v[:, :, hf * 512:(hf + 1) * 512], writes=[Bw])
                wo.append((wt, Bw))
            for t in range(4):
                r0 = (gi * 4 + t) * 128
                xt, Bx = xsl[xcnt[0] % 2]
                xcnt[0] += 1
                S.dma("sp", xt, x_d[r0:r0 + 128, :], writes=[Bx])
                for hf in range(2):
                    ps, Bp = bank()
                    wt, Bw = wo[hf]
                    mm([(ps, MIXT[:, kc, t * 128:(t + 1) * 128], wt[:, kc, :], kc == 0, kc == 7) for kc in range(8)],
                       BMX + [Bw], [Bp])
                    hsl = slice(hf * 512, (hf + 1) * 512)
                    tt("dve", xn[:, hsl], ps, g1bc[:, hsl], ALU.mult, [Bp, Bbc], [Bxn])
                    tt("pool", xn[:, hsl], xn[:, hsl], xt[:, hsl], ALU.add, [Bxn, Bx], [Bxn])
                S.dma("act", x2_d[r0:r0 + 128, :], xn, reads=[Bxn], slot=Bxn, out_final=dbg)
                rs, Brs = rstd_of(xn, Bxn)
                stt(junk, xn, rs, A2bc, ALU.mult, ALU.mult, [Bxn, Brs, Bbc], [Bjunk])
                tt("pool", junk, junk, B2bc, ALU.add, [Bjunk, Bbc], [Bjunk])
                S.dma("act", hx2_d[r0:r0 + 128, :], junk, reads=[Bjunk], slot=Bjunk, out_final=dbg)
                for half in range(2):
                    ps, Bp = bank()
                    trs([(ps[:, q * 128:(q + 1) * 128], junk[:, (half * 4 + q) * 128:(half * 4 + q + 1) * 128], ident)
                         for q in range(4)], [Bjunk, Bc], [Bp])
                    cp("act", hx2T[:, half * 4:(half + 1) * 4, :].rearrange("p k t -> p (k t)"), ps, [Bp], [Bh2T])
                ps, Bp = bank()
                mm([(ps[0:16, 0:128], wr[:, k * 16:(k + 1) * 16], hx2T[:, k, :], k == 0, k == 7) for k in range(8)],
                   [Bwr, Bh2T], [Bp])
                act(eT, ps[0:16, 0:128], AF.Exp, [Bp], [BeT])
                ps2, Bp2 = bank()
                mm([(ps2[0:16, 0:128], C("ones", rows=16, hi=16), eT, True, True)], [BeT, Bc], [Bp2])
                S.op("dve", lambda e, ps2=ps2: e.reciprocal(affsb, ps2[0:16, 0:128]), reads=[Bp2], writes=[Baf])
                tt("dve", affsb, affsb, eT, ALU.mult, [Baf, BeT], [Baf])
                S.dma("act", aff_d[:, r0:r0 + 128], affsb, reads=[Baf], slot=Baf, out_final=dbg)

    sweep(1)
    if stage == 2:
        S.dma("act", out_d[0:128, :], nfin, reads=[Bnf], slot=Bnf, out_final=True)
        S.scope_pop(); S.scope_pop()
        S.finish()
        return nc
    sweep(0)
    S.scope_pop()
    S.scope_pop()
    if stage == 3:
        S.dma("act", out_d[0:128, :], nfin, reads=[Bnf], slot=Bnf, out_final=True)
        S.finish()
        return nc
    return _build_moe(nc, S, L, locals(), stage, dbg)


def _build_moe(nc, S, L, L2, stage, dbg):
    import concourse.bass as bass_
    act, tt, ts, stt, cp, mm, trs, C = [L[n] for n in ("act", "tt", "ts", "stt", "cp", "mm", "trs", "C")]
    x2_d, hx2_d, out_d, nfin, Bnf, g2bc, Bbc, Bc, ident, ipb = [L[n] for n in (
        "x2_d", "hx2_d", "out_d", "nfin", "Bnf", "g2bc", "Bbc", "Bc", "ident", "ipb")]
    wg_d, wu_d, wd_d = L["wg_d"], L["wu_d"], L["wd_d"]
    aff_d = L2["aff_d"]
    yacc = [(L["sc_ps"], L["Bsc"]), (L["kv_ps"], L["Bkv"]), (L["o_ps"], L["Bo"]), (L["trb_ps"], L["Btrb"])]
    R_ps, BR = ipb[3]
    rot = ipb[0:3]
    rn = [0]

    def bank3():
        r = rot[rn[0] % 3]
        rn[0] += 1
        return r
    Bx2all = S.buf("x2all")
    Bhx2all = S.buf("hx2all")

    S.scope_push()
    slotT = S.sbs("slotT", [128, 4, 128], F32); BslT = S.buf()
    affT = S.sbs("affT", [128, 4, 128], F32); BafT = S.buf()
    vals = S.sbs("vals", [128, 4, 128, 3], F32R); Bvals = S.buf()
    Rsb = S.sbs("Rsb", [3, 512], F32); BRsb = S.buf()
    IG = S.sbs("IG", [128, 12], F32); BIG = S.buf()
    idxs = [(S.sbs(f"idx{i}", [128, 4], I32), S.buf(f"idx{i}")) for i in range(2)]
    gates = [(S.sbs(f"gate{i}", [128, 4], F32), S.buf(f"gate{i}")) for i in range(2)]
    sm = S.sbs("smalls", [128, 16], F32); Bsm = S.buf()
    lo, hi, mid, cnt, ge, dl, nge, off = [sm[:, i:i + 1] for i in range(8)]
    S.scope_push()
    aff128 = S.sbs("aff128", [128, 512], F32); Ba128 = S.buf()
    msk = S.sbs("msk", [128, 512], F32); Bmsk = S.buf()
    cum = S.sbs("cum", [128, 512], F32); Bcum = S.buf()

    S.dma("sp", aff128, aff_d.rearrange("e (s t) -> (e s) t", t=512), writes=[Ba128])
    S.op("dve", lambda e: e.memset(sm, 0.0), writes=[Bsm])
    S.op("dve", lambda e: e.memset(hi, 2.0), reads=[Bsm], writes=[Bsm])
    for it in range(34):
        tt("dve", mid, lo, hi, ALU.add, [Bsm], [Bsm])
        ts("dve", mid, mid, 0.5, None, ALU.mult, None, [Bsm], [Bsm])
        ts("dve", msk, aff128, mid, None, ALU.is_ge, None, [Ba128, Bsm], [Bmsk])
        S.op("dve", lambda e: e.reduce_sum(cnt, msk, AX.X), reads=[Bmsk], writes=[Bsm])
        ps, Bp = bank3()
        mm([(ps[:, 0:1], C("bones"), cnt, True, True)], [Bc, Bsm], [Bp])
        ts("dve", ge, ps[:, 0:1], float(CAP), None, ALU.is_ge, None, [Bp], [Bsm])
        tt("dve", dl, mid, lo, ALU.subtract, [Bsm], [Bsm])
        stt(lo, dl, ge, lo, ALU.mult, ALU.add, [Bsm], [Bsm])
        tt("dve", dl, hi, mid, ALU.subtract, [Bsm], [Bsm])
        stt(hi, dl, ge, mid, ALU.mult, ALU.add, [Bsm], [Bsm])
    import os
    MOECUT = int(os.environ.get("MOECUT", "0"))
    MOEN = int(os.environ.get("MOEN", str(N_EXP)))
    ones512 = S.sbs("ones512", [128, 512], F32); Bo512 = S.buf()
    S.op("pool", lambda e: e.memset(ones512, 1.0), writes=[Bo512])
    ts("dve", msk, aff128, lo, None, ALU.is_ge, None, [Ba128, Bsm], [Bmsk])
    S.op("dve", lambda e: e.reduce_sum(cnt, msk, AX.X), reads=[Bmsk], writes=[Bsm])
    ps, Bp = bank3()
    mm([(ps[:, 0:1], C("segpre"), cnt, True, True)], [Bc, Bsm], [Bp])
    cp("dve", off, ps[:, 0:1], [Bp], [Bsm])
    S.op("dve", lambda e: e.tensor_tensor_scan(cum, ones512, msk, off, ALU.mult, ALU.add),
         reads=[Bmsk, Bsm, Bo512], writes=[Bcum])
    tt("dve", cum, cum, msk, ALU.mult, [Bcum, Bmsk], [Bcum])
    ts("dve", cum, cum, -1.0, None, ALU.add, None, [Bcum], [Bcum])
    for (src, Bsrc, dst, Bdst) in ((cum, Bcum, slotT, BslT), (aff128, Ba128, affT, BafT)):
        ps, Bp = bank3()
        trs([(ps[:, c * 128:(c + 1) * 128], src[:, c * 128:(c + 1) * 128], ident) for c in range(4)], [Bsrc, Bc], [Bp])
        cp("act", dst.rearrange("p c q -> p (c q)"), ps, [Bp], [Bdst])
    valsf = vals.bitcast(F32)
    cp("dve", vals[:, :, :, 1], affT, [BafT], [Bvals])
    tt("dve", vals[:, :, :, 2], affT, valsf[:, :, :, 1], ALU.subtract, [BafT, Bvals], [Bvals])
    tki = C("tokidx").rearrange("p (c o s) -> p c o s", c=4, o=1)
    for c in range(4):
        cp("dve", vals[:, c, :, 0].rearrange("p (e s) -> p e s", s=8), tki[:, c, :, :].to_broadcast([128, 16, 8]),
           [Bc], [Bvals])

    S.scope_pop()
    S.scope_push()
    wsl = [(S.sbs(f"mw{i}", [128, 8, 512], F32R), S.buf(f"mw{i}")) for i in range(3)]
    wn = [0]

    def wslot():
        r = wsl[wn[0] % 3]
        wn[0] += 1
        return r
    xsT = [(S.sbs(f"xsT{i}", [128, 8, 512], F32R), S.buf(f"xsT{i}")) for i in range(2)]
    hidT = S.sbs("hidT", [128, 16, 512], F32R); Bhid = [S.buf() for _ in range(16)]
    xstok = [(S.sbs(f"xstok{i}", [128, 1024], F32), S.buf(f"xstok{i}")) for i in range(2)]
    ysb = [(S.sbs(f"ysb{i}", [128, 1024], F32), S.buf(f"ysb{i}")) for i in range(4)]
    sgt = [(S.sbs(f"sgt{i}", [128, 512], F32), S.buf(f"sgt{i}")) for i in range(2)]
    Qb = [(S.sbs(f"Qb{i}", [128, 512], F32R), S.buf(f"Qb{i}")) for i in range(3)]

    if dbg:
        dbg_lo = nc.dram_tensor("dbg_lo", [128, 16], F32, kind="ExternalOutput").ap()
        dbg_idx = nc.dram_tensor("dbg_idx", [16, 128, 4], I32, kind="ExternalOutput").ap()
        dbg_gate = nc.dram_tensor("dbg_gate", [16, 128, 4], F32, kind="ExternalOutput").ap()
        dbg_slot = nc.dram_tensor("dbg_slot", [128, 512], F32, kind="ExternalOutput").ap()
        S.dma("sp", dbg_lo, sm, reads=[Bsm], slot=S.buf(), out_final=True)
        S.dma("sp", dbg_slot, slotT.rearrange("p c q -> p (c q)"), reads=[BslT], slot=S.buf(), out_final=True)

    def prep(e):
        xT, BxT = xsT[e % 2]
        idx, Bidx = idxs[e % 2]
        gat, Bgat = gates[e % 2]
        for i in range(32):
            s_, c = i // 4, i % 4
            col = e * 8 + s_
            Q, BQ = Qb[i % 3]
            ts("dve", Q, C("iota"), slotT[:, c, col:col + 1], None, ALU.is_equal, None,
               [Bc, BslT], [BQ])
            mm([(R_ps[0:3, :], vals[:, c, col, :], Q, i == 0, i == 31)], [Bvals, BQ], [BR])
            yield
        cp("act", Rsb, R_ps[0:3, :], [BR], [BRsb])
        ps, Bp = bank3()
        trs([(ps[:, b * 3:(b + 1) * 3], Rsb[0:3, b * 128:(b + 1) * 128], ident[0:3, 0:3]) for b in range(4)],
            [BRsb, Bc], [Bp])
        cp("dve", IG, ps[:, 0:12], [Bp], [BIG])
        IG3 = IG.rearrange("p (b j) -> p b j", j=3)
        cp("dve", idx, IG3[:, :, 0], [BIG], [Bidx])
        tt("dve", gat, IG3[:, :, 1], IG3[:, :, 2], ALU.add, [BIG], [Bgat])
        if dbg:
            S.dma("sp", dbg_idx[e], idx, reads=[Bidx], slot=S.buf(), out_final=True)
            S.dma("sp", dbg_gate[e], gat, reads=[Bgat], slot=S.buf(), out_final=True)
        yield
        for b in range(4):
            xt, Bx = xstok[b % 2]
            S.op_dma_ind("pool", lambda en, xt=xt, b=b: en.indirect_dma_start(
                out=xt, out_offset=None, in_=hx2_d, in_offset=bass_.IndirectOffsetOnAxis(ap=idx[:, b:b + 1], axis=0)),
                reads=[Bidx, Bhx2all], writes=[Bx])
            for half in range(2):
                ps, Bp = bank3()
                trs([(ps[:, q * 128:(q + 1) * 128], xt[:, (half * 4 + q) * 128:(half * 4 + q + 1) * 128], ident)
                     for q in range(4)], [Bx, Bc], [Bp])
                for q in range(4):
                    cp("act", xT[:, half * 4 + q, b * 128:(b + 1) * 128], ps[:, q * 128:(q + 1) * 128], [Bp], [BxT])
            yield

    def drain(gen):
        for _ in gen:
            pass

    def tick(gen):
        if gen is not None:
            next(gen, None)

    if MOECUT != 2:
        drain(prep(0))
    for e in range(MOEN if MOECUT not in (2, 3) else 0):
        gen = prep(e + 1) if e + 1 < MOEN else None
        xT, BxT = xsT[e % 2]
        idx, Bidx = idxs[e % 2]
        gat, Bgat = gates[e % 2]
        wgv = wg_d[e].rearrange("(k p) f -> p k f", p=128)
        wuv = wu_d[e].rearrange("(k p) f -> p k f", p=128)
        wdv = wd_d[e].rearrange("(g c p) d -> g p c d", p=128, c=4)
        for fg in range(4):
            wgt, Bwg = wslot()
            S.dma("sp", wgt, wgv[:, :, fg * 512:(fg + 1) * 512], writes=[Bwg])
            wut, Bwu = wslot()
            S.dma("sp", wut, wuv[:, :, fg * 512:(fg + 1) * 512], writes=[Bwu])
            for fc in range(4):
                f = fg * 4 + fc
                sg, Bsg = sgt[f % 2]
                ps, Bp = bank3()
                mm([(ps, wgt[:, k, fc * 128:(fc + 1) * 128], xT[:, k, :], k == 0, k == 7) for k in range(8)],
                   [Bwg, BxT], [Bp])
                act(sg, ps, AF.Silu, [Bp], [Bsg])
                tick(gen)
                ps2, Bp2 = bank3()
                mm([(ps2, wut[:, k, fc * 128:(fc + 1) * 128], xT[:, k, :], k == 0, k == 7) for k in range(8)],
                   [Bwu, BxT], [Bp2])
                tt("dve", hidT[:, f, :], ps2, sg, ALU.mult, [Bp2, Bsg], [Bhid[f]])
                tick(gen)
        for dh in range(2):
            for fg in range(4):
                wt, Bw = wslot()
                wt4 = wt.rearrange("p (a c) d -> p a c d", a=2)[:, 0, :, :]
                S.dma("sp", wt4, wdv[fg][:, :, dh * 512:(dh + 1) * 512], writes=[Bw])
                for t4 in range(4):
                    ya, Bya = yacc[t4]
                    mm([(ya, hidT[:, fg * 4 + fc, t4 * 128:(t4 + 1) * 128], wt4[:, fc, :], fg == 0 and fc == 0,
                         fg == 3 and fc == 3) for fc in range(4)], [Bhid[fg * 4 + fc] for fc in range(4)] + [Bw], [Bya])
                    tick(gen)
            for t4 in range(4):
                ya, Bya = yacc[t4]
                yt, By = ysb[t4]
                hsl = slice(dh * 512, (dh + 1) * 512)
                stt(yt[:, hsl], ya, gat[:, t4:t4 + 1], g2bc[:, hsl], ALU.mult, ALU.mult, [Bya, Bgat, Bbc], [By])
        for t4 in range(4):
            yt, By = ysb[t4]
            S.op_dma_ind("pool", lambda en, yt=yt, t4=t4, idx=idx: en.indirect_dma_start(
                out=x2_d, out_offset=bass_.IndirectOffsetOnAxis(ap=idx[:, t4:t4 + 1], axis=0), in_=yt, in_offset=None,
                compute_op=ALU.add), reads=[Bidx, By, Bx2all], writes=[Bx2all], slot=By)
        if gen is not None:
            drain(gen)
    S.scope_pop()
    S.scope_pop()

    S.scope_push()
    xs = [(S.sbs(f"fx{i}", [128, 1024], F32), S.buf()) for i in range(2)]
    ys = [(S.sbs(f"fy{i}", [128, 1024], F32), S.buf()) for i in range(2)]
    jk = S.sbs("fjunk", [128, 1024], F32); Bjk = S.buf()
    ss = S.sbs("fss", [128, 4], F32); Bs4 = [S.buf() for _ in range(4)]
    for t in range(32):
        xt, Bx = xs[t % 2]; yt, By = ys[t % 2]
        S.dma("sp", xt, x2_d[t * 128:(t + 1) * 128, :], reads=[Bx2all], writes=[Bx])
        col = ss[:, t % 4:t % 4 + 1]; Bcol = Bs4[t % 4]
        act(jk, xt, AF.Square, [Bx], [Bjk])
        S.op("dve", lambda en, col=col: en.reduce_sum(col, jk, AX.X), reads=[Bjk], writes=[Bcol])
        ts("dve", col, col, 1.0 / D_MODEL, EPS, ALU.mult, ALU.add, [Bcol], [Bcol])
        act(col, col, AF.Sqrt, [Bcol], [Bcol])
        S.op("dve", lambda en, col=col: en.reciprocal(col, col), reads=[Bcol], writes=[Bcol])
        stt(yt, xt, col, nfin, ALU.mult, ALU.mult, [Bx, Bcol, Bnf], [By])
        S.dma("act", out_d[t * 128:(t + 1) * 128, :], yt, reads=[By], slot=By, out_final=True)
    S.scope_pop()
    S.finish()
    return nc
```

```python
import numpy as np
import concourse.bass as bass
import concourse.mybir as mybir
from concourse.bass_utils import run_bass_kernel_spmd

F32 = mybir.dt.float32
BF16 = mybir.dt.bfloat16
I32 = mybir.dt.int32
F32R = mybir.dt.float32r
AF = mybir.ActivationFunctionType
ALU = mybir.AluOpType
AX = mybir.AxisListType

ENGS = ("pe", "act", "dve", "pool", "sp")


class Counter:
    def __init__(self, nc, name, step, epoch):
        self.nc, self.name, self.step, self.epoch = nc, name, step, epoch
        self.n = 0
        self.sems = []

    def next(self):
        self.n += 1
        e = (self.n - 1) // self.epoch
        while len(self.sems) <= e:
            self.sems.append(self.nc.alloc_semaphore(f"{self.name}_e{len(self.sems)}"))
        return self.n

    def sem_of(self, n):
        e = (n - 1) // self.epoch
        return self.sems[e], (n - e * self.epoch) * self.step


class Buf:
    __slots__ = ("name", "w", "r", "ctr")

    def __init__(self, name=""):
        self.name = name
        self.w = {}
        self.r = {}
        self.ctr = None


class Sched:
    def __init__(self, nc):
        self.nc = nc
        self.q = {e: [] for e in ENGS}
        self.eng_ctr = {e: Counter(nc, f"tk_{e}", 1, 12000) for e in ("pe", "act", "dve", "pool")}
        self.waited = {e: {} for e in ENGS}
        self.finals = {}
        self.n_dma_ctr = 0
        self.nbuf = 0
        self.scopes = []
        self.all_dma_ctrs = []

    def sb(self, name, shape, dt):
        assert not self.scopes, "persistent alloc inside scope: " + name
        return self.nc.alloc_sbuf_tensor("sb_" + name, list(shape), dt).ap()

    def ps(self, name, shape, dt=F32):
        return self.nc.alloc_psum_tensor("ps_" + name, list(shape), dt).ap()

    def buf(self, name=None):
        self.nbuf += 1
        return Buf(name or f"b{self.nbuf}")

    def _ctr_for(self, b):
        if b.ctr is None:
            self.n_dma_ctr += 1
            b.ctr = Counter(self.nc, f"dq{self.n_dma_ctr}", 16, 900)
            self.all_dma_ctrs.append(b.ctr)
        return b.ctr

    def _deps(self, eng, reads, writes):
        need = {}

        def add(c, n, src_eng):
            if src_eng == eng and eng == "pe":
                return
            if need.get(c, 0) < n:
                need[c] = n

        for b in reads:
            for c, (n, se) in b.w.items():
                add(c, n, se)
        for b in writes:
            for c, (n, se) in b.w.items():
                if se == eng and se != "dma":
                    continue
                add(c, n, se)
            for c, (n, se) in b.r.items():
                if se == eng and se != "dma":
                    continue
                add(c, n, se)
        waits = []
        wd = self.waited[eng]
        for c, n in need.items():
            if wd.get(c, 0) >= n:
                continue
            wd[c] = n
            waits.append(c.sem_of(n))
        return waits

    def _record(self, c, n, src_eng, reads, writes):
        for b in reads:
            old = b.r.get(c)
            if old is None or old[0] < n:
                b.r[c] = (n, src_eng)
        for b in writes:
            b.w = {c: (n, src_eng)}
            b.r = {}

    def op(self, eng, fn, reads=(), writes=()):
        waits = self._deps(eng, reads, writes)
        c = self.eng_ctr[eng]
        n = c.next()
        sem, _ = c.sem_of(n)
        self._record(c, n, eng, reads, writes)

        def emit(e, waits=waits, fn=fn, sem=sem):
            for s, v in waits:
                e.wait_ge(s, v)
            ins = fn(e)
            ins.then_inc(sem, 1)
        self.q[eng].append(emit)

    def dma(self, qeng, out, in_, reads=(), writes=(), slot=None, out_final=False, **kw):
        self.op_dma_ind(qeng, lambda e: e.dma_start(out=out, in_=in_, **kw), reads=reads, writes=writes,
                        slot=slot, out_final=out_final)

    def op_dma_ind(self, qeng, fn, reads=(), writes=(), slot=None, out_final=False):
        if slot is None:
            slot = writes[0] if len(writes) else reads[0]
        waits = self._deps(qeng, reads, writes)
        c = self._ctr_for(slot)
        n = c.next()
        sem, _ = c.sem_of(n)
        self._record(c, n, "dma", reads, writes)
        if out_final:
            self.finals[c] = n

        def emit(e, waits=waits, fn=fn, sem=sem):
            for s, v in waits:
                e.wait_ge(s, v)
            fn(e).then_inc(sem, 16)
        self.q[qeng].append(emit)

    def scope_push(self):
        from contextlib import ExitStack
        st = ExitStack()
        self.scopes.append(st)

    def sbs(self, name, shape, dt):
        return self.scopes[-1].enter_context(self.nc.sbuf_tensor("sb_" + name, list(shape), dt)).ap()

    def scope_pop(self):
        self.barrier()
        self.scopes.pop().close()

    def barrier(self):
        ctrs = list(self.eng_ctr.values()) + self.all_dma_ctrs
        for eng in ENGS:
            waits = []
            wd = self.waited[eng]
            for c in ctrs:
                if c.n > 0 and wd.get(c, 0) < c.n:
                    wd[c] = c.n
                    waits.append(c.sem_of(c.n))

            def emit(e, waits=waits):
                for s_, v in waits:
                    e.wait_ge(s_, v)
            self.q[eng].append(emit)

    def finish(self):
        nc = self.nc
        finals = [c.sem_of(n) for c, n in self.finals.items()]
        q = self.q
        with nc.Block() as block:
            @block.sync
            def _(e):
                for f in q["sp"]:
                    f(e)
                for s, v in finals:
                    e.wait_ge(s, v)

            @block.tensor
            def _(e):
                for f in q["pe"]:
                    f(e)

            @block.scalar
            def _(e):
                for f in q["act"]:
                    f(e)

            @block.vector
            def _(e):
                for f in q["dve"]:
                    f(e)

            @block.gpsimd
            def _(e):
                for f in q["pool"]:
                    f(e)


def _const_layout():
    lay = {}
    off = 0
    for name, n in (("ident", 128), ("maskf", 128), ("maskb", 128), ("mgt", 128), ("mlt", 128), ("ones", 128),
                    ("rsf", 512), ("rsb", 512), ("cind", 4), ("iota", 512), ("tokab", 64), ("bones", 128),
                    ("wcomb", 2), ("segpre", 128), ("tokidx", 32)):
        lay[name] = (off, n)
        off += n
    return lay, off


CL, NCONST = _const_layout()


def make_consts():
    c = np.zeros((128, NCONST), np.float32)

    def put(name, arr):
        o, n = CL[name]
        c[:arr.shape[0], o:o + n] = arr
    p = np.arange(128)
    put("ident", np.eye(128, dtype=np.float32))
    j = p[:, None]; i = p[None, :]
    same = (j // 32) == (i // 32)
    mf = (same & (j <= i)).astype(np.float32)
    mb = (same & (j >= i)).astype(np.float32)
    put("maskf", mf); put("maskb", mb)
    put("mgt", (j > i).astype(np.float32))
    put("mlt", (j < i).astype(np.float32))
    put("ones", np.ones((128, 128), np.float32))
    t = np.arange(512)
    put("rsf", np.broadcast_to((t % 32 != 0).astype(np.float32), (128, 512)))
    put("rsb", np.broadcast_to((t % 32 != 31).astype(np.float32), (128, 512)))
    put("cind", (p[:, None] // 32 == np.arange(4)[None, :]).astype(np.float32))
    put("iota", np.broadcast_to(t.astype(np.float32), (128, 512)))
    tok = (np.arange(32)[None, :] * 128 + p[:, None])
    ab = np.stack([tok // 64, tok % 64], axis=-1).reshape(128, 64).astype(np.float32)
    put("tokab", ab)
    put("bones", ((p[:, None] // 8) == (p[None, :] // 8)).astype(np.float32))
    wc = np.zeros((128, 2), np.float32)
    wc[0, 0] = 64.0; wc[1, 0] = 1.0; wc[2, 1] = 1.0; wc[3, 1] = 1.0; wc[4, 1] = 1.0
    put("wcomb", wc)
    put("segpre", (((p[:, None] // 8) == (p[None, :] // 8)) & ((p[:, None] % 8) < (p[None, :] % 8))).astype(np.float32))
    cs = np.arange(32)
    put("tokidx", ((cs[None, :] % 8) * 512 + (cs[None, :] // 8) * 128 + p[:, None]).astype(np.float32))
    return c


T_SEQ, D_MODEL, N_EXP, CAP, DFF = 4096, 1024, 16, 512, 2048
HAS_MOE = True
EPS = 1e-6


def build_program(stage=99, dbg=False):
    nc = bass.Bass("TRN2", target_bir_lowering=False)
    nc.dge_precook = False
    S = Sched(nc)

    def din(name, shape, dt=F32):
        return nc.dram_tensor(name, list(shape), dt, kind="ExternalInput").ap()

    def dscr(name, shape, dt=F32, out=False):
        if out:
            return nc.dram_tensor(name, list(shape), dt, kind="ExternalOutput").ap()
        return nc.dram_tensor(name, list(shape), dt).ap()

    x_d = din("x", [T_SEQ, D_MODEL]); ctx_d = din("ctx", [256, D_MODEL]); cc_d = din("ccT", [128, 16])
    wada_d = din("w_ada", [1024, 6144], F32R); bada_d = din("b_ada", [1, 6144])
    nmix_d = din("nmix_fm", [128, 8]); nffn_d = din("nffn_bc", [128, 1024]); nfin_d = din("nfin_bc", [128, 1024])
    win_d = din("w_in", [1024, 4096], F32R)
    lblfm_d = din("lbl_fm", [128, 16]); lblbc_d = din("lbl_bc", [128, 2048])
    hgn_d = din("hgn_fm", [128, 1]); convw_d = din("convw_fm", [128, 12])
    wout_d = din("w_out", [1024, 1024], F32R); wr_d = din("w_r_fm", [128, 128])
    if stage >= 4 and HAS_MOE:
        wg_d = din("w_gate", [N_EXP, 1024, DFF], F32R); wu_d = din("w_up", [N_EXP, 1024, DFF], F32R)
        wd_d = din("w_down", [N_EXP, DFF, 1024], F32R)
    consts_d = din("consts", [128, NCONST])
    out_d = nc.dram_tensor("out", [T_SEQ, D_MODEL], F32, kind="ExternalOutput").ap()
    x2_d = dscr("x2", [T_SEQ, D_MODEL], F32, out=dbg)
    hx2_d = dscr("hx2", [T_SEQ, D_MODEL], F32, out=dbg)
    st_qd = dscr("st_qd", [8, 128, 4, 512], BF16); st_ki = dscr("st_ki", [8, 128, 4, 512], BF16)
    st_ke = dscr("st_ke", [8, 128, 4, 4, 128], BF16); st_v = dscr("st_v", [8, 128, 4, 512], BF16)
    st_el = dscr("st_el", [8, 128, 4, 16], F32); st_of = dscr("st_of", [8, 128, 4, 512], F32)
    st_sg = dscr("st_sg", [8, 128, 4, 512], BF16); st_yb = dscr("st_yb", [8, 128, 4, 512], F32R)
    dbg_d = {}
    if dbg:
        dbg_d["s0"] = dscr("dbg_s0", [8, 128, 128], F32, out=True)
        dbg_d["aff"] = dscr("dbg_aff", [16, T_SEQ], F32, out=True)
        dbg_d["of"] = dscr("dbg_of", [8, 128, 4, 512], F32, out=True)
        dbg_d["mod"] = dscr("dbg_mod", [2, 6144], F32, out=True)

    def act(out, in_, func, rd, wr, **kw):
        S.op("act", lambda e: e.activation(out, in_, func, **kw), reads=rd, writes=wr)

    def tt(eng, out, a, b, op, rd, wr):
        S.op(eng, lambda e: e.tensor_tensor(out, a, b, op), reads=rd, writes=wr)

    def ts(eng, out, a, s1, s2, op0, op1, rd, wr):
        if s2 is None:
            S.op(eng, lambda e: e.tensor_scalar(out, a, s1, None, op0), reads=rd, writes=wr)
        else:
            S.op(eng, lambda e: e.tensor_scalar(out, a, s1, s2, op0, op1), reads=rd, writes=wr)

    def stt(out, a, s, b, op0, op1, rd, wr):
        S.op("dve", lambda e: e.scalar_tensor_tensor(out, a, s, b, op0, op1), reads=rd, writes=wr)

    def cp(eng, out, in_, rd, wr):
        if eng == "act":
            S.op("act", lambda e: e.activation(out, in_, AF.Copy), reads=rd, writes=wr)
        else:
            S.op(eng, lambda e: e.tensor_copy(out, in_), reads=rd, writes=wr)

    def mm(lst, rd, wr):
        def f(e, lst=lst):
            ins = None
            for (o, l, r, st, sp) in lst:
                ins = e.matmul(o, lhsT=l, rhs=r, start=st, stop=sp)
            return ins
        S.op("pe", f, reads=rd, writes=wr)

    def trs(lst, rd, wr):
        def f(e, lst=lst):
            ins = None
            for (o, i, idn) in lst:
                ins = e.transpose(o, i, idn)
            return ins
        S.op("pe", f, reads=rd, writes=wr)

    ipb = [(S.ps(f"ip{i}", [128, 512], F32), S.buf(f"ip{i}")) for i in range(4)]
    ipn = [0]

    def bank():
        r = ipb[ipn[0] % 4]
        ipn[0] += 1
        return r
    sc_ps, Bsc = S.ps("sc", [128, 512], F32), S.buf("sc")
    kv_ps, Bkv = S.ps("kv", [128, 512], F32), S.buf("kv")
    o_ps, Bo = S.ps("o", [128, 512], F32), S.buf("o")
    trb_ps, Btrb = S.ps("trb", [128, 512], F32), S.buf("trb")

    consts = S.sb("consts", [128, NCONST], F32); Bc = S.buf("consts")
    S.dma("sp", consts, consts_d, writes=[Bc])

    def C(name, rows=128, lo=0, hi=None):
        o, n = CL[name]
        return consts[0:rows, o + lo: o + (n if hi is None else hi)]
    ident = C("ident")
    identb = None; Bib = S.buf()
    onesr = S.sb("onesr", [128, 128], F32R); Bor = S.buf()
    cp("dve", onesr, C("ones"), [Bc], [Bor])
    mhalf = S.sb("mhalf", [128, 1], F32); Bmh = S.buf()
    S.op("pool", lambda e: e.memset(mhalf, -0.5), writes=[Bmh])

    def small_in(name, src, shape):
        t = S.sb(name, shape, F32); b = S.buf(name)
        S.dma("sp", t, src, writes=[b])
        return t, b
    ccT, Bcc = small_in("ccT", cc_d, [128, 16])
    nmix, Bnm = small_in("nmix", nmix_d, [128, 8])
    lblfm, Blf = small_in("lblfm", lblfm_d, [128, 16])
    hgn, Bhg = small_in("hgn", hgn_d, [128, 1])
    convw, Bcw = small_in("convw", convw_d, [128, 12])
    wr, Bwr = small_in("wr", wr_d, [128, 128])
    nfin, Bnf = small_in("nfin", nfin_d, [128, 1024])
    lbfm = S.sb("lbfm", [128, 8], F32); omlfm = S.sb("omlfm", [128, 8], F32); nomlfm = S.sb("nomlfm", [128, 8], F32)
    tmp8 = S.sb("tmp8", [128, 8], F32); Bl = S.buf(); Bt8 = S.buf()
    tt("dve", tmp8, lblfm[:, 0:8], lblfm[:, 8:16], ALU.subtract, [Blf], [Bt8])
    act(lbfm, tmp8, AF.Sigmoid, [Bt8], [Bl])
    ts("dve", omlfm, lbfm, -1.0, 1.0, ALU.mult, ALU.add, [Bl], [Bl])
    ts("dve", nomlfm, lbfm, 1.0, -1.0, ALU.mult, ALU.add, [Bl], [Bl])

    S32 = S.sb("S32", [128, 2, 8, 128], F32R)
    S16 = None
    BS32 = [S.buf(f"S32_{i}") for i in range(8)]
    BS32b = [[BS32[i] for i in range(8)], [S.buf(f"S32b_{i}") for i in range(8)]]
    stver = [0, 0]
    BS16 = [[S.buf(f"S16_{v}_{i}") for i in range(8)] for v in range(2)]
    sver = [0] * 8

    A1 = S.sb("A1", [128, 8], F32); B1 = S.sb("B1", [128, 8], F32)
    Ac = S.sb("Acx", [128, 8], F32); Bcx = S.sb("Bcx", [128, 8], F32); Bab = S.buf("A1B1")
    g1bc = S.sb("g1bc", [128, 1024], F32); A2bc = S.sb("A2bc", [128, 1024], F32)
    B2bc = S.sb("B2bc", [128, 1024], F32); g2bc = S.sb("g2bc", [128, 1024], F32); Bbc = S.buf("bcs")

    sc = S.sb("silu_c", [128, 16], F32R); Bscx = S.buf()
    modT = S.sb("modT", [128, 32], F32); BmT = S.buf()
    ss_t = S.sb("ss_t", [128, 4], F32); Bss = [S.buf() for _ in range(4)]
    S.scope_push()
    wsl = [(S.sbs(f"wsl{i}", [128, 8, 512], F32R), S.buf(f"wsl{i}")) for i in range(2)]
    wsn = [0]

    def wslot():
        r = wsl[wsn[0] % 2]
        wsn[0] += 1
        return r
    junk = S.sbs("junk", [128, 1024], F32); Bjunk = S.buf()
    xsl = [(S.sbs(f"xsl{i}", [128, 1024], F32), S.buf(f"xsl{i}")) for i in range(2)]
    xn = S.sbs("xn", [128, 1024], F32); Bxn = S.buf()
    hxT = S.sbs("hxT", [128, 8, 512], F32R); BhxT = S.buf("hxT")
    S.scope_push()
    wada_v = wada_d.rearrange("(k p) n -> p k n", p=128)
    win_v = win_d.rearrange("(k p) n -> p k n", p=128)

    act(sc, ccT, AF.Silu, [Bcc], [Bscx])
    sc3 = sc.rearrange("p (k j) -> p k j", j=2)
    nffn = S.sbs("nffn", [128, 1024], F32); Bnff = S.buf()
    S.dma("sp", nffn, nffn_d, writes=[Bnff])
    mrow = [(S.sbs(f"mrow{i}", [2, 512], F32), S.sbs(f"brow{i}", [2, 512], F32), S.buf(), S.buf()) for i in range(2)]
    psT, BpT = trb_ps, Btrb
    bcdst = {4: (g1bc, 0, 0), 5: (g1bc, 1, 0), 6: (B2bc, 0, 0), 7: (B2bc, 1, 0), 8: (A2bc, 0, 1), 9: (A2bc, 1, 1),
             10: (g2bc, 0, 0), 11: (g2bc, 1, 0)}
    for j in range(12):
        wt, Bw = wslot()
        S.dma("sp", wt, wada_v[:, :, j * 512:(j + 1) * 512], writes=[Bw])
        mr, br, Bmr, Bbr = mrow[j % 2]
        S.dma("sp", br[0:1, :], bada_d[:, j * 512:(j + 1) * 512], writes=[Bbr])
        S.dma("sp", br[1:2, :], bada_d[:, j * 512:(j + 1) * 512], writes=[Bbr])
        ps, Bp = bank()
        mm([(ps[0:2, :], sc3[:, k, :], wt[:, k, :], k == 0, k == 7) for k in range(8)], [Bscx, Bw], [Bp])
        tt("dve", mr, ps[0:2, :], br, ALU.add, [Bp, Bbr], [Bmr])
        if dbg:
            S.dma("act", dbg_d["mod"][:, j * 512:(j + 1) * 512], mr, reads=[Bmr], slot=Bmr, out_final=True)
        if j < 4:
            trs([(psT[:, (j * 4 + c) * 2:(j * 4 + c + 1) * 2], mr[0:2, c * 128:(c + 1) * 128], ident[0:2, 0:2])
                 for c in range(4)], [Bmr, Bc], [BpT])
        else:
            dst, hf, kind = bcdst[j]
            ps2, Bp2 = bank()
            mm([(ps2, C("ones", rows=1), mr[0:1, :], True, True)], [Bmr, Bc], [Bp2])
            if kind == 0:
                cp("act", dst[:, hf * 512:(hf + 1) * 512], ps2, [Bp2], [Bbc])
            else:
                stt(dst[:, hf * 512:(hf + 1) * 512], ps2, 1.0, nffn[:, hf * 512:(hf + 1) * 512], ALU.add, ALU.mult,
                    [Bp2, Bnff], [Bbc])
        if j == 3:
            cp("dve", modT, psT[:, 0:32], [BpT], [BmT])
            modT3 = modT.rearrange("p (c j) -> p c j", j=2)
            stt(A1, modT3[:, 8:16, 0], 1.0, nmix, ALU.add, ALU.mult, [BmT, Bnm], [Bab])
            cp("dve", B1, modT3[:, 0:8, 0], [BmT], [Bab])
            stt(Ac, modT3[:, 8:16, 1], 1.0, nmix, ALU.add, ALU.mult, [BmT, Bnm], [Bab])
            cp("dve", Bcx, modT3[:, 0:8, 1], [BmT], [Bab])

    if stage == 0:
        S.dma("act", out_d[0:128, :], nfin, reads=[Bnf], slot=Bnf, out_final=True)
        S.finish()
        return nc
    ssn = [0]

    def rstd_of(xt, Bx, scr=None, Bscr=None):
        i = ssn[0] % 4
        ssn[0] += 1
        col = ss_t[:, i:i + 1]
        scr = junk if scr is None else scr
        Bscr = [Bjunk] if Bscr is None else Bscr
        Bx = Bx if isinstance(Bx, list) else [Bx]
        act(scr, xt, AF.Square, Bx, Bscr)
        S.op("dve", lambda e: e.reduce_sum(col, scr, AX.X), reads=Bscr, writes=[Bss[i]])
        ts("dve", col, col, 1.0 / D_MODEL, EPS, ALU.mult, ALU.add, [Bss[i]], [Bss[i]])
        act(col, col, AF.Sqrt, [Bss[i]], [Bss[i]])
        S.op("dve", lambda e: e.reciprocal(col, col), reads=[Bss[i]], writes=[Bss[i]])
        return col, Bss[i]

    def prep_tile(src_rows, An, Bn, col0, xi):
        xt, Bx = xsl[xi % 2]
        S.dma("sp", xt, src_rows, writes=[Bx])
        rs, Brs = rstd_of(xt, Bx)
        act(xn, xt, AF.Copy, [Bx, Brs], [Bxn], scale=rs)
        for half in range(2):
            ps, Bp = bank()
            trs([(ps[:, q * 128:(q + 1) * 128], xn[:, (half * 4 + q) * 128:(half * 4 + q + 1) * 128], ident)
                 for q in range(4)], [Bxn, Bc], [Bp])
            for q in range(4):
                k = half * 4 + q
                act(hxT[:, k, col0:col0 + 128], ps[:, q * 128:(q + 1) * 128], AF.Identity, [Bp, Bab], [BhxT],
                    scale=An[:, k:k + 1], bias=Bn[:, k:k + 1])

    lblbc = S.sbs("lblbc", [128, 2048], F32); Blb = S.buf()
    S.dma("sp", lblbc, lblbc_d, writes=[Blb])
    lbbc = S.sbs("lbbc", [128, 1024], F32); omlbc = S.sbs("omlbc", [128, 1024], F32); Blbb = S.buf()
    tt("dve", omlbc, lblbc[:, 0:1024], lblbc[:, 1024:2048], ALU.subtract, [Blb], [Blbb])
    act(lbbc, omlbc, AF.Sigmoid, [Blbb], [Blbb])
    ts("dve", omlbc, lbbc, -1.0, 1.0, ALU.mult, ALU.add, [Blbb], [Blbb])
    clogf = S.sbs("clogf", [128, 2, 2, 512], F32R); ck = S.sbs("ck", [128, 2, 2, 512], F32)
    cv16 = S.sbs("cv16", [128, 2, 512], F32R); ckd16 = S.sbs("ckd16", [128, 2, 2, 512], F32R)
    Bclf = [[S.buf() for _ in range(2)] for _ in range(2)]; Bck = [[S.buf() for _ in range(2)] for _ in range(2)]
    Bcv = [S.buf() for _ in range(2)]; Bckd = [[S.buf() for _ in range(2)] for _ in range(2)]
    csig = S.sbs("csig", [128, 512], F32); Bcs = S.buf()
    cf = S.sbs("cf", [128, 512], F32); Bcf = S.buf()
    import os
    KCUT = int(os.environ.get("KCUT", "0"))
    for i in range(2):
        prep_tile(ctx_d[i * 128:(i + 1) * 128, :], Ac, Bcx, i * 128, i)
    if KCUT == 1:
        S.dma("act", out_d[0:128, :], nfin, reads=[Bnf], slot=Bnf, out_final=True)
        S.scope_pop(); S.scope_pop()
        S.finish()
        return nc
    for j in (1, 2, 3):
        wt, Bw = wslot()
        S.dma("sp", wt, win_v[:, :, j * 512:(j + 1) * 512], writes=[Bw])
        for i in range(2):
            ps, Bp = bank()
            mm([(ps, hxT[:, k, i * 128:(i + 1) * 128], wt[:, k, :], k == 0, k == 7) for k in range(8)],
               [BhxT, Bw], [Bp])
            if j == 3:
                cp("act", cv16[:, i, :], ps, [Bp], [Bcv[i]])
            else:
                d = j - 1
                act(csig, ps, AF.Sigmoid, [Bp], [Bcs])
                tt("dve", cf, csig, omlbc[:, d * 512:(d + 1) * 512], ALU.mult, [Bcs, Blbb], [Bcf])
                tt("dve", cf, cf, lbbc[:, d * 512:(d + 1) * 512], ALU.add, [Bcf, Blbb], [Bcf])
                act(clogf[:, d, i, :], cf, AF.Ln, [Bcf], [Bclf[d][i]])
                ts("dve", ck[:, d, i, :], cf, -1.0, 1.0, ALU.mult, ALU.add, [Bcf], [Bck[d][i]])
    if KCUT == 2:
        S.dma("act", out_d[0:128, :], nfin, reads=[Bnf], slot=Bnf, out_final=True)
        S.scope_pop(); S.scope_pop()
        S.finish()
        return nc
    ones_f = onesr
    trir = S.sbs("trir", [128, 256], F32R); Btri = S.buf()
    cp("dve", trir[:, 0:128], C("mgt"), [Bc], [Btri])
    cp("dve", trir[:, 128:256], C("mlt"), [Bc], [Btri])
    for d in range(2):
        for i in range(2):
            ps, Bp = bank()
            tri = trir[:, 0:128] if d == 0 else trir[:, 128:256]
            other = 1 - i
            lst = [(ps, tri, clogf[:, d, i, :], True, False)]
            if (d == 0 and i == 0) or (d == 1 and i == 1):
                lst.append((ps, ones_f, clogf[:, d, other, :], False, True))
                rd = [Bclf[d][0], Bclf[d][1], Btri, Bor]
            else:
                lst[0] = (ps, tri, clogf[:, d, i, :], True, True)
                rd = [Bclf[d][i], Btri]
            mm(lst, rd, [Bp])
            act(csig, ps, AF.Exp, [Bp], [Bcs])
            tt("dve", ckd16[:, d, i, :], ck[:, d, i, :], csig, ALU.mult, [Bck[d][i], Bcs], [Bckd[d][i]])
    if KCUT == 3:
        S.dma("act", out_d[0:128, :], nfin, reads=[Bnf], slot=Bnf, out_final=True)
        S.scope_pop(); S.scope_pop()
        S.finish()
        return nc
    for d in range(2):
        for h in range(4):
            ps, Bp = bank()
            hs = slice(h * 128, (h + 1) * 128)
            mm([(ps[:, 0:128], ckd16[:, d, i, hs], cv16[:, i, hs], i == 0, i == 1) for i in range(2)],
               [Bckd[d][0], Bckd[d][1], Bcv[0], Bcv[1]], [Bp])
            cp("dve", S32[:, 0, d * 4 + h, :], ps[:, 0:128], [Bp], [BS32[d * 4 + h]])
    if dbg and stage == 1:
        for nm, tns, shp, bufs in (("clogf", clogf, [128, 2048], [x for y in Bclf for x in y]), ("ck", ck, [128, 2048], [x for y in Bck for x in y]),
                                   ("ckd", ckd16, [128, 2048], [x for y in Bckd for x in y]), ("cv", cv16, [128, 1024], Bcv),
                                   )[:int(os.environ.get("NDBG", "4"))]:
            dd_ = dscr("dbg_" + nm, shp, F32, out=True)
            flat = tns.bitcast(F32) if nm != "ck" else tns
            if flat.ndim == 4:
                flat = flat.rearrange("p a b c -> p (a b c)")
            elif flat.ndim == 3:
                flat = flat.rearrange("p a b -> p (a b)")
            S.dma("sp", dd_, flat, reads=bufs, slot=S.buf(), out_final=True)
    if dbg and stage == 1:
        for hh in range(2):
            dd_ = dscr(f"dbg_hcT{hh}", [128, 2048], F32, out=True)
            S.dma("sp", dd_, hxT.bitcast(F32)[:, hh * 4:(hh + 1) * 4, :].rearrange("p a b -> p (a b)"), reads=[BhxT], slot=S.buf(), out_final=True)
        dd_ = dscr("dbg_xn", [128, 1024], F32, out=True)
        S.dma("sp", dd_, xn, reads=[Bxn], slot=S.buf(), out_final=True)
        dd_ = dscr("dbg_ss", [128, 4], F32, out=True)
        S.dma("sp", dd_, ss_t, reads=Bss, slot=S.buf(), out_final=True)
    if KCUT == 4:
        S.dma("act", out_d[0:128, :], nfin, reads=[Bnf], slot=Bnf, out_final=True)
        S.scope_pop(); S.scope_pop()
        S.finish()
        return nc
    if dbg:
        S.dma("act", dbg_d["s0"].rearrange("s p v -> p s v"), S32.bitcast(F32)[:, 0, :, :], reads=BS32, slot=BS32[0], out_final=True)
    S.scope_pop()
    if stage == 1:
        S.dma("act", out_d[0:128, :], nfin, reads=[Bnf], slot=Bnf, out_final=True)
        S.finish()
        return nc
    return _build_rest(nc, S, locals(), stage, dbg)


def _host_layouts(inputs):
    f = lambda a: np.ascontiguousarray(np.asarray(a, dtype=np.float32))
    fm = lambda v: f(np.asarray(v).reshape(-1, 128).T)
    sh = {}
    sh["w_ada"] = f(inputs["w_ada"][0]); sh["b_ada"] = f(inputs["b_ada"][0]).reshape(1, 6144)
    sh["nmix_fm"] = fm(inputs["norm_mix"][0])
    sh["nffn_bc"] = f(np.broadcast_to(np.asarray(inputs["norm_ffn"][0])[None, :], (128, 1024)))
    sh["nfin_bc"] = f(np.broadcast_to(np.asarray(inputs["norm_final"])[None, :], (128, 1024)))
    sh["w_in"] = f(inputs["w_in"][0])
    lbl = np.asarray(inputs["lb_logits"], dtype=np.float32)
    sh["lbl_fm"] = f(np.concatenate([fm(lbl[0].reshape(-1)), fm(lbl[1].reshape(-1))], axis=1))
    sh["lbl_bc"] = f(np.broadcast_to(lbl.reshape(1, 2048), (128, 2048)))
    sh["hgn_fm"] = f(np.asarray(inputs["hg_norm"][0]).reshape(128, 1))
    cw = np.asarray(inputs["conv_w"][0], dtype=np.float32)
    sh["convw_fm"] = f(cw.reshape(3, 4, 128).transpose(2, 1, 0).reshape(128, 12))
    sh["w_out"] = f(inputs["w_out"][0])
    wr = np.asarray(inputs["w_router"][0], dtype=np.float32)
    sh["w_r_fm"] = f(wr.reshape(8, 128, 16).transpose(1, 0, 2).reshape(128, 128))
    sh["w_gate"] = f(inputs["w_gate"][0]); sh["w_up"] = f(inputs["w_up"][0]); sh["w_down"] = f(inputs["w_down"][0])
    sh["consts"] = make_consts()
    return sh


def _core_inputs(inputs, sh, b):
    f = lambda a: np.ascontiguousarray(np.asarray(a, dtype=np.float32))
    m = dict(sh)
    m["x"] = f(inputs["x"][b]); m["ctx"] = f(inputs["ctx"][b])
    cc = np.stack([np.asarray(inputs["c"][b]).reshape(8, 128).T, np.asarray(inputs["c_ctx"]).reshape(8, 128).T],
                  axis=-1)
    m["ccT"] = f(cc.reshape(128, 16))
    return m


_PROG = {}


def kernel(**inputs):
    if "full" not in _PROG:
        _PROG["full"] = build_program()
    nc = _PROG["full"]
    sh = _host_layouts(inputs)
    in_maps = [_core_inputs(inputs, sh, b) for b in range(8)]
    if not HAS_MOE:
        for m in in_maps:
            for k in ("w_gate", "w_up", "w_down"):
                m.pop(k, None)
    res = run_bass_kernel_spmd(nc, in_maps, core_ids=list(range(8)))
    return np.stack([np.asarray(r["out"], dtype=np.float32) for r in res.results], axis=0)


def _build_rest(nc, S, L, stage, dbg):
    g = lambda n: L[n]
    (act, tt, ts, stt, cp, mm, trs, bank, C, wslot, prep_tile, rstd_of) = [g(n) for n in (
        "act", "tt", "ts", "stt", "cp", "mm", "trs", "bank", "C", "wslot", "prep_tile", "rstd_of")]
    (x_d, out_d, x2_d, hx2_d, win_v, wout_d, dbg_d, st_of) = [g(n) for n in (
        "x_d", "out_d", "x2_d", "hx2_d", "win_v", "wout_d", "dbg_d", "st_of")]
    (ident, identb, onesr, Bc, Bib, Bor, lbfm, omlfm, nomlfm, Bl, S32, S16, BS32, BS16, sver, A1, B1, Bab,
     g1bc, A2bc, B2bc, g2bc, Bbc, hxT, BhxT, xsl, xn, Bxn, junk, Bjunk, hgn, Bhg, convw, Bcw, wr, Bwr, nfin, Bnf,
     sc_ps, Bsc, kv_ps, Bkv, o_ps, Bo, trb_ps, Btrb) = [g(n) for n in (
         "ident", "identb", "onesr", "Bc", "Bib", "Bor", "lbfm", "omlfm", "nomlfm", "Bl", "S32", "S16", "BS32",
         "BS16", "sver", "A1", "B1", "Bab", "g1bc", "A2bc", "B2bc", "g2bc", "Bbc", "hxT", "BhxT", "xsl", "xn",
         "Bxn", "junk", "Bjunk", "hgn", "Bhg", "convw", "Bcw", "wr", "Bwr", "nfin", "Bnf",
         "sc_ps", "Bsc", "kv_ps", "Bkv", "o_ps", "Bo", "trb_ps", "Btrb")]
    aff_d = nc.dram_tensor("aff_scr", [16, T_SEQ], F32, kind=("ExternalOutput" if dbg else "Internal")).ap()
    wout_v = wout_d.rearrange("(k p) n -> p k n", p=128)
    xcnt = [0]

    S.scope_push()
    TT4 = S.sbs("TT4", [128, 4, 512], F32)
    Ta, Tb, Tc, Td = TT4[:, 0, :], TT4[:, 1, :], TT4[:, 2, :], TT4[:, 3, :]
    BTa, BTb, BTc, BTd = S.buf(), S.buf(), S.buf(), S.buf()
    kend16 = S.sbs("kend16", [128, 512], F32); Bke16 = S.buf()
    Eall = S.sbs("Eall", [128, 4, 512], F32); BE = [S.buf() for _ in range(4)]
    QD = S.sbs("QD", [128, 4, 512], F32R); BQD = [S.buf() for _ in range(4)]
    KI = S.sbs("KI", [128, 4, 512], F32R); BKI = [S.buf() for _ in range(4)]
    KE = S.sbs("KE", [128, 4, 4, 128], F32R); BKE = [S.buf() for _ in range(4)]
    EL = S.sbs("EL", [128, 4, 16], F32); BEL = [S.buf() for _ in range(4)]
    V16 = S.sbs("V16", [128, 4, 512], F32R); BV = [S.buf() for _ in range(4)]
    VMs = [(S.sbs(f"VM{i}", [128, 512], F32R), S.buf(f"VM{i}")) for i in range(2)]
    vmn = [0]
    AT16 = S.sbs("AT16", [128, 512], F32R); BAT = S.buf()
    OB = S.sbs("OB", [128, 4, 512], F32); BOB = [S.buf() for _ in range(4)]
    SG = S.sbs("SG", [128, 4, 512], BF16); BSG = [S.buf() for _ in range(4)]
    cvs = S.sbs("cvs", [128, 4, 512], F32); Bcvs = [S.buf() for _ in range(4)]
    MIXT = S.sbs("MIXT", [128, 8, 512], F32R); BMX = [S.buf() for _ in range(8)]
    MIXTf = MIXT.bitcast(F32)
    hx2T = S.sbs("hx2T", [128, 8, 128], F32); Bh2T = S.buf()
    osq = S.sbs("osq", [128, 512], F32R); Bosq = S.buf()
    affsb = S.sbs("affsb", [16, 128], F32); Baf = S.buf()
    eT = S.sbs("eT", [16, 128], F32); BeT = S.buf()
    Bstash = [S.buf(f"stash{i}") for i in range(8)]
    eT2 = S.sbs("eT2", [16, 128], F32); BeT2 = S.buf()
    affsb2 = S.sbs("affsb2", [16, 128], F32); Baf2 = S.buf()
    epn = [0]
    xnb = [(xn, [Bxn]), (TT4[:, 0:2, :].rearrange("p a b -> p (a b)"), [BTa, BTb])]
    jkb = [(junk, [Bjunk]), (TT4[:, 2:4, :].rearrange("p a b -> p (a b)"), [BTc, BTd])]
    h2b = [(hx2T, [Bh2T]), (cvs[:, 0:2, :].rearrange("p a (k t) -> p (a k) t", t=128), [Bcvs[0], Bcvs[1]])]
    smb = [(eT, BeT, affsb, Baf), (eT2, BeT2, affsb2, Baf2)]

    def gate_prep(d, h, ps, Bp):
        col = d * 4 + h
        act(Ta, ps, AF.Sigmoid, [Bp], [BTa])
        act(Tb, Ta, AF.Ln, [BTa, Bl], [BTb], scale=omlfm[:, col:col + 1], bias=lbfm[:, col:col + 1])
        ts("dve", Tc, Ta, nomlfm[:, col:col + 1], omlfm[:, col:col + 1], ALU.mult, ALU.add, [BTa, Bl], [BTc])
        if d == 0:
            S.op("dve", lambda e: e.tensor_tensor_scan(Td, C("rsf"), Tb, 0.0, ALU.mult, ALU.add),
                 reads=[BTb, Bc], writes=[BTd])
        else:
            S.op("dve", lambda e: e.tensor_tensor_scan(Td[:, ::-1], C("rsb")[:, ::-1], Tb[:, ::-1], 0.0,
                                                       ALU.mult, ALU.add), reads=[BTb, Bc], writes=[BTd])
        act(Eall[:, h, :], Td, AF.Exp, [BTd], [BE[h]])
        act(Ta, Td, AF.Exp, [BTd], [BTa], scale=-1.0)
        tt("dve", Tc, Tc, Ta, ALU.mult, [BTc, BTa], [BTc])
        cp("act", KI[:, h, :], Tc, [BTc], [BKI[h]])
        E3 = Eall[:, h, :].rearrange("p (c k) -> p c k", k=32)
        cp("pool", EL[:, h, :], E3[:, :, 31] if d == 0 else E3[:, :, 0], [BE[h]], [BEL[h]])
        elb = EL[:, h, :].rearrange("p (c o) -> p c o", o=1).to_broadcast([128, 16, 32])
        tt("dve", kend16.rearrange("p (c k) -> p c k", k=32), Tc.rearrange("p (c k) -> p c k", k=32), elb,
           ALU.mult, [BTc, BEL[h]], [Bke16])
        trs([(trb_ps[:, t * 128:(t + 1) * 128], kend16[:, t * 128:(t + 1) * 128], ident) for t in range(4)],
            [Bke16, Bc], [Btrb])
        cp("act", KE[:, h, :, :].rearrange("p t d -> p (t d)"), trb_ps[:, 0:512], [Btrb], [BKE[h]])

    kvb = [(kv_ps, Bkv), (trb_ps, Btrb)]
    BS32b = L["BS32b"]; stver = L["stver"]
    S32f = S32.bitcast(F32)
    V16f = V16.bitcast(F32)

    def gla_group(d):
        order = list(range(4)) if d == 0 else [3, 2, 1, 0]
        mk = (C("maskf") if d == 0 else C("maskb")).rearrange("p (o q) -> p o q", o=1).to_broadcast([128, 4, 128])
        hsl = [slice(h * 128, (h + 1) * 128) for h in range(4)]
        for t in order:
            tcol = slice(t * 128, (t + 1) * 128)
            mm([(sc_ps[:, hsl[h]], KI[:, h, tcol], QD[:, h, tcol], True, True) for h in range(4)], BKI + BQD, [Bsc])
            tt("dve", AT16.rearrange("p (t q) -> p t q", q=128), sc_ps.rearrange("p (t q) -> p t q", q=128), mk, ALU.mult,
               [Bsc, Bc], [BAT])
            mm([(o_ps[:, hsl[h]], V16[:, t, hsl[h]], AT16[:, hsl[h]], h == 0, False) for h in range(4)], [BV[t], BAT], [Bo])
            for ci, cc in enumerate(order):
                VMb, BVMb = VMs[vmn[0] % 2]
                kvp, Bkvp = kvb[vmn[0] % 2]
                vmn[0] += 1
                act(VMb, V16f[:, t, :], AF.Copy, [BV[t], Bc], [BVMb], scale=C("cind")[:, cc:cc + 1])
                mm([(kvp[:, hsl[h]], KE[:, h, t, :], VMb[:, hsl[h]], True, True) for h in range(4)], BKE + [BVMb], [Bkvp])
                v = stver[d]
                Bcur = [BS32b[v][d * 4 + h] for h in range(4)]
                Bnxt = [BS32b[1 - v][d * 4 + h] for h in range(4)]
                mm([(o_ps[:, h * 128 + cc * 32: h * 128 + cc * 32 + 32], S32[:, v, d * 4 + h, :],
                     QD[:, h, t * 128 + cc * 32: t * 128 + cc * 32 + 32], False, ci == 3) for h in range(4)],
                   Bcur + BQD, [Bo])
                chunk = t * 4 + cc

                def upd(e, kvp=kvp, chunk=chunk, v=v):
                    ins = None
                    for h in range(4):
                        col = d * 4 + h
                        ins = e.scalar_tensor_tensor(S32[:, 1 - v, col, :], S32f[:, v, col, :], EL[:, h, chunk:chunk + 1],
                                                     kvp[:, hsl[h]], ALU.mult, ALU.add)
                    return ins
                S.op("dve", upd, reads=Bcur + BEL + [Bkvp], writes=Bnxt)
                stver[d] = 1 - v
            o3 = o_ps.rearrange("p (h q) -> p h q", q=128)
            if d == 1:
                cp("act", OB[:, :, tcol], o3, [Bo], BOB)
            else:
                tt("dve", OB[:, :, tcol], o3, OB[:, :, tcol], ALU.add, [Bo] + BOB, BOB)

    def sweep(d):
        groups = range(8) if d == 0 else range(7, -1, -1)
        pieces = [1, 0, 3, 4, 7, 6, 5] if d == 0 else [2, 0, 3]
        for gi in groups:
            for t in range(4):
                r0 = (gi * 4 + t) * 128
                prep_tile(x_d[r0:r0 + 128, :], A1, B1, t * 128, xcnt[0])
                xcnt[0] += 1
            if d == 0:
                S.dma("sp", OB, st_of[gi], reads=[Bstash[gi]], writes=BOB, slot=BOB[0])
            for j in pieces:
                wt, Bw = wslot()
                S.dma("sp", wt, win_v[:, :, j * 512:(j + 1) * 512], writes=[Bw])
                for q in range(4):
                    ps, Bp = bank()
                    if j == 3:
                        mm([(ps, hxT[:, k, q * 128:(q + 1) * 128], wt[:, k, :], k == 0, k == 7) for k in range(8)],
                           [BhxT, Bw], [Bp])
                        cp("act", V16[:, q, :], ps, [Bp], [BV[q]])
                        continue
                    mm([(ps, wt[:, k, q * 128:(q + 1) * 128], hxT[:, k, :], k == 0, k == 7) for k in range(8)],
                       [BhxT, Bw], [Bp])
                    if j in (1, 2):
                        gate_prep(d, q, ps, Bp)
                    elif j == 0:
                        tt("dve", QD[:, q, :], ps, Eall[:, q, :], ALU.mult, [Bp, BE[q]], [BQD[q]])
                    elif j == 4:
                        act(SG[:, q, :], ps, AF.Silu, [Bp], [BSG[q]])
                    elif j == 7:
                        cp("act", cvs[:, q, :], ps, [Bp], [Bcvs[q]])
                    elif j == 6:
                        u = cvs[:, q, :]
                        y = Eall[:, q, :]
                        tt("dve", u, ps, u, ALU.mult, [Bp, Bcvs[q]], [Bcvs[q]])
                        ts("dve", y, u, convw[:, q * 3 + 1:q * 3 + 2], None, ALU.mult, None, [Bcvs[q], Bcw], [BE[q]])
                        u3 = u.rearrange("p (r w) -> p r w", w=64); y3 = y.rearrange("p (r w) -> p r w", w=64)
                        stt(y3[:, :, 1:64], u3[:, :, 0:63], convw[:, q * 3:q * 3 + 1], y3[:, :, 1:64], ALU.mult, ALU.add,
                            [Bcvs[q], Bcw, BE[q]], [BE[q]])
                        stt(y3[:, :, 0:63], u3[:, :, 1:64], convw[:, q * 3 + 2:q * 3 + 3], y3[:, :, 0:63], ALU.mult,
                            ALU.add, [Bcvs[q], Bcw, BE[q]], [BE[q]])
                    elif j == 5:
                        tt("dve", MIXT[:, 4 + q, :], ps, Eall[:, q, :], ALU.mult, [Bp, BE[q]], [BMX[4 + q]])
            gla_group(d)
            if d == 0:
                for h in range(4):
                    osum = OB[:, h, :]
                    act(osq, osum, AF.Square, [BOB[h]], [Bosq])
                    ps, Bp = bank()
                    mm([(ps, onesr, osq, True, True)], [Bosq, Bor], [Bp])
                    ts("dve", Td, ps, 1.0 / 128.0, EPS, ALU.mult, ALU.add, [Bp], [BTd])
                    act(Td, Td, AF.Sqrt, [BTd], [BTd])
                    S.op("dve", lambda e: e.reciprocal(Td, Td), reads=[BTd], writes=[BTd])
                    tt("dve", Tb, osum, Td, ALU.mult, [BOB[h], BTd], [BTb])
                    stt(MIXT[:, h, :], Tb, hgn[:, 0:1], SG[:, h, :], ALU.mult, ALU.mult, [BTb, Bhg, BSG[h]], [BMX[h]])
            if d == 1:
                S.dma("act", st_of[gi], OB, reads=BOB, writes=[Bstash[gi]], slot=BOB[0])
                if dbg:
                    S.dma("act", dbg_d["of"][gi], OB, reads=BOB, slot=BOB[0], out_final=True)
                continue
            wo = []
            for hf in range(2):
                wt, Bw = wslot()
                S.dma("sp", wt, wout_v[:, :, hf * 512:(hf + 1) * 512], writes=[Bw])
                wo.append((wt, Bw))
            for t in range(4):
                r0 = (gi * 4 + t) * 128
                xt, Bx = xsl[xcnt[0] % 2]
                xcnt[0] += 1
                par = epn[0] % 2
                epn[0] += 1
                x1, Bx1 = xnb[par]
                hx, Bhx = jkb[par]
                h2T, Bh2 = h2b[par]
                eTt, BeTt, aft, Baft = smb[par]
                S.dma("sp", xt, x_d[r0:r0 + 128, :], writes=[Bx])
                for hf in range(2):
                    ps, Bp = bank()
                    wt, Bw = wo[hf]
                    mm([(ps, MIXT[:, kc, t * 128:(t + 1) * 128], wt[:, kc, :], kc == 0, kc == 7) for kc in range(8)],
                       BMX + [Bw], [Bp])
                    hsl = slice(hf * 512, (hf + 1) * 512)
                    tt("dve", x1[:, hsl], ps, g1bc[:, hsl], ALU.mult, [Bp, Bbc], Bx1)
                    tt("pool", x1[:, hsl], x1[:, hsl], xt[:, hsl], ALU.add, Bx1 + [Bx], Bx1)
                S.dma("act", x2_d[r0:r0 + 128, :], x1, reads=Bx1, slot=Bx1[0], out_final=dbg)
                rs, Brs = rstd_of(x1, Bx1, hx, Bhx)
                stt(hx, x1, rs, A2bc, ALU.mult, ALU.mult, Bx1 + [Brs, Bbc], Bhx)
                tt("pool", hx, hx, B2bc, ALU.add, Bhx + [Bbc], Bhx)
                S.dma("act", hx2_d[r0:r0 + 128, :], hx, reads=Bhx, slot=Bhx[0], out_final=dbg)
                for half in range(2):
                    ps, Bp = bank()
                    trs([(ps[:, q * 128:(q + 1) * 128], hx[:, (half * 4 + q) * 128:(half * 4 + q + 1) * 128], ident)
                         for q in range(4)], Bhx + [Bc], [Bp])
                    cp("act", h2T[:, half * 4:(half + 1) * 4, :].rearrange("p k t -> p (k t)"), ps, [Bp], Bh2)
                ps, Bp = bank()
                mm([(ps[0:16, 0:128], wr[:, k * 16:(k + 1) * 16], h2T[:, k, :], k == 0, k == 7) for k in range(8)],
                   [Bwr] + Bh2, [Bp])
                act(eTt, ps[0:16, 0:128], AF.Exp, [Bp], [BeTt])
                ps2, Bp2 = bank()
                mm([(ps2[0:16, 0:128], C("ones", rows=16, hi=16), eTt, True, True)], [BeTt, Bc], [Bp2])
                S.op("dve", lambda e, ps2=ps2, aft=aft: e.reciprocal(aft, ps2[0:16, 0:128]), reads=[Bp2], writes=[Baft])
                tt("dve", aft, aft, eTt, ALU.mult, [Baft, BeTt], [Baft])
                S.dma("act", aff_d[:, r0:r0 + 128], aft, reads=[Baft], slot=Baft, out_final=dbg)

    sweep(1)
    if stage == 2:
        S.dma("act", out_d[0:128, :], nfin, reads=[Bnf], slot=Bnf, out_final=True)
        S.scope_pop(); S.scope_pop()
        S.finish()
        return nc
    sweep(0)
    S.scope_pop()
    S.scope_pop()
    if stage == 3:
        S.dma("act", out_d[0:128, :], nfin, reads=[Bnf], slot=Bnf, out_final=True)
        S.finish()
        return nc
    return _build_moe(nc, S, L, locals(), stage, dbg)


def _build_moe(nc, S, L, L2, stage, dbg):
    import concourse.bass as bass_
    act, tt, ts, stt, cp, mm, trs, C = [L[n] for n in ("act", "tt", "ts", "stt", "cp", "mm", "trs", "C")]
    x2_d, hx2_d, out_d, nfin, Bnf, g2bc, Bbc, Bc, ident, ipb = [L[n] for n in (
        "x2_d", "hx2_d", "out_d", "nfin", "Bnf", "g2bc", "Bbc", "Bc", "ident", "ipb")]
    wg_d, wu_d, wd_d = L["wg_d"], L["wu_d"], L["wd_d"]
    aff_d = L2["aff_d"]
    yacc = [(L["sc_ps"], L["Bsc"]), (L["kv_ps"], L["Bkv"]), (L["o_ps"], L["Bo"]), (L["trb_ps"], L["Btrb"])]
    R_ps, BR = ipb[3]
    rot = ipb[0:3]
    rn = [0]

    def bank3():
        r = rot[rn[0] % 3]
        rn[0] += 1
        return r
    Bx2all = S.buf("x2all")
    Bhx2all = S.buf("hx2all")

    S.scope_push()
    slotT = S.sbs("slotT", [128, 4, 128], F32); BslT = S.buf()
    affT = S.sbs("affT", [128, 4, 128], F32); BafT = S.buf()
    vals = S.sbs("vals", [128, 4, 128, 3], F32R); Bvals = S.buf()
    Rsb = S.sbs("Rsb", [3, 512], F32); BRsb = S.buf()
    IG = S.sbs("IG", [128, 12], F32); BIG = S.buf()
    idxs = [(S.sbs(f"idx{i}", [128, 4], I32), S.buf(f"idx{i}")) for i in range(2)]
    gates = [(S.sbs(f"gate{i}", [128, 4], F32), S.buf(f"gate{i}")) for i in range(2)]
    sm = S.sbs("smalls", [128, 16], F32); Bsm = S.buf()
    lo, hi, mid, cnt, ge, dl, nge, off = [sm[:, i:i + 1] for i in range(8)]
    S.scope_push()
    aff128 = S.sbs("aff128", [128, 512], F32); Ba128 = S.buf()
    msk = S.sbs("msk", [128, 512], F32); Bmsk = S.buf()
    cum = S.sbs("cum", [128, 512], F32); Bcum = S.buf()

    S.dma("sp", aff128, aff_d.rearrange("e (s t) -> (e s) t", t=512), writes=[Ba128])
    S.op("dve", lambda e: e.memset(sm, 0.0), writes=[Bsm])
    S.op("dve", lambda e: e.memset(hi, 2.0), reads=[Bsm], writes=[Bsm])
    for it in range(34):
        tt("dve", mid, lo, hi, ALU.add, [Bsm], [Bsm])
        ts("dve", mid, mid, 0.5, None, ALU.mult, None, [Bsm], [Bsm])
        ts("dve", msk, aff128, mid, None, ALU.is_ge, None, [Ba128, Bsm], [Bmsk])
        S.op("dve", lambda e: e.reduce_sum(cnt, msk, AX.X), reads=[Bmsk], writes=[Bsm])
        ps, Bp = bank3()
        mm([(ps[:, 0:1], C("bones"), cnt, True, True)], [Bc, Bsm], [Bp])
        ts("dve", ge, ps[:, 0:1], float(CAP), None, ALU.is_ge, None, [Bp], [Bsm])
        tt("dve", dl, mid, lo, ALU.subtract, [Bsm], [Bsm])
        stt(lo, dl, ge, lo, ALU.mult, ALU.add, [Bsm], [Bsm])
        tt("dve", dl, hi, mid, ALU.subtract, [Bsm], [Bsm])
        stt(hi, dl, ge, mid, ALU.mult, ALU.add, [Bsm], [Bsm])
    import os
    MOECUT = int(os.environ.get("MOECUT", "0"))
    MOEN = int(os.environ.get("MOEN", str(N_EXP)))
    ones512 = S.sbs("ones512", [128, 512], F32); Bo512 = S.buf()
    S.op("pool", lambda e: e.memset(ones512, 1.0), writes=[Bo512])
    ts("dve", msk, aff128, lo, None, ALU.is_ge, None, [Ba128, Bsm], [Bmsk])
    S.op("dve", lambda e: e.reduce_sum(cnt, msk, AX.X), reads=[Bmsk], writes=[Bsm])
    ps, Bp = bank3()
    mm([(ps[:, 0:1], C("segpre"), cnt, True, True)], [Bc, Bsm], [Bp])
    cp("dve", off, ps[:, 0:1], [Bp], [Bsm])
    S.op("dve", lambda e: e.tensor_tensor_scan(cum, ones512, msk, off, ALU.mult, ALU.add),
         reads=[Bmsk, Bsm, Bo512], writes=[Bcum])
    tt("dve", cum, cum, msk, ALU.mult, [Bcum, Bmsk], [Bcum])
    ts("dve", cum, cum, -1.0, None, ALU.add, None, [Bcum], [Bcum])
    for (src, Bsrc, dst, Bdst) in ((cum, Bcum, slotT, BslT), (aff128, Ba128, affT, BafT)):
        ps, Bp = bank3()
        trs([(ps[:, c * 128:(c + 1) * 128], src[:, c * 128:(c + 1) * 128], ident) for c in range(4)], [Bsrc, Bc], [Bp])
        cp("act", dst.rearrange("p c q -> p (c q)"), ps, [Bp], [Bdst])
    valsf = vals.bitcast(F32)
    cp("dve", vals[:, :, :, 1], affT, [BafT], [Bvals])
    tt("dve", vals[:, :, :, 2], affT, valsf[:, :, :, 1], ALU.subtract, [BafT, Bvals], [Bvals])
    tki = C("tokidx").rearrange("p (c o s) -> p c o s", c=4, o=1)
    for c in range(4):
        cp("dve", vals[:, c, :, 0].rearrange("p (e s) -> p e s", s=8), tki[:, c, :, :].to_broadcast([128, 16, 8]),
           [Bc], [Bvals])

    S.scope_pop()
    S.scope_push()
    wsl = [(S.sbs(f"mw{i}", [128, 8, 512], F32R), S.buf(f"mw{i}")) for i in range(3)]
    wn = [0]

    def wslot():
        r = wsl[wn[0] % 3]
        wn[0] += 1
        return r
    xsT = [(S.sbs(f"xsT{i}", [128, 8, 512], F32R), S.buf(f"xsT{i}")) for i in range(2)]
    hidT = S.sbs("hidT", [128, 16, 512], F32R); Bhid = [S.buf() for _ in range(16)]
    xstok = [(S.sbs(f"xstok{i}", [128, 1024], F32), S.buf(f"xstok{i}")) for i in range(2)]
    ysb = [(S.sbs(f"ysb{i}", [128, 1024], F32), S.buf(f"ysb{i}")) for i in range(4)]
    sgt = [(S.sbs(f"sgt{i}", [128, 512], F32), S.buf(f"sgt{i}")) for i in range(2)]
    Qb = [(S.sbs(f"Qb{i}", [128, 512], F32R), S.buf(f"Qb{i}")) for i in range(3)]

    if dbg:
        dbg_lo = nc.dram_tensor("dbg_lo", [128, 16], F32, kind="ExternalOutput").ap()
        dbg_idx = nc.dram_tensor("dbg_idx", [16, 128, 4], I32, kind="ExternalOutput").ap()
        dbg_gate = nc.dram_tensor("dbg_gate", [16, 128, 4], F32, kind="ExternalOutput").ap()
        dbg_slot = nc.dram_tensor("dbg_slot", [128, 512], F32, kind="ExternalOutput").ap()
        S.dma("sp", dbg_lo, sm, reads=[Bsm], slot=S.buf(), out_final=True)
        S.dma("sp", dbg_slot, slotT.rearrange("p c q -> p (c q)"), reads=[BslT], slot=S.buf(), out_final=True)

    def prep(e):
        xT, BxT = xsT[e % 2]
        idx, Bidx = idxs[e % 2]
        gat, Bgat = gates[e % 2]
        for i in range(32):
            s_, c = i // 4, i % 4
            col = e * 8 + s_
            Q, BQ = Qb[i % 3]
            ts("dve", Q, C("iota"), slotT[:, c, col:col + 1], None, ALU.is_equal, None,
               [Bc, BslT], [BQ])
            mm([(R_ps[0:3, :], vals[:, c, col, :], Q, i == 0, i == 31)], [Bvals, BQ], [BR])
            yield
        cp("act", Rsb, R_ps[0:3, :], [BR], [BRsb])
        ps, Bp = bank3()
        trs([(ps[:, b * 3:(b + 1) * 3], Rsb[0:3, b * 128:(b + 1) * 128], ident[0:3, 0:3]) for b in range(4)],
            [BRsb, Bc], [Bp])
        cp("dve", IG, ps[:, 0:12], [Bp], [BIG])
        IG3 = IG.rearrange("p (b j) -> p b j", j=3)
        cp("dve", idx, IG3[:, :, 0], [BIG], [Bidx])
        tt("dve", gat, IG3[:, :, 1], IG3[:, :, 2], ALU.add, [BIG], [Bgat])
        if dbg:
            S.dma("sp", dbg_idx[e], idx, reads=[Bidx], slot=S.buf(), out_final=True)
            S.dma("sp", dbg_gate[e], gat, reads=[Bgat], slot=S.buf(), out_final=True)
        yield
        for b in range(4):
            xt, Bx = xstok[b % 2]
            S.op_dma_ind("pool", lambda en, xt=xt, b=b: en.indirect_dma_start(
                out=xt, out_offset=None, in_=hx2_d, in_offset=bass_.IndirectOffsetOnAxis(ap=idx[:, b:b + 1], axis=0)),
                reads=[Bidx, Bhx2all], writes=[Bx])
            for half in range(2):
                ps, Bp = bank3()
                trs([(ps[:, q * 128:(q + 1) * 128], xt[:, (half * 4 + q) * 128:(half * 4 + q + 1) * 128], ident)
                     for q in range(4)], [Bx, Bc], [Bp])
                for q in range(4):
                    cp("act", xT[:, half * 4 + q, b * 128:(b + 1) * 128], ps[:, q * 128:(q + 1) * 128], [Bp], [BxT])
            yield

    def drain(gen):
        for _ in gen:
            pass

    def tick(gen):
        if gen is not None:
            next(gen, None)

    if MOECUT != 2:
        drain(prep(0))
    for e in range(MOEN if MOECUT not in (2, 3) else 0):
        gen = prep(e + 1) if e + 1 < MOEN else None
        xT, BxT = xsT[e % 2]
        idx, Bidx = idxs[e % 2]
        gat, Bgat = gates[e % 2]
        wgv = wg_d[e].rearrange("(k p) f -> p k f", p=128)
        wuv = wu_d[e].rearrange("(k p) f -> p k f", p=128)
        wdv = wd_d[e].rearrange("(g c p) d -> g p c d", p=128, c=4)
        for fg in range(4):
            wgt, Bwg = wslot()
            S.dma("sp", wgt, wgv[:, :, fg * 512:(fg + 1) * 512], writes=[Bwg])
            wut, Bwu = wslot()
            S.dma("sp", wut, wuv[:, :, fg * 512:(fg + 1) * 512], writes=[Bwu])
            for fc in range(4):
                f = fg * 4 + fc
                sg, Bsg = sgt[f % 2]
                ps, Bp = bank3()
                mm([(ps, wgt[:, k, fc * 128:(fc + 1) * 128], xT[:, k, :], k == 0, k == 7) for k in range(8)],
                   [Bwg, BxT], [Bp])
                act(sg, ps, AF.Silu, [Bp], [Bsg])
                tick(gen)
                ps2, Bp2 = bank3()
                mm([(ps2, wut[:, k, fc * 128:(fc + 1) * 128], xT[:, k, :], k == 0, k == 7) for k in range(8)],
                   [Bwu, BxT], [Bp2])
                tt("dve", hidT[:, f, :], ps2, sg, ALU.mult, [Bp2, Bsg], [Bhid[f]])
                tick(gen)
        for dh in range(2):
            for fg in range(4):
                wt, Bw = wslot()
                wt4 = wt.rearrange("p (a c) d -> p a c d", a=2)[:, 0, :, :]
                S.dma("sp", wt4, wdv[fg][:, :, dh * 512:(dh + 1) * 512], writes=[Bw])
                for t4 in range(4):
                    ya, Bya = yacc[t4]
                    mm([(ya, hidT[:, fg * 4 + fc, t4 * 128:(t4 + 1) * 128], wt4[:, fc, :], fg == 0 and fc == 0,
                         fg == 3 and fc == 3) for fc in range(4)], [Bhid[fg * 4 + fc] for fc in range(4)] + [Bw], [Bya])
                    tick(gen)
            for t4 in range(4):
                ya, Bya = yacc[t4]
                yt, By = ysb[t4]
                hsl = slice(dh * 512, (dh + 1) * 512)
                stt(yt[:, hsl], ya, gat[:, t4:t4 + 1], g2bc[:, hsl], ALU.mult, ALU.mult, [Bya, Bgat, Bbc], [By])
        for t4 in range(4):
            yt, By = ysb[t4]
            S.op_dma_ind("pool", lambda en, yt=yt, t4=t4, idx=idx: en.indirect_dma_start(
                out=x2_d, out_offset=bass_.IndirectOffsetOnAxis(ap=idx[:, t4:t4 + 1], axis=0), in_=yt, in_offset=None,
                compute_op=ALU.add), reads=[Bidx, By, Bx2all], writes=[Bx2all], slot=By)
        if gen is not None:
            drain(gen)
    S.scope_pop()
    S.scope_pop()

    S.scope_push()
    xs = [(S.sbs(f"fx{i}", [128, 1024], F32), S.buf()) for i in range(2)]
    ys = [(S.sbs(f"fy{i}", [128, 1024], F32), S.buf()) for i in range(2)]
    jk = S.sbs("fjunk", [128, 1024], F32); Bjk = S.buf()
    ss = S.sbs("fss", [128, 4], F32); Bs4 = [S.buf() for _ in range(4)]
    for t in range(32):
        xt, Bx = xs[t % 2]; yt, By = ys[t % 2]
        S.dma("sp", xt, x2_d[t * 128:(t + 1) * 128, :], reads=[Bx2all], writes=[Bx])
        col = ss[:, t % 4:t % 4 + 1]; Bcol = Bs4[t % 4]
        act(jk, xt, AF.Square, [Bx], [Bjk])
        S.op("dve", lambda en, col=col: en.reduce_sum(col, jk, AX.X), reads=[Bjk], writes=[Bcol])
        ts("dve", col, col, 1.0 / D_MODEL, EPS, ALU.mult, ALU.add, [Bcol], [Bcol])
        act(col, col, AF.Sqrt, [Bcol], [Bcol])
        S.op("dve", lambda en, col=col: en.reciprocal(col, col), reads=[Bcol], writes=[Bcol])
        stt(yt, xt, col, nfin, ALU.mult, ALU.mult, [Bx, Bcol, Bnf], [By])
        S.dma("act", out_d[t * 128:(t + 1) * 128, :], yt, reads=[By], slot=By, out_final=True)
    S.scope_pop()
    S.finish()
    return nc
```

```python
import numpy as np
import concourse.bass as bass
import concourse.mybir as mybir
from concourse.bass_utils import run_bass_kernel_spmd

F32 = mybir.dt.float32
BF16 = mybir.dt.bfloat16
I32 = mybir.dt.int32
F32R = mybir.dt.float32r
AF = mybir.ActivationFunctionType
ALU = mybir.AluOpType
AX = mybir.AxisListType

ENGS = ("pe", "act", "dve", "pool", "sp")


class Counter:
    def __init__(self, nc, name, step, epoch):
        self.nc, self.name, self.step, self.epoch = nc, name, step, epoch
        self.n = 0
        self.sems = []

    def next(self):
        self.n += 1
        e = (self.n - 1) // self.epoch
        while len(self.sems) <= e:
            self.sems.append(self.nc.alloc_semaphore(f"{self.name}_e{len(self.sems)}"))
        return self.n

    def sem_of(self, n):
        e = (n - 1) // self.epoch
        return self.sems[e], (n - e * self.epoch) * self.step


class Buf:
    __slots__ = ("name", "w", "r", "ctr")

    def __init__(self, name=""):
        self.name = name
        self.w = {}
        self.r = {}
        self.ctr = None


class Sched:
    def __init__(self, nc):
        self.nc = nc
        self.q = {e: [] for e in ENGS}
        self.eng_ctr = {e: Counter(nc, f"tk_{e}", 1, 12000) for e in ("pe", "act", "dve", "pool")}
        self.waited = {e: {} for e in ENGS}
        self.finals = {}
        self.n_dma_ctr = 0
        self.nbuf = 0
        self.scopes = []
        self.all_dma_ctrs = []

    def sb(self, name, shape, dt):
        assert not self.scopes, "persistent alloc inside scope: " + name
        return self.nc.alloc_sbuf_tensor("sb_" + name, list(shape), dt).ap()

    def ps(self, name, shape, dt=F32):
        return self.nc.alloc_psum_tensor("ps_" + name, list(shape), dt).ap()

    def buf(self, name=None):
        self.nbuf += 1
        return Buf(name or f"b{self.nbuf}")

    def _ctr_for(self, b):
        if b.ctr is None:
            self.n_dma_ctr += 1
            b.ctr = Counter(self.nc, f"dq{self.n_dma_ctr}", 16, 900)
            self.all_dma_ctrs.append(b.ctr)
        return b.ctr

    def _deps(self, eng, reads, writes):
        need = {}

        def add(c, n, src_eng):
            if src_eng == eng and eng == "pe":
                return
            if need.get(c, 0) < n:
                need[c] = n

        for b in reads:
            for c, (n, se) in b.w.items():
                add(c, n, se)
        for b in writes:
            for c, (n, se) in b.w.items():
                if se == eng and se != "dma":
                    continue
                add(c, n, se)
            for c, (n, se) in b.r.items():
                if se == eng and se != "dma":
                    continue
                add(c, n, se)
        waits = []
        wd = self.waited[eng]
        for c, n in need.items():
            if wd.get(c, 0) >= n:
                continue
            wd[c] = n
            waits.append(c.sem_of(n))
        return waits

    def _record(self, c, n, src_eng, reads, writes):
        for b in reads:
            old = b.r.get(c)
            if old is None or old[0] < n:
                b.r[c] = (n, src_eng)
        for b in writes:
            b.w = {c: (n, src_eng)}
            b.r = {}

    def op(self, eng, fn, reads=(), writes=()):
        waits = self._deps(eng, reads, writes)
        c = self.eng_ctr[eng]
        n = c.next()
        sem, _ = c.sem_of(n)
        self._record(c, n, eng, reads, writes)

        def emit(e, waits=waits, fn=fn, sem=sem):
            for s, v in waits:
                e.wait_ge(s, v)
            ins = fn(e)
            ins.then_inc(sem, 1)
        self.q[eng].append(emit)

    def dma(self, qeng, out, in_, reads=(), writes=(), slot=None, out_final=False, **kw):
        self.op_dma_ind(qeng, lambda e: e.dma_start(out=out, in_=in_, **kw), reads=reads, writes=writes,
                        slot=slot, out_final=out_final)

    def op_dma_ind(self, qeng, fn, reads=(), writes=(), slot=None, out_final=False):
        if slot is None:
            slot = writes[0] if len(writes) else reads[0]
        waits = self._deps(qeng, reads, writes)
        c = self._ctr_for(slot)
        n = c.next()
        sem, _ = c.sem_of(n)
        self._record(c, n, "dma", reads, writes)
        if out_final:
            self.finals[c] = n

        def emit(e, waits=waits, fn=fn, sem=sem):
            for s, v in waits:
                e.wait_ge(s, v)
            fn(e).then_inc(sem, 16)
        self.q[qeng].append(emit)

    def scope_push(self):
        from contextlib import ExitStack
        st = ExitStack()
        self.scopes.append(st)

    def sbs(self, name, shape, dt):
        return self.scopes[-1].enter_context(self.nc.sbuf_tensor("sb_" + name, list(shape), dt)).ap()

    def scope_pop(self):
        self.barrier()
        self.scopes.pop().close()

    def barrier(self):
        ctrs = list(self.eng_ctr.values()) + self.all_dma_ctrs
        for eng in ENGS:
            waits = []
            wd = self.waited[eng]
            for c in ctrs:
                if c.n > 0 and wd.get(c, 0) < c.n:
                    wd[c] = c.n
                    waits.append(c.sem_of(c.n))

            def emit(e, waits=waits):
                for s_, v in waits:
                    e.wait_ge(s_, v)
            self.q[eng].append(emit)

    def finish(self):
        nc = self.nc
        finals = [c.sem_of(n) for c, n in self.finals.items()]
        q = self.q
        with nc.Block() as block:
            @block.sync
            def _(e):
                for f in q["sp"]:
                    f(e)
                for s, v in finals:
                    e.wait_ge(s, v)

            @block.tensor
            def _(e):
                for f in q["pe"]:
                    f(e)

            @block.scalar
            def _(e):
                for f in q["act"]:
                    f(e)

            @block.vector
            def _(e):
                for f in q["dve"]:
                    f(e)

            @block.gpsimd
            def _(e):
                for f in q["pool"]:
                    f(e)


def _const_layout():
    lay = {}
    off = 0
    for name, n in (("ident", 128), ("maskf", 128), ("maskb", 128), ("mgt", 128), ("mlt", 128), ("ones", 128),
                    ("rsf", 512), ("rsb", 512), ("cind", 4), ("iota", 512), ("tokab", 64), ("bones", 128),
                    ("wcomb", 2), ("segpre", 128), ("tokidx", 32)):
        lay[name] = (off, n)
        off += n
    return lay, off


CL, NCONST = _const_layout()


def make_consts():
    c = np.zeros((128, NCONST), np.float32)

    def put(name, arr):
        o, n = CL[name]
        c[:arr.shape[0], o:o + n] = arr
    p = np.arange(128)
    put("ident", np.eye(128, dtype=np.float32))
    j = p[:, None]; i = p[None, :]
    same = (j // 32) == (i // 32)
    mf = (same & (j <= i)).astype(np.float32)
    mb = (same & (j >= i)).astype(np.float32)
    put("maskf", mf); put("maskb", mb)
    put("mgt", (j > i).astype(np.float32))
    put("mlt", (j < i).astype(np.float32))
    put("ones", np.ones((128, 128), np.float32))
    t = np.arange(512)
    put("rsf", np.broadcast_to((t % 32 != 0).astype(np.float32), (128, 512)))
    put("rsb", np.broadcast_to((t % 32 != 31).astype(np.float32), (128, 512)))
    put("cind", (p[:, None] // 32 == np.arange(4)[None, :]).astype(np.float32))
    put("iota", np.broadcast_to(t.astype(np.float32), (128, 512)))
    tok = (np.arange(32)[None, :] * 128 + p[:, None])
    ab = np.stack([tok // 64, tok % 64], axis=-1).reshape(128, 64).astype(np.float32)
    put("tokab", ab)
    put("bones", ((p[:, None] // 8) == (p[None, :] // 8)).astype(np.float32))
    wc = np.zeros((128, 2), np.float32)
    wc[0, 0] = 64.0; wc[1, 0] = 1.0; wc[2, 1] = 1.0; wc[3, 1] = 1.0; wc[4, 1] = 1.0
    put("wcomb", wc)
    put("segpre", (((p[:, None] // 8) == (p[None, :] // 8)) & ((p[:, None] % 8) < (p[None, :] % 8))).astype(np.float32))
    cs = np.arange(32)
    put("tokidx", ((cs[None, :] % 8) * 512 + (cs[None, :] // 8) * 128 + p[:, None]).astype(np.float32))
    return c


T_SEQ, D_MODEL, N_EXP, CAP, DFF = 4096, 1024, 16, 512, 2048
HAS_MOE = True
EPS = 1e-6


def build_program(stage=99, dbg=False):
    nc = bass.Bass("TRN2", target_bir_lowering=False)
    nc.dge_precook = False
    S = Sched(nc)

    def din(name, shape, dt=F32):
        return nc.dram_tensor(name, list(shape), dt, kind="ExternalInput").ap()

    def dscr(name, shape, dt=F32, out=False):
        if out:
            return nc.dram_tensor(name, list(shape), dt, kind="ExternalOutput").ap()
        return nc.dram_tensor(name, list(shape), dt).ap()

    x_d = din("x", [T_SEQ, D_MODEL]); ctx_d = din("ctx", [256, D_MODEL]); cc_d = din("ccT", [128, 16])
    wada_d = din("w_ada", [1024, 6144], F32R); bada_d = din("b_ada", [1, 6144])
    nmix_d = din("nmix_fm", [128, 8]); nffn_d = din("nffn_bc", [128, 1024]); nfin_d = din("nfin_bc", [128, 1024])
    win_d = din("w_in", [1024, 4096], F32R)
    lblfm_d = din("lbl_fm", [128, 16]); lblbc_d = din("lbl_bc", [128, 2048])
    hgn_d = din("hgn_fm", [128, 1]); convw_d = din("convw_fm", [128, 12])
    wout_d = din("w_out", [1024, 1024], F32R); wr_d = din("w_r_fm", [128, 128])
    if stage >= 4 and HAS_MOE:
        wg_d = din("w_gate", [N_EXP, 1024, DFF], F32R); wu_d = din("w_up", [N_EXP, 1024, DFF], F32R)
        wd_d = din("w_down", [N_EXP, DFF, 1024], F32R)
    consts_d = din("consts", [128, NCONST])
    out_d = nc.dram_tensor("out", [T_SEQ, D_MODEL], F32, kind="ExternalOutput").ap()
    x2_d = dscr("x2", [T_SEQ, D_MODEL], F32, out=dbg)
    hx2_d = dscr("hx2", [T_SEQ, D_MODEL], F32, out=dbg)
    st_qd = dscr("st_qd", [8, 128, 4, 512], BF16); st_ki = dscr("st_ki", [8, 128, 4, 512], BF16)
    st_ke = dscr("st_ke", [8, 128, 4, 4, 128], BF16); st_v = dscr("st_v", [8, 128, 4, 512], BF16)
    st_el = dscr("st_el", [8, 128, 4, 16], F32); st_of = dscr("st_of", [8, 128, 4, 512], F32)
    st_sg = dscr("st_sg", [8, 128, 4, 512], BF16); st_yb = dscr("st_yb", [8, 128, 4, 512], F32R)
    dbg_d = {}
    if dbg:
        dbg_d["s0"] = dscr("dbg_s0", [8, 128, 128], F32, out=True)
        dbg_d["aff"] = dscr("dbg_aff", [16, T_SEQ], F32, out=True)
        dbg_d["of"] = dscr("dbg_of", [8, 128, 4, 512], F32, out=True)
        dbg_d["mod"] = dscr("dbg_mod", [2, 6144], F32, out=True)

    def act(out, in_, func, rd, wr, **kw):
        S.op("act", lambda e: e.activation(out, in_, func, **kw), reads=rd, writes=wr)

    def tt(eng, out, a, b, op, rd, wr):
        S.op(eng, lambda e: e.tensor_tensor(out, a, b, op), reads=rd, writes=wr)

    def ts(eng, out, a, s1, s2, op0, op1, rd, wr):
        if s2 is None:
            S.op(eng, lambda e: e.tensor_scalar(out, a, s1, None, op0), reads=rd, writes=wr)
        else:
            S.op(eng, lambda e: e.tensor_scalar(out, a, s1, s2, op0, op1), reads=rd, writes=wr)

    def stt(out, a, s, b, op0, op1, rd, wr):
        S.op("dve", lambda e: e.scalar_tensor_tensor(out, a, s, b, op0, op1), reads=rd, writes=wr)

    def cp(eng, out, in_, rd, wr):
        if eng == "act":
            S.op("act", lambda e: e.activation(out, in_, AF.Copy), reads=rd, writes=wr)
        else:
            S.op(eng, lambda e: e.tensor_copy(out, in_), reads=rd, writes=wr)

    def mm(lst, rd, wr):
        def f(e, lst=lst):
            ins = None
            for (o, l, r, st, sp) in lst:
                ins = e.matmul(o, lhsT=l, rhs=r, start=st, stop=sp)
            return ins
        S.op("pe", f, reads=rd, writes=wr)

    def trs(lst, rd, wr):
        def f(e, lst=lst):
            ins = None
            for (o, i, idn) in lst:
                ins = e.transpose(o, i, idn)
            return ins
        S.op("pe", f, reads=rd, writes=wr)

    ipb = [(S.ps(f"ip{i}", [128, 512], F32), S.buf(f"ip{i}")) for i in range(4)]
    ipn = [0]

    def bank():
        r = ipb[ipn[0] % 4]
        ipn[0] += 1
        return r
    sc_ps, Bsc = S.ps("sc", [128, 512], F32), S.buf("sc")
    kv_ps, Bkv = S.ps("kv", [128, 512], F32), S.buf("kv")
    o_ps, Bo = S.ps("o", [128, 512], F32), S.buf("o")
    trb_ps, Btrb = S.ps("trb", [128, 512], F32), S.buf("trb")

    consts = S.sb("consts", [128, NCONST], F32); Bc = S.buf("consts")
    S.dma("sp", consts, consts_d, writes=[Bc])

    def C(name, rows=128, lo=0, hi=None):
        o, n = CL[name]
        return consts[0:rows, o + lo: o + (n if hi is None else hi)]
    ident = C("ident")
    identb = None; Bib = S.buf()
    onesr = S.sb("onesr", [128, 128], F32R); Bor = S.buf()
    cp("dve", onesr, C("ones"), [Bc], [Bor])
    mhalf = S.sb("mhalf", [128, 1], F32); Bmh = S.buf()
    S.op("pool", lambda e: e.memset(mhalf, -0.5), writes=[Bmh])

    def small_in(name, src, shape):
        t = S.sb(name, shape, F32); b = S.buf(name)
        S.dma("sp", t, src, writes=[b])
        return t, b
    ccT, Bcc = small_in("ccT", cc_d, [128, 16])
    nmix, Bnm = small_in("nmix", nmix_d, [128, 8])
    lblfm, Blf = small_in("lblfm", lblfm_d, [128, 16])
    hgn, Bhg = small_in("hgn", hgn_d, [128, 1])
    convw, Bcw = small_in("convw", convw_d, [128, 12])
    wr, Bwr = small_in("wr", wr_d, [128, 128])
    nfin, Bnf = small_in("nfin", nfin_d, [128, 1024])
    lbfm = S.sb("lbfm", [128, 8], F32); omlfm = S.sb("omlfm", [128, 8], F32); nomlfm = S.sb("nomlfm", [128, 8], F32)
    tmp8 = S.sb("tmp8", [128, 8], F32); Bl = S.buf(); Bt8 = S.buf()
    tt("dve", tmp8, lblfm[:, 0:8], lblfm[:, 8:16], ALU.subtract, [Blf], [Bt8])
    act(lbfm, tmp8, AF.Sigmoid, [Bt8], [Bl])
    ts("dve", omlfm, lbfm, -1.0, 1.0, ALU.mult, ALU.add, [Bl], [Bl])
    ts("dve", nomlfm, lbfm, 1.0, -1.0, ALU.mult, ALU.add, [Bl], [Bl])

    S32 = S.sb("S32", [128, 2, 8, 128], F32R)
    S16 = None
    BS32 = [S.buf(f"S32_{i}") for i in range(8)]
    BS32b = [[BS32[i] for i in range(8)], [S.buf(f"S32b_{i}") for i in range(8)]]
    stver = [0, 0]
    BS16 = [[S.buf(f"S16_{v}_{i}") for i in range(8)] for v in range(2)]
    sver = [0] * 8

    A1 = S.sb("A1", [128, 8], F32); B1 = S.sb("B1", [128, 8], F32)
    Ac = S.sb("Acx", [128, 8], F32); Bcx = S.sb("Bcx", [128, 8], F32); Bab = S.buf("A1B1")
    g1bc = S.sb("g1bc", [128, 1024], F32); A2bc = S.sb("A2bc", [128, 1024], F32)
    B2bc = S.sb("B2bc", [128, 1024], F32); g2bc = S.sb("g2bc", [128, 1024], F32); Bbc = S.buf("bcs")

    sc = S.sb("silu_c", [128, 16], F32R); Bscx = S.buf()
    modT = S.sb("modT", [128, 32], F32); BmT = S.buf()
    ss_t = S.sb("ss_t", [128, 4], F32); Bss = [S.buf() for _ in range(4)]
    S.scope_push()
    wsl = [(S.sbs(f"wsl{i}", [128, 8, 512], F32R), S.buf(f"wsl{i}")) for i in range(2)]
    wsn = [0]

    def wslot():
        r = wsl[wsn[0] % 2]
        wsn[0] += 1
        return r
    junk = S.sbs("junk", [128, 1024], F32); Bjunk = S.buf()
    xsl = [(S.sbs(f"xsl{i}", [128, 1024], F32), S.buf(f"xsl{i}")) for i in range(2)]
    xn = S.sbs("xn", [128, 1024], F32); Bxn = S.buf()
    hxT = S.sbs("hxT", [128, 8, 512], F32R); BhxT = S.buf("hxT")
    S.scope_push()
    wada_v = wada_d.rearrange("(k p) n -> p k n", p=128)
    win_v = win_d.rearrange("(k p) n -> p k n", p=128)

    act(sc, ccT, AF.Silu, [Bcc], [Bscx])
    sc3 = sc.rearrange("p (k j) -> p k j", j=2)
    nffn = S.sbs("nffn", [128, 1024], F32); Bnff = S.buf()
    S.dma("sp", nffn, nffn_d, writes=[Bnff])
    mrow = [(S.sbs(f"mrow{i}", [2, 512], F32), S.sbs(f"brow{i}", [2, 512], F32), S.buf(), S.buf()) for i in range(2)]
    psT, BpT = trb_ps, Btrb
    bcdst = {4: (g1bc, 0, 0), 5: (g1bc, 1, 0), 6: (B2bc, 0, 0), 7: (B2bc, 1, 0), 8: (A2bc, 0, 1), 9: (A2bc, 1, 1),
             10: (g2bc, 0, 0), 11: (g2bc, 1, 0)}
    for j in range(12):
        wt, Bw = wslot()
        S.dma("sp", wt, wada_v[:, :, j * 512:(j + 1) * 512], writes=[Bw])
        mr, br, Bmr, Bbr = mrow[j % 2]
        S.dma("sp", br[0:1, :], bada_d[:, j * 512:(j + 1) * 512], writes=[Bbr])
        S.dma("sp", br[1:2, :], bada_d[:, j * 512:(j + 1) * 512], writes=[Bbr])
        ps, Bp = bank()
        mm([(ps[0:2, :], sc3[:, k, :], wt[:, k, :], k == 0, k == 7) for k in range(8)], [Bscx, Bw], [Bp])
        tt("dve", mr, ps[0:2, :], br, ALU.add, [Bp, Bbr], [Bmr])
        if dbg:
            S.dma("act", dbg_d["mod"][:, j * 512:(j + 1) * 512], mr, reads=[Bmr], slot=Bmr, out_final=True)
        if j < 4:
            trs([(psT[:, (j * 4 + c) * 2:(j * 4 + c + 1) * 2], mr[0:2, c * 128:(c + 1) * 128], ident[0:2, 0:2])
                 for c in range(4)], [Bmr, Bc], [BpT])
        else:
            dst, hf, kind = bcdst[j]
            ps2, Bp2 = bank()
            mm([(ps2, C("ones", rows=1), mr[0:1, :], True, True)], [Bmr, Bc], [Bp2])
            if kind == 0:
                cp("act", dst[:, hf * 512:(hf + 1) * 512], ps2, [Bp2], [Bbc])
            else:
                stt(dst[:, hf * 512:(hf + 1) * 512], ps2, 1.0, nffn[:, hf * 512:(hf + 1) * 512], ALU.add, ALU.mult,
                    [Bp2, Bnff], [Bbc])
        if j == 3:
            cp("dve", modT, psT[:, 0:32], [BpT], [BmT])
            modT3 = modT.rearrange("p (c j) -> p c j", j=2)
            stt(A1, modT3[:, 8:16, 0], 1.0, nmix, ALU.add, ALU.mult, [BmT, Bnm], [Bab])
            cp("dve", B1, modT3[:, 0:8, 0], [BmT], [Bab])
            stt(Ac, modT3[:, 8:16, 1], 1.0, nmix, ALU.add, ALU.mult, [BmT, Bnm], [Bab])
            cp("dve", Bcx, modT3[:, 0:8, 1], [BmT], [Bab])

    if stage == 0:
        S.dma("act", out_d[0:128, :], nfin, reads=[Bnf], slot=Bnf, out_final=True)
        S.finish()
        return nc
    ssn = [0]

    def rstd_of(xt, Bx, scr=None, Bscr=None):
        i = ssn[0] % 4
        ssn[0] += 1
        col = ss_t[:, i:i + 1]
        scr = junk if scr is None else scr
        Bscr = [Bjunk] if Bscr is None else Bscr
        Bx = Bx if isinstance(Bx, list) else [Bx]
        act(scr, xt, AF.Square, Bx, Bscr)
        S.op("dve", lambda e: e.reduce_sum(col, scr, AX.X), reads=Bscr, writes=[Bss[i]])
        ts("dve", col, col, 1.0 / D_MODEL, EPS, ALU.mult, ALU.add, [Bss[i]], [Bss[i]])
        act(col, col, AF.Sqrt, [Bss[i]], [Bss[i]])
        S.op("dve", lambda e: e.reciprocal(col, col), reads=[Bss[i]], writes=[Bss[i]])
        return col, Bss[i]

    def prep_tile(src_rows, An, Bn, col0, xi):
        xt, Bx = xsl[xi % 2]
        S.dma("sp", xt, src_rows, writes=[Bx])
        rs, Brs = rstd_of(xt, Bx)
        act(xn, xt, AF.Copy, [Bx, Brs], [Bxn], scale=rs)
        for half in range(2):
            ps, Bp = bank()
            trs([(ps[:, q * 128:(q + 1) * 128], xn[:, (half * 4 + q) * 128:(half * 4 + q + 1) * 128], ident)
                 for q in range(4)], [Bxn, Bc], [Bp])
            for q in range(4):
                k = half * 4 + q
                act(hxT[:, k, col0:col0 + 128], ps[:, q * 128:(q + 1) * 128], AF.Identity, [Bp, Bab], [BhxT],
                    scale=An[:, k:k + 1], bias=Bn[:, k:k + 1])

    lblbc = S.sbs("lblbc", [128, 2048], F32); Blb = S.buf()
    S.dma("sp", lblbc, lblbc_d, writes=[Blb])
    lbbc = S.sbs("lbbc", [128, 1024], F32); omlbc = S.sbs("omlbc", [128, 1024], F32); Blbb = S.buf()
    tt("dve", omlbc, lblbc[:, 0:1024], lblbc[:, 1024:2048], ALU.subtract, [Blb], [Blbb])
    act(lbbc, omlbc, AF.Sigmoid, [Blbb], [Blbb])
    ts("dve", omlbc, lbbc, -1.0, 1.0, ALU.mult, ALU.add, [Blbb], [Blbb])
    clogf = S.sbs("clogf", [128, 2, 2, 512], F32R); ck = S.sbs("ck", [128, 2, 2, 512], F32)
    cv16 = S.sbs("cv16", [128, 2, 512], F32R); ckd16 = S.sbs("ckd16", [128, 2, 2, 512], F32R)
    Bclf = [[S.buf() for _ in range(2)] for _ in range(2)]; Bck = [[S.buf() for _ in range(2)] for _ in range(2)]
    Bcv = [S.buf() for _ in range(2)]; Bckd = [[S.buf() for _ in range(2)] for _ in range(2)]
    csig = S.sbs("csig", [128, 512], F32); Bcs = S.buf()
    cf = S.sbs("cf", [128, 512], F32); Bcf = S.buf()
    import os
    KCUT = int(os.environ.get("KCUT", "0"))
    for i in range(2):
        prep_tile(ctx_d[i * 128:(i + 1) * 128, :], Ac, Bcx, i * 128, i)
    if KCUT == 1:
        S.dma("act", out_d[0:128, :], nfin, reads=[Bnf], slot=Bnf, out_final=True)
        S.scope_pop(); S.scope_pop()
        S.finish()
        return nc
    for j in (1, 2, 3):
        wt, Bw = wslot()
        S.dma("sp", wt, win_v[:, :, j * 512:(j + 1) * 512], writes=[Bw])
        for i in range(2):
            ps, Bp = bank()
            mm([(ps, hxT[:, k, i * 128:(i + 1) * 128], wt[:, k, :], k == 0, k == 7) for k in range(8)],
               [BhxT, Bw], [Bp])
            if j == 3:
                cp("act", cv16[:, i, :], ps, [Bp], [Bcv[i]])
            else:
                d = j - 1
                act(csig, ps, AF.Sigmoid, [Bp], [Bcs])
                tt("dve", cf, csig, omlbc[:, d * 512:(d + 1) * 512], ALU.mult, [Bcs, Blbb], [Bcf])
                tt("dve", cf, cf, lbbc[:, d * 512:(d + 1) * 512], ALU.add, [Bcf, Blbb], [Bcf])
                act(clogf[:, d, i, :], cf, AF.Ln, [Bcf], [Bclf[d][i]])
                ts("dve", ck[:, d, i, :], cf, -1.0, 1.0, ALU.mult, ALU.add, [Bcf], [Bck[d][i]])
    if KCUT == 2:
        S.dma("act", out_d[0:128, :], nfin, reads=[Bnf], slot=Bnf, out_final=True)
        S.scope_pop(); S.scope_pop()
        S.finish()
        return nc
    ones_f = onesr
    trir = S.sbs("trir", [128, 256], F32R); Btri = S.buf()
    cp("dve", trir[:, 0:128], C("mgt"), [Bc], [Btri])
    cp("dve", trir[:, 128:256], C("mlt"), [Bc], [Btri])
    for d in range(2):
        for i in range(2):
            ps, Bp = bank()
            tri = trir[:, 0:128] if d == 0 else trir[:, 128:256]
            other = 1 - i
            lst = [(ps, tri, clogf[:, d, i, :], True, False)]
            if (d == 0 and i == 0) or (d == 1 and i == 1):
                lst.append((ps, ones_f, clogf[:, d, other, :], False, True))
                rd = [Bclf[d][0], Bclf[d][1], Btri, Bor]
            else:
                lst[0] = (ps, tri, clogf[:, d, i, :], True, True)
                rd = [Bclf[d][i], Btri]
            mm(lst, rd, [Bp])
            act(csig, ps, AF.Exp, [Bp], [Bcs])
            tt("dve", ckd16[:, d, i, :], ck[:, d, i, :], csig, ALU.mult, [Bck[d][i], Bcs], [Bckd[d][i]])
    if KCUT == 3:
        S.dma("act", out_d[0:128, :], nfin, reads=[Bnf], slot=Bnf, out_final=True)
        S.scope_pop(); S.scope_pop()
        S.finish()
        return nc
    for d in range(2):
        for h in range(4):
            ps, Bp = bank()
            hs = slice(h * 128, (h + 1) * 128)
            mm([(ps[:, 0:128], ckd16[:, d, i, hs], cv16[:, i, hs], i == 0, i == 1) for i in range(2)],
               [Bckd[d][0], Bckd[d][1], Bcv[0], Bcv[1]], [Bp])
            cp("dve", S32[:, 0, d * 4 + h, :], ps[:, 0:128], [Bp], [BS32[d * 4 + h]])
    if dbg and stage == 1:
        for nm, tns, shp, bufs in (("clogf", clogf, [128, 2048], [x for y in Bclf for x in y]), ("ck", ck, [128, 2048], [x for y in Bck for x in y]),
                                   ("ckd", ckd16, [128, 2048], [x for y in Bckd for x in y]), ("cv", cv16, [128, 1024], Bcv),
                                   )[:int(os.environ.get("NDBG", "4"))]:
            dd_ = dscr("dbg_" + nm, shp, F32, out=True)
            flat = tns.bitcast(F32) if nm != "ck" else tns
            if flat.ndim == 4:
                flat = flat.rearrange("p a b c -> p (a b c)")
            elif flat.ndim == 3:
                flat = flat.rearrange("p a b -> p (a b)")
            S.dma("sp", dd_, flat, reads=bufs, slot=S.buf(), out_final=True)
    if dbg and stage == 1:
        for hh in range(2):
            dd_ = dscr(f"dbg_hcT{hh}", [128, 2048], F32, out=True)
            S.dma("sp", dd_, hxT.bitcast(F32)[:, hh * 4:(hh + 1) * 4, :].rearrange("p a b -> p (a b)"), reads=[BhxT], slot=S.buf(), out_final=True)
        dd_ = dscr("dbg_xn", [128, 1024], F32, out=True)
        S.dma("sp", dd_, xn, reads=[Bxn], slot=S.buf(), out_final=True)
        dd_ = dscr("dbg_ss", [128, 4], F32, out=True)
        S.dma("sp", dd_, ss_t, reads=Bss, slot=S.buf(), out_final=True)
    if KCUT == 4:
        S.dma("act", out_d[0:128, :], nfin, reads=[Bnf], slot=Bnf, out_final=True)
        S.scope_pop(); S.scope_pop()
        S.finish()
        return nc
    if dbg:
        S.dma("act", dbg_d["s0"].rearrange("s p v -> p s v"), S32.bitcast(F32)[:, 0, :, :], reads=BS32, slot=BS32[0], out_final=True)
    S.scope_pop()
    if stage == 1:
        S.dma("act", out_d[0:128, :], nfin, reads=[Bnf], slot=Bnf, out_final=True)
        S.finish()
        return nc
    return _build_rest(nc, S, locals(), stage, dbg)


def _host_layouts(inputs):
    f = lambda a: np.ascontiguousarray(np.asarray(a, dtype=np.float32))
    fm = lambda v: f(np.asarray(v).reshape(-1, 128).T)
    sh = {}
    sh["w_ada"] = f(inputs["w_ada"][0]); sh["b_ada"] = f(inputs["b_ada"][0]).reshape(1, 6144)
    sh["nmix_fm"] = fm(inputs["norm_mix"][0])
    sh["nffn_bc"] = f(np.broadcast_to(np.asarray(inputs["norm_ffn"][0])[None, :], (128, 1024)))
    sh["nfin_bc"] = f(np.broadcast_to(np.asarray(inputs["norm_final"])[None, :], (128, 1024)))
    sh["w_in"] = f(inputs["w_in"][0])
    lbl = np.asarray(inputs["lb_logits"], dtype=np.float32)
    sh["lbl_fm"] = f(np.concatenate([fm(lbl[0].reshape(-1)), fm(lbl[1].reshape(-1))], axis=1))
    sh["lbl_bc"] = f(np.broadcast_to(lbl.reshape(1, 2048), (128, 2048)))
    sh["hgn_fm"] = f(np.asarray(inputs["hg_norm"][0]).reshape(128, 1))
    cw = np.asarray(inputs["conv_w"][0], dtype=np.float32)
    sh["convw_fm"] = f(cw.reshape(3, 4, 128).transpose(2, 1, 0).reshape(128, 12))
    sh["w_out"] = f(inputs["w_out"][0])
    wr = np.asarray(inputs["w_router"][0], dtype=np.float32)
    sh["w_r_fm"] = f(wr.reshape(8, 128, 16).transpose(1, 0, 2).reshape(128, 128))
    sh["w_gate"] = f(inputs["w_gate"][0]); sh["w_up"] = f(inputs["w_up"][0]); sh["w_down"] = f(inputs["w_down"][0])
    sh["consts"] = make_consts()
    return sh


def _core_inputs(inputs, sh, b):
    f = lambda a: np.ascontiguousarray(np.asarray(a, dtype=np.float32))
    m = dict(sh)
    m["x"] = f(inputs["x"][b]); m["ctx"] = f(inputs["ctx"][b])
    cc = np.stack([np.asarray(inputs["c"][b]).reshape(8, 128).T, np.asarray(inputs["c_ctx"]).reshape(8, 128).T],
                  axis=-1)
    m["ccT"] = f(cc.reshape(128, 16))
    return m


_PROG = {}


def kernel(**inputs):
    if "full" not in _PROG:
        _PROG["full"] = build_program()
    nc = _PROG["full"]
    sh = _host_layouts(inputs)
    in_maps = [_core_inputs(inputs, sh, b) for b in range(8)]
    if not HAS_MOE:
        for m in in_maps:
            for k in ("w_gate", "w_up", "w_down"):
                m.pop(k, None)
    res = run_bass_kernel_spmd(nc, in_maps, core_ids=list(range(8)))
    return np.stack([np.asarray(r["out"], dtype=np.float32) for r in res.results], axis=0)


def _build_rest(nc, S, L, stage, dbg):
    g = lambda n: L[n]
    (act, tt, ts, stt, cp, mm, trs, bank, C, wslot, prep_tile, rstd_of) = [g(n) for n in (
        "act", "tt", "ts", "stt", "cp", "mm", "trs", "bank", "C", "wslot", "prep_tile", "rstd_of")]
    (x_d, out_d, x2_d, hx2_d, win_v, wout_d, dbg_d, st_of) = [g(n) for n in (
        "x_d", "out_d", "x2_d", "hx2_d", "win_v", "wout_d", "dbg_d", "st_of")]
    (ident, identb, onesr, Bc, Bib, Bor, lbfm, omlfm, nomlfm, Bl, S32, S16, BS32, BS16, sver, A1, B1, Bab,
     g1bc, A2bc, B2bc, g2bc, Bbc, hxT, BhxT, xsl, xn, Bxn, junk, Bjunk, hgn, Bhg, convw, Bcw, wr, Bwr, nfin, Bnf,
     sc_ps, Bsc, kv_ps, Bkv, o_ps, Bo, trb_ps, Btrb) = [g(n) for n in (
         "ident", "identb", "onesr", "Bc", "Bib", "Bor", "lbfm", "omlfm", "nomlfm", "Bl", "S32", "S16", "BS32",
         "BS16", "sver", "A1", "B1", "Bab", "g1bc", "A2bc", "B2bc", "g2bc", "Bbc", "hxT", "BhxT", "xsl", "xn",
         "Bxn", "junk", "Bjunk", "hgn", "Bhg", "convw", "Bcw", "wr", "Bwr", "nfin", "Bnf",
         "sc_ps", "Bsc", "kv_ps", "Bkv", "o_ps", "Bo", "trb_ps", "Btrb")]
    aff_d = nc.dram_tensor("aff_scr", [16, T_SEQ], F32, kind=("ExternalOutput" if dbg else "Internal")).ap()
    wout_v = wout_d.rearrange("(k p) n -> p k n", p=128)
    xcnt = [0]

    S.scope_push()
    TT4 = S.sbs("TT4", [128, 4, 512], F32)
    Ta, Tb, Tc, Td = TT4[:, 0, :], TT4[:, 1, :], TT4[:, 2, :], TT4[:, 3, :]
    BTa, BTb, BTc, BTd = S.buf(), S.buf(), S.buf(), S.buf()
    kend16 = S.sbs("kend16", [128, 512], F32); Bke16 = S.buf()
    Eall = S.sbs("Eall", [128, 4, 512], F32); BE = [S.buf() for _ in range(4)]
    QD = S.sbs("QD", [128, 4, 512], F32R); BQD = [S.buf() for _ in range(4)]
    KI = S.sbs("KI", [128, 4, 512], F32R); BKI = [S.buf() for _ in range(4)]
    KE = S.sbs("KE", [128, 4, 4, 128], F32R); BKE = [S.buf() for _ in range(4)]
    EL = S.sbs("EL", [128, 4, 16], F32); BEL = [S.buf() for _ in range(4)]
    V16 = S.sbs("V16", [128, 4, 512], F32R); BV = [S.buf() for _ in range(4)]
    VMs = [(S.sbs(f"VM{i}", [128, 512], F32R), S.buf(f"VM{i}")) for i in range(2)]
    vmn = [0]
    AT16 = S.sbs("AT16", [128, 512], F32R); BAT = S.buf()
    OB = S.sbs("OB", [128, 4, 512], F32); BOB = [S.buf() for _ in range(4)]
    SG = S.sbs("SG", [128, 4, 512], BF16); BSG = [S.buf() for _ in range(4)]
    cvs = S.sbs("cvs", [128, 4, 512], F32); Bcvs = [S.buf() for _ in range(4)]
    MIXT = S.sbs("MIXT", [128, 8, 512], F32R); BMX = [S.buf() for _ in range(8)]
    MIXTf = MIXT.bitcast(F32)
    hx2T = S.sbs("hx2T", [128, 8, 128], F32); Bh2T = S.buf()
    osq = S.sbs("osq", [128, 512], F32R); Bosq = S.buf()
    affsb = S.sbs("affsb", [16, 128], F32); Baf = S.buf()
    eT = S.sbs("eT", [16, 128], F32); BeT = S.buf()
    Bstash = [S.buf(f"stash{i}") for i in range(8)]
    eT2 = S.sbs("eT2", [16, 128], F32); BeT2 = S.buf()
    affsb2 = S.sbs("affsb2", [16, 128], F32); Baf2 = S.buf()
    epn = [0]
    xnb = [(xn, [Bxn]), (TT4[:, 0:2, :].rearrange("p a b -> p (a b)"), [BTa, BTb])]
    jkb = [(junk, [Bjunk]), (TT4[:, 2:4, :].rearrange("p a b -> p (a b)"), [BTc, BTd])]
    h2b = [(hx2T, [Bh2T]), (cvs[:, 0:2, :].rearrange("p a (k t) -> p (a k) t", t=128), [Bcvs[0], Bcvs[1]])]
    smb = [(eT, BeT, affsb, Baf), (eT2, BeT2, affsb2, Baf2)]

    def gate_prep(d, h, ps, Bp):
        col = d * 4 + h
        act(Ta, ps, AF.Sigmoid, [Bp], [BTa])
        act(Tb, Ta, AF.Ln, [BTa, Bl], [BTb], scale=omlfm[:, col:col + 1], bias=lbfm[:, col:col + 1])
        ts("dve", Tc, Ta, nomlfm[:, col:col + 1], omlfm[:, col:col + 1], ALU.mult, ALU.add, [BTa, Bl], [BTc])
        if d == 0:
            S.op("dve", lambda e: e.tensor_tensor_scan(Td, C("rsf"), Tb, 0.0, ALU.mult, ALU.add),
                 reads=[BTb, Bc], writes=[BTd])
        else:
            S.op("dve", lambda e: e.tensor_tensor_scan(Td[:, ::-1], C("rsb")[:, ::-1], Tb[:, ::-1], 0.0,
                                                       ALU.mult, ALU.add), reads=[BTb, Bc], writes=[BTd])
        act(Eall[:, h, :], Td, AF.Exp, [BTd], [BE[h]])
        act(Ta, Td, AF.Exp, [BTd], [BTa], scale=-1.0)
        tt("dve", Tc, Tc, Ta, ALU.mult, [BTc, BTa], [BTc])
        cp("act", KI[:, h, :], Tc, [BTc], [BKI[h]])
        E3 = Eall[:, h, :].rearrange("p (c k) -> p c k", k=32)
        cp("pool", EL[:, h, :], E3[:, :, 31] if d == 0 else E3[:, :, 0], [BE[h]], [BEL[h]])
        elb = EL[:, h, :].rearrange("p (c o) -> p c o", o=1).to_broadcast([128, 16, 32])
        tt("dve", kend16.rearrange("p (c k) -> p c k", k=32), Tc.rearrange("p (c k) -> p c k", k=32), elb,
           ALU.mult, [BTc, BEL[h]], [Bke16])
        trs([(trb_ps[:, t * 128:(t + 1) * 128], kend16[:, t * 128:(t + 1) * 128], ident) for t in range(4)],
            [Bke16, Bc], [Btrb])
        cp("act", KE[:, h, :, :].rearrange("p t d -> p (t d)"), trb_ps[:, 0:512], [Btrb], [BKE[h]])

    kvb = [(kv_ps, Bkv), (trb_ps, Btrb)]
    BS32b = L["BS32b"]; stver = L["stver"]
    S32f = S32.bitcast(F32)
    V16f = V16.bitcast(F32)

    def gla_group(d):
        order = list(range(4)) if d == 0 else [3, 2, 1, 0]
        mk = (C("maskf") if d == 0 else C("maskb")).rearrange("p (o q) -> p o q", o=1).to_broadcast([128, 4, 128])
        hsl = [slice(h * 128, (h + 1) * 128) for h in range(4)]
        for t in order:
            tcol = slice(t * 128, (t + 1) * 128)
            mm([(sc_ps[:, hsl[h]], KI[:, h, tcol], QD[:, h, tcol], True, True) for h in range(4)], BKI + BQD, [Bsc])
            tt("dve", AT16.rearrange("p (t q) -> p t q", q=128), sc_ps.rearrange("p (t q) -> p t q", q=128), mk, ALU.mult,
               [Bsc, Bc], [BAT])
            mm([(o_ps[:, hsl[h]], V16[:, t, hsl[h]], AT16[:, hsl[h]], h == 0, False) for h in range(4)], [BV[t], BAT], [Bo])
            for ci, cc in enumerate(order):
                VMb, BVMb = VMs[vmn[0] % 2]
                kvp, Bkvp = kvb[vmn[0] % 2]
                vmn[0] += 1
                act(VMb, V16f[:, t, :], AF.Copy, [BV[t], Bc], [BVMb], scale=C("cind")[:, cc:cc + 1])
                mm([(kvp[:, hsl[h]], KE[:, h, t, :], VMb[:, hsl[h]], True, True) for h in range(4)], BKE + [BVMb], [Bkvp])
                v = stver[d]
                Bcur = [BS32b[v][d * 4 + h] for h in range(4)]
                Bnxt = [BS32b[1 - v][d * 4 + h] for h in range(4)]
                mm([(o_ps[:, h * 128 + cc * 32: h * 128 + cc * 32 + 32], S32[:, v, d * 4 + h, :],
                     QD[:, h, t * 128 + cc * 32: t * 128 + cc * 32 + 32], False, ci == 3) for h in range(4)],
                   Bcur + BQD, [Bo])
                chunk = t * 4 + cc

                def upd(e, kvp=kvp, chunk=chunk, v=v):
                    ins = None
                    for h in range(4):
                        col = d * 4 + h
                        ins = e.scalar_tensor_tensor(S32[:, 1 - v, col, :], S32f[:, v, col, :], EL[:, h, chunk:chunk + 1],
                                                     kvp[:, hsl[h]], ALU.mult, ALU.add)
                    return ins
                S.op("dve", upd, reads=Bcur + BEL + [Bkvp], writes=Bnxt)
                stver[d] = 1 - v
            o3 = o_ps.rearrange("p (h q) -> p h q", q=128)
            if d == 1:
                cp("act", OB[:, :, tcol], o3, [Bo], BOB)
            else:
                tt("dve", OB[:, :, tcol], o3, OB[:, :, tcol], ALU.add, [Bo] + BOB, BOB)

    def sweep(d):
        groups = range(8) if d == 0 else range(7, -1, -1)
        pieces = [1, 0, 3, 4, 7, 6, 5] if d == 0 else [2, 0, 3]
        groups = list(groups)

        def xprep(gi):
            for t in range(4):
                r0 = (gi * 4 + t) * 128
                prep_tile(x_d[r0:r0 + 128, :], A1, B1, t * 128, xcnt[0])
                xcnt[0] += 1
        xprep(groups[0])
        for gn, gi in enumerate(groups):
            if d == 0:
                S.dma("sp", OB, st_of[gi], reads=[Bstash[gi]], writes=BOB, slot=BOB[0])
            for j in pieces:
                wt, Bw = wslot()
                S.dma("sp", wt, win_v[:, :, j * 512:(j + 1) * 512], writes=[Bw])
                for q in range(4):
                    ps, Bp = bank()
                    if j == 3:
                        mm([(ps, hxT[:, k, q * 128:(q + 1) * 128], wt[:, k, :], k == 0, k == 7) for k in range(8)],
                           [BhxT, Bw], [Bp])
                        cp("act", V16[:, q, :], ps, [Bp], [BV[q]])
                        continue
                    mm([(ps, wt[:, k, q * 128:(q + 1) * 128], hxT[:, k, :], k == 0, k == 7) for k in range(8)],
                       [BhxT, Bw], [Bp])
                    if j in (1, 2):
                        gate_prep(d, q, ps, Bp)
                    elif j == 0:
                        tt("dve", QD[:, q, :], ps, Eall[:, q, :], ALU.mult, [Bp, BE[q]], [BQD[q]])
                    elif j == 4:
                        act(SG[:, q, :], ps, AF.Silu, [Bp], [BSG[q]])
                    elif j == 7:
                        cp("act", cvs[:, q, :], ps, [Bp], [Bcvs[q]])
                    elif j == 6:
                        u = cvs[:, q, :]
                        y = Eall[:, q, :]
                        tt("dve", u, ps, u, ALU.mult, [Bp, Bcvs[q]], [Bcvs[q]])
                        ts("dve", y, u, convw[:, q * 3 + 1:q * 3 + 2], None, ALU.mult, None, [Bcvs[q], Bcw], [BE[q]])
                        u3 = u.rearrange("p (r w) -> p r w", w=64); y3 = y.rearrange("p (r w) -> p r w", w=64)
                        stt(y3[:, :, 1:64], u3[:, :, 0:63], convw[:, q * 3:q * 3 + 1], y3[:, :, 1:64], ALU.mult, ALU.add,
                            [Bcvs[q], Bcw, BE[q]], [BE[q]])
                        stt(y3[:, :, 0:63], u3[:, :, 1:64], convw[:, q * 3 + 2:q * 3 + 3], y3[:, :, 0:63], ALU.mult,
                            ALU.add, [Bcvs[q], Bcw, BE[q]], [BE[q]])
                    elif j == 5:
                        tt("dve", MIXT[:, 4 + q, :], ps, Eall[:, q, :], ALU.mult, [Bp, BE[q]], [BMX[4 + q]])
            if gn + 1 < len(groups):
                xprep(groups[gn + 1])
            gla_group(d)
            if d == 0:
                for h in range(4):
                    osum = OB[:, h, :]
                    act(osq, osum, AF.Square, [BOB[h]], [Bosq])
                    ps, Bp = bank()
                    mm([(ps, onesr, osq, True, True)], [Bosq, Bor], [Bp])
                    ts("dve", Td, ps, 1.0 / 128.0, EPS, ALU.mult, ALU.add, [Bp], [BTd])
                    act(Td, Td, AF.Sqrt, [BTd], [BTd])
                    S.op("dve", lambda e: e.reciprocal(Td, Td), reads=[BTd], writes=[BTd])
                    tt("dve", Tb, osum, Td, ALU.mult, [BOB[h], BTd], [BTb])
                    stt(MIXT[:, h, :], Tb, hgn[:, 0:1], SG[:, h, :], ALU.mult, ALU.mult, [BTb, Bhg, BSG[h]], [BMX[h]])
            if d == 1:
                S.dma("act", st_of[gi], OB, reads=BOB, writes=[Bstash[gi]], slot=BOB[0])
                if dbg:
                    S.dma("act", dbg_d["of"][gi], OB, reads=BOB, slot=BOB[0], out_final=True)
                continue
            wo = []
            for hf in range(2):
                wt, Bw = wslot()
                S.dma("sp", wt, wout_v[:, :, hf * 512:(hf + 1) * 512], writes=[Bw])
                wo.append((wt, Bw))
            for t in range(4):
                r0 = (gi * 4 + t) * 128
                xt, Bx = xsl[xcnt[0] % 2]
                xcnt[0] += 1
                par = epn[0] % 2
                epn[0] += 1
                x1, Bx1 = xnb[par]
                hx, Bhx = jkb[par]
                h2T, Bh2 = h2b[par]
                eTt, BeTt, aft, Baft = smb[par]
                S.dma("sp", xt, x_d[r0:r0 + 128, :], writes=[Bx])
                for hf in range(2):
                    ps, Bp = bank()
                    wt, Bw = wo[hf]
                    mm([(ps, MIXT[:, kc, t * 128:(t + 1) * 128], wt[:, kc, :], kc == 0, kc == 7) for kc in range(8)],
                       BMX + [Bw], [Bp])
                    hsl = slice(hf * 512, (hf + 1) * 512)
                    tt("dve", x1[:, hsl], ps, g1bc[:, hsl], ALU.mult, [Bp, Bbc], Bx1)
                    tt("pool", x1[:, hsl], x1[:, hsl], xt[:, hsl], ALU.add, Bx1 + [Bx], Bx1)
                S.dma("act", x2_d[r0:r0 + 128, :], x1, reads=Bx1, slot=Bx1[0], out_final=dbg)
                rs, Brs = rstd_of(x1, Bx1, hx, Bhx)
                stt(hx, x1, rs, A2bc, ALU.mult, ALU.mult, Bx1 + [Brs, Bbc], Bhx)
                tt("pool", hx, hx, B2bc, ALU.add, Bhx + [Bbc], Bhx)
                S.dma("act", hx2_d[r0:r0 + 128, :], hx, reads=Bhx, slot=Bhx[0], out_final=dbg)
                for half in range(2):
                    ps, Bp = bank()
                    trs([(ps[:, q * 128:(q + 1) * 128], hx[:, (half * 4 + q) * 128:(half * 4 + q + 1) * 128], ident)
                         for q in range(4)], Bhx + [Bc], [Bp])
                    cp("act", h2T[:, half * 4:(half + 1) * 4, :].rearrange("p k t -> p (k t)"), ps, [Bp], Bh2)
                ps, Bp = bank()
                mm([(ps[0:16, 0:128], wr[:, k * 16:(k + 1) * 16], h2T[:, k, :], k == 0, k == 7) for k in range(8)],
                   [Bwr] + Bh2, [Bp])
                act(eTt, ps[0:16, 0:128], AF.Exp, [Bp], [BeTt])
                ps2, Bp2 = bank()
                mm([(ps2[0:16, 0:128], C("ones", rows=16, hi=16), eTt, True, True)], [BeTt, Bc], [Bp2])
                S.op("dve", lambda e, ps2=ps2, aft=aft: e.reciprocal(aft, ps2[0:16, 0:128]), reads=[Bp2], writes=[Baft])
                tt("dve", aft, aft, eTt, ALU.mult, [Baft, BeTt], [Baft])
                S.dma("act", aff_d[:, r0:r0 + 128], aft, reads=[Baft], slot=Baft, out_final=dbg)

    sweep(1)
    if stage == 2:
        S.dma("act", out_d[0:128, :], nfin, reads=[Bnf], slot=Bnf, out_final=True)
        S.scope_pop(); S.scope_pop()
        S.finish()
        return nc
    sweep(0)
    S.scope_pop()
    S.scope_pop()
    if stage == 3:
        S.dma("act", out_d[0:128, :], nfin, reads=[Bnf], slot=Bnf, out_final=True)
        S.finish()
        return nc
    return _build_moe(nc, S, L, locals(), stage, dbg)


def _build_moe(nc, S, L, L2, stage, dbg):
    import concourse.bass as bass_
    act, tt, ts, stt, cp, mm, trs, C = [L[n] for n in ("act", "tt", "ts", "stt", "cp", "mm", "trs", "C")]
    x2_d, hx2_d, out_d, nfin, Bnf, g2bc, Bbc, Bc, ident, ipb = [L[n] for n in (
        "x2_d", "hx2_d", "out_d", "nfin", "Bnf", "g2bc", "Bbc", "Bc", "ident", "ipb")]
    wg_d, wu_d, wd_d = L["wg_d"], L["wu_d"], L["wd_d"]
    aff_d = L2["aff_d"]
    yacc = [(L["sc_ps"], L["Bsc"]), (L["kv_ps"], L["Bkv"]), (L["o_ps"], L["Bo"]), (L["trb_ps"], L["Btrb"])]
    R_ps, BR = ipb[3]
    rot = ipb[0:3]
    rn = [0]

    def bank3():
        r = rot[rn[0] % 3]
        rn[0] += 1
        return r
    Bx2all = S.buf("x2all")
    Bhx2all = S.buf("hx2all")

    S.scope_push()
    slotT = S.sbs("slotT", [128, 4, 128], F32); BslT = S.buf()
    affT = S.sbs("affT", [128, 4, 128], F32); BafT = S.buf()
    vals = S.sbs("vals", [128, 4, 128, 3], F32R); Bvals = S.buf()
    Rsb = S.sbs("Rsb", [3, 512], F32); BRsb = S.buf()
    IG = S.sbs("IG", [128, 12], F32); BIG = S.buf()
    idxs = [(S.sbs(f"idx{i}", [128, 4], I32), S.buf(f"idx{i}")) for i in range(2)]
    gates = [(S.sbs(f"gate{i}", [128, 4], F32), S.buf(f"gate{i}")) for i in range(2)]
    sm = S.sbs("smalls", [128, 16], F32); Bsm = S.buf()
    lo, hi, mid, cnt, ge, dl, nge, off = [sm[:, i:i + 1] for i in range(8)]
    S.scope_push()
    aff128 = S.sbs("aff128", [128, 512], F32); Ba128 = S.buf()
    msk = S.sbs("msk", [128, 512], F32); Bmsk = S.buf()
    cum = S.sbs("cum", [128, 512], F32); Bcum = S.buf()

    S.dma("sp", aff128, aff_d.rearrange("e (s t) -> (e s) t", t=512), writes=[Ba128])
    S.op("dve", lambda e: e.memset(sm, 0.0), writes=[Bsm])
    S.op("dve", lambda e: e.memset(hi, 2.0), reads=[Bsm], writes=[Bsm])
    for it in range(34):
        tt("dve", mid, lo, hi, ALU.add, [Bsm], [Bsm])
        ts("dve", mid, mid, 0.5, None, ALU.mult, None, [Bsm], [Bsm])
        ts("dve", msk, aff128, mid, None, ALU.is_ge, None, [Ba128, Bsm], [Bmsk])
        S.op("dve", lambda e: e.reduce_sum(cnt, msk, AX.X), reads=[Bmsk], writes=[Bsm])
        ps, Bp = bank3()
        mm([(ps[:, 0:1], C("bones"), cnt, True, True)], [Bc, Bsm], [Bp])
        ts("dve", ge, ps[:, 0:1], float(CAP), None, ALU.is_ge, None, [Bp], [Bsm])
        tt("dve", dl, mid, lo, ALU.subtract, [Bsm], [Bsm])
        stt(lo, dl, ge, lo, ALU.mult, ALU.add, [Bsm], [Bsm])
        tt("dve", dl, hi, mid, ALU.subtract, [Bsm], [Bsm])
        stt(hi, dl, ge, mid, ALU.mult, ALU.add, [Bsm], [Bsm])
    import os
    MOECUT = int(os.environ.get("MOECUT", "0"))
    MOEN = int(os.environ.get("MOEN", str(N_EXP)))
    ones512 = S.sbs("ones512", [128, 512], F32); Bo512 = S.buf()
    S.op("pool", lambda e: e.memset(ones512, 1.0), writes=[Bo512])
    ts("dve", msk, aff128, lo, None, ALU.is_ge, None, [Ba128, Bsm], [Bmsk])
    S.op("dve", lambda e: e.reduce_sum(cnt, msk, AX.X), reads=[Bmsk], writes=[Bsm])
    ps, Bp = bank3()
    mm([(ps[:, 0:1], C("segpre"), cnt, True, True)], [Bc, Bsm], [Bp])
    cp("dve", off, ps[:, 0:1], [Bp], [Bsm])
    S.op("dve", lambda e: e.tensor_tensor_scan(cum, ones512, msk, off, ALU.mult, ALU.add),
         reads=[Bmsk, Bsm, Bo512], writes=[Bcum])
    tt("dve", cum, cum, msk, ALU.mult, [Bcum, Bmsk], [Bcum])
    ts("dve", cum, cum, -1.0, None, ALU.add, None, [Bcum], [Bcum])
    for (src, Bsrc, dst, Bdst) in ((cum, Bcum, slotT, BslT), (aff128, Ba128, affT, BafT)):
        ps, Bp = bank3()
        trs([(ps[:, c * 128:(c + 1) * 128], src[:, c * 128:(c + 1) * 128], ident) for c in range(4)], [Bsrc, Bc], [Bp])
        cp("act", dst.rearrange("p c q -> p (c q)"), ps, [Bp], [Bdst])
    valsf = vals.bitcast(F32)
    cp("dve", vals[:, :, :, 1], affT, [BafT], [Bvals])
    tt("dve", vals[:, :, :, 2], affT, valsf[:, :, :, 1], ALU.subtract, [BafT, Bvals], [Bvals])
    tki = C("tokidx").rearrange("p (c o s) -> p c o s", c=4, o=1)
    for c in range(4):
        cp("dve", vals[:, c, :, 0].rearrange("p (e s) -> p e s", s=8), tki[:, c, :, :].to_broadcast([128, 16, 8]),
           [Bc], [Bvals])

    S.scope_pop()
    S.scope_push()
    wsl = [(S.sbs(f"mw{i}", [128, 8, 512], F32R), S.buf(f"mw{i}")) for i in range(3)]
    wn = [0]

    def wslot():
        r = wsl[wn[0] % 3]
        wn[0] += 1
        return r
    xsT = [(S.sbs(f"xsT{i}", [128, 8, 512], F32R), S.buf(f"xsT{i}")) for i in range(2)]
    hidT = S.sbs("hidT", [128, 16, 512], F32R); Bhid = [S.buf() for _ in range(16)]
    xstok = [(S.sbs(f"xstok{i}", [128, 1024], F32), S.buf(f"xstok{i}")) for i in range(2)]
    ysb = [(S.sbs(f"ysb{i}", [128, 1024], F32), S.buf(f"ysb{i}")) for i in range(4)]
    sgt = [(S.sbs(f"sgt{i}", [128, 512], F32), S.buf(f"sgt{i}")) for i in range(2)]
    Qb = [(S.sbs(f"Qb{i}", [128, 512], F32R), S.buf(f"Qb{i}")) for i in range(3)]

    if dbg:
        dbg_lo = nc.dram_tensor("dbg_lo", [128, 16], F32, kind="ExternalOutput").ap()
        dbg_idx = nc.dram_tensor("dbg_idx", [16, 128, 4], I32, kind="ExternalOutput").ap()
        dbg_gate = nc.dram_tensor("dbg_gate", [16, 128, 4], F32, kind="ExternalOutput").ap()
        dbg_slot = nc.dram_tensor("dbg_slot", [128, 512], F32, kind="ExternalOutput").ap()
        S.dma("sp", dbg_lo, sm, reads=[Bsm], slot=S.buf(), out_final=True)
        S.dma("sp", dbg_slot, slotT.rearrange("p c q -> p (c q)"), reads=[BslT], slot=S.buf(), out_final=True)

    def prep(e):
        xT, BxT = xsT[e % 2]
        idx, Bidx = idxs[e % 2]
        gat, Bgat = gates[e % 2]
        for i in range(32):
            s_, c = i // 4, i % 4
            col = e * 8 + s_
            Q, BQ = Qb[i % 3]
            ts("dve", Q, C("iota"), slotT[:, c, col:col + 1], None, ALU.is_equal, None,
               [Bc, BslT], [BQ])
            mm([(R_ps[0:3, :], vals[:, c, col, :], Q, i == 0, i == 31)], [Bvals, BQ], [BR])
            yield
        cp("act", Rsb, R_ps[0:3, :], [BR], [BRsb])
        ps, Bp = bank3()
        trs([(ps[:, b * 3:(b + 1) * 3], Rsb[0:3, b * 128:(b + 1) * 128], ident[0:3, 0:3]) for b in range(4)],
            [BRsb, Bc], [Bp])
        cp("dve", IG, ps[:, 0:12], [Bp], [BIG])
        IG3 = IG.rearrange("p (b j) -> p b j", j=3)
        cp("dve", idx, IG3[:, :, 0], [BIG], [Bidx])
        tt("dve", gat, IG3[:, :, 1], IG3[:, :, 2], ALU.add, [BIG], [Bgat])
        if dbg:
            S.dma("sp", dbg_idx[e], idx, reads=[Bidx], slot=S.buf(), out_final=True)
            S.dma("sp", dbg_gate[e], gat, reads=[Bgat], slot=S.buf(), out_final=True)
        yield
        for b in range(4):
            xt, Bx = xstok[b % 2]
            S.op_dma_ind("pool", lambda en, xt=xt, b=b: en.indirect_dma_start(
                out=xt, out_offset=None, in_=hx2_d, in_offset=bass_.IndirectOffsetOnAxis(ap=idx[:, b:b + 1], axis=0)),
                reads=[Bidx, Bhx2all], writes=[Bx])
            for half in range(2):
                ps, Bp = bank3()
                trs([(ps[:, q * 128:(q + 1) * 128], xt[:, (half * 4 + q) * 128:(half * 4 + q + 1) * 128], ident)
                     for q in range(4)], [Bx, Bc], [Bp])
                for q in range(4):
                    cp("act", xT[:, half * 4 + q, b * 128:(b + 1) * 128], ps[:, q * 128:(q + 1) * 128], [Bp], [BxT])
            yield

    def drain(gen):
        for _ in gen:
            pass

    def tick(gen):
        if gen is not None:
            next(gen, None)

    if MOECUT != 2:
        drain(prep(0))
    for e in range(MOEN if MOECUT not in (2, 3) else 0):
        gen = prep(e + 1) if e + 1 < MOEN else None
        xT, BxT = xsT[e % 2]
        idx, Bidx = idxs[e % 2]
        gat, Bgat = gates[e % 2]
        wgv = wg_d[e].rearrange("(k p) f -> p k f", p=128)
        wuv = wu_d[e].rearrange("(k p) f -> p k f", p=128)
        wdv = wd_d[e].rearrange("(g c p) d -> g p c d", p=128, c=4)
        for fg in range(4):
            wgt, Bwg = wslot()
            S.dma("sp", wgt, wgv[:, :, fg * 512:(fg + 1) * 512], writes=[Bwg])
            wut, Bwu = wslot()
            S.dma("sp", wut, wuv[:, :, fg * 512:(fg + 1) * 512], writes=[Bwu])
            for fc in range(4):
                f = fg * 4 + fc
                sg, Bsg = sgt[f % 2]
                ps, Bp = bank3()
                mm([(ps, wgt[:, k, fc * 128:(fc + 1) * 128], xT[:, k, :], k == 0, k == 7) for k in range(8)],
                   [Bwg, BxT], [Bp])
                act(sg, ps, AF.Silu, [Bp], [Bsg])
                tick(gen)
                ps2, Bp2 = bank3()
                mm([(ps2, wut[:, k, fc * 128:(fc + 1) * 128], xT[:, k, :], k == 0, k == 7) for k in range(8)],
                   [Bwu, BxT], [Bp2])
                tt("dve", hidT[:, f, :], ps2, sg, ALU.mult, [Bp2, Bsg], [Bhid[f]])
                tick(gen)
        for dh in range(2):
            for fg in range(4):
                wt, Bw = wslot()
                wt4 = wt.rearrange("p (a c) d -> p a c d", a=2)[:, 0, :, :]
                S.dma("sp", wt4, wdv[fg][:, :, dh * 512:(dh + 1) * 512], writes=[Bw])
                for t4 in range(4):
                    ya, Bya = yacc[t4]
                    mm([(ya, hidT[:, fg * 4 + fc, t4 * 128:(t4 + 1) * 128], wt4[:, fc, :], fg == 0 and fc == 0,
                         fg == 3 and fc == 3) for fc in range(4)], [Bhid[fg * 4 + fc] for fc in range(4)] + [Bw], [Bya])
                    tick(gen)
            for t4 in range(4):
                ya, Bya = yacc[t4]
                yt, By = ysb[t4]
                hsl = slice(dh * 512, (dh + 1) * 512)
                stt(yt[:, hsl], ya, gat[:, t4:t4 + 1], g2bc[:, hsl], ALU.mult, ALU.mult, [Bya, Bgat, Bbc], [By])
        for t4 in range(4):
            yt, By = ysb[t4]
            S.op_dma_ind("pool", lambda en, yt=yt, t4=t4, idx=idx: en.indirect_dma_start(
                out=x2_d, out_offset=bass_.IndirectOffsetOnAxis(ap=idx[:, t4:t4 + 1], axis=0), in_=yt, in_offset=None,
                compute_op=ALU.add), reads=[Bidx, By, Bx2all], writes=[Bx2all], slot=By)
        if gen is not None:
            drain(gen)
    S.scope_pop()
    S.scope_pop()

    S.scope_push()
    xs = [(S.sbs(f"fx{i}", [128, 1024], F32), S.buf()) for i in range(2)]
    ys = [(S.sbs(f"fy{i}", [128, 1024], F32), S.buf()) for i in range(2)]
    jk = S.sbs("fjunk", [128, 1024], F32); Bjk = S.buf()
    ss = S.sbs("fss", [128, 4], F32); Bs4 = [S.buf() for _ in range(4)]
    for t in range(32):
        xt, Bx = xs[t % 2]; yt, By = ys[t % 2]
        S.dma("sp", xt, x2_d[t * 128:(t + 1) * 128, :], reads=[Bx2all], writes=[Bx])
        col = ss[:, t % 4:t % 4 + 1]; Bcol = Bs4[t % 4]
        act(jk, xt, AF.Square, [Bx], [Bjk])
        S.op("dve", lambda en, col=col: en.reduce_sum(col, jk, AX.X), reads=[Bjk], writes=[Bcol])
        ts("dve", col, col, 1.0 / D_MODEL, EPS, ALU.mult, ALU.add, [Bcol], [Bcol])
        act(col, col, AF.Sqrt, [Bcol], [Bcol])
        S.op("dve", lambda en, col=col: en.reciprocal(col, col), reads=[Bcol], writes=[Bcol])
        stt(yt, xt, col, nfin, ALU.mult, ALU.mult, [Bx, Bcol, Bnf], [By])
        S.dma("act", out_d[t * 128:(t + 1) * 128, :], yt, reads=[By], slot=By, out_final=True)
    S.scope_pop()
    S.finish()
    return nc
```
